# Optimizing a Trainium2 kernel written in Bass

```python
import math
import jax
import jax.numpy as jnp
from jax import lax
import numpy as np


D_MODEL = 2048
BATCH = 4
SEQ = 4096
DEPTH = 2

HEAD_DIM = 128
W_A = D_MODEL // 4
CONV_A = 3
DH_B = 64
DV_B = 2 * DH_B
H_B = (3 * D_MODEL // 8) // DV_B
W_B = H_B * DV_B
ROT_DIM = DH_B // 4
ROPE_THETA = 500000.0
Q_BLOCK = 128
DK_C = HEAD_DIM
DV_C = HEAD_DIM
H_C = (3 * D_MODEL // 8) // DV_C
W_C = H_C * DV_C
CONV_C = 3
CHUNK = 64
N_BRANCH = 3
N_EXPERTS = 16
CAPACITY_FACTOR = 2
EXPERT_FF = D_MODEL // 2
EPS = 1e-6

SPLIT_SIZES = (W_A, W_A, W_A,
               H_B * 2 * DH_B, H_B * 2 * DH_B, W_B,
               H_C * DK_C, H_C * DK_C, W_C, W_C,
               H_C, H_C, H_C, H_C)
N_IN = sum(SPLIT_SIZES)

kernel_name = 'hybrid_gated_parallel_encoder'


def rms_norm(x, gain):
    xf = x.astype(jnp.float32)
    y = xf * lax.rsqrt(jnp.mean(xf * xf, axis=-1, keepdims=True) + EPS)
    return (y * gain.astype(jnp.float32)).astype(x.dtype)


def l2_normalize(x):
    xf = x.astype(jnp.float32)
    return (xf * lax.rsqrt(jnp.sum(xf * xf, axis=-1, keepdims=True) + EPS)).astype(x.dtype)


def centred_depthwise_conv(x, w):
    width = w.shape[0]
    r = width // 2
    T = x.shape[1]
    xp = jnp.pad(x, ((0, 0), (r, r), (0, 0)))
    return sum(xp[:, i:i + T] * w[i] for i in range(width))


def partial_rotary(x, cos, sin):
    half = ROT_DIM // 2
    c = cos[:, :, None, None, :].astype(x.dtype)
    s = sin[:, :, None, None, :].astype(x.dtype)
    x1 = x[..., :half]
    x2 = x[..., half:ROT_DIM]
    return jnp.concatenate([x1 * c - x2 * s, x2 * c + x1 * s, x[..., ROT_DIM:]], axis=-1)


def short_conv_mixer(b, c, v, conv_w):
    return b * centred_depthwise_conv(c * v, conv_w)


def blockwise_diff_attention(q, k, v, lam):
    B_, T, H = q.shape[:3]
    nb = T // Q_BLOCK
    qb = q.reshape(B_, nb, Q_BLOCK, H, 2, DH_B).transpose(1, 0, 3, 4, 2, 5)
    kt = k.transpose(0, 2, 3, 1, 4)
    vt = v.transpose(0, 2, 1, 3)
    scale = DH_B ** -0.5

    def one_block(qi):
        s = jnp.einsum('bhcqd,bhckd->bhcqk', qi, kt).astype(jnp.float32) * scale
        p = jax.nn.softmax(s, axis=-1)
        a = p[:, :, 0] - lam * p[:, :, 1]
        return jnp.einsum('bhqk,bhkd->bhqd', a.astype(vt.dtype), vt)

    o = lax.map(one_block, qb)
    return o.transpose(1, 0, 3, 2, 4).reshape(B_, T, H, DV_B)


def diff_attention_mixer(q, k, v, cos, sin, q_gain, k_gain, lq1, lk1, lq2, lk2, sub_gain, lam_init):
    B_, T, _ = q.shape
    q = partial_rotary(rms_norm(q.reshape(B_, T, H_B, 2, DH_B), q_gain), cos, sin)
    k = partial_rotary(rms_norm(k.reshape(B_, T, H_B, 2, DH_B), k_gain), cos, sin)
    v = v.reshape(B_, T, H_B, DV_B)
    f32 = jnp.float32
    lam = (jnp.exp(jnp.sum(lq1.astype(f32) * lk1.astype(f32)))
           - jnp.exp(jnp.sum(lq2.astype(f32) * lk2.astype(f32))) + lam_init)
    o = blockwise_diff_attention(q, k, v, lam)
    o = rms_norm(o, sub_gain) * (1.0 - lam_init)
    return o.reshape(B_, T, W_B)


def chunk_gated_delta_rule(q, k, v, g, beta):
    B_, T, H, Dk = q.shape
    Dv = v.shape[-1]
    n = T // CHUNK
    f32 = jnp.float32

    def to_chunks(t):
        return t.astype(f32).reshape(B_, n, CHUNK, H, -1).transpose(0, 3, 1, 2, 4)

    q, k, v = to_chunks(q), to_chunks(k), to_chunks(v)
    g = g.astype(f32).reshape(B_, n, CHUNK, H).transpose(0, 3, 1, 2)
    beta = beta.astype(f32).reshape(B_, n, CHUNK, H).transpose(0, 3, 1, 2)
    g = jnp.cumsum(g, axis=-1)
    incl = jnp.tril(jnp.ones((CHUNK, CHUNK), dtype=bool))
    strict = jnp.tril(jnp.ones((CHUNK, CHUNK), dtype=bool), k=-1)
    diff = g[..., :, None] - g[..., None, :]
    decay = jnp.where(incl, jnp.exp(jnp.where(incl, diff, 0.0)), 0.0)
    k_beta = k * beta[..., None]
    lower = jnp.where(strict, jnp.einsum('bhncd,bhnsd->bhncs', k_beta, k) * decay, 0.0)
    rhs = jnp.concatenate([v * beta[..., None], k_beta * jnp.exp(g)[..., None]], axis=-1)
    sol = lax.linalg.triangular_solve(jnp.eye(CHUNK, dtype=f32) + lower, rhs,
                                      left_side=True, lower=True, unit_diagonal=True)
    u, w = sol[..., :Dv], sol[..., Dv:]
    attn = jnp.einsum('bhncd,bhnsd->bhncs', q, k) * decay
    q_dec = q * jnp.exp(g)[..., None]
    k_dec = k * jnp.exp(g[..., -1:] - g)[..., None]
    g_end = jnp.exp(g[..., -1])

    def step(state, xs):
        u_i, w_i, q_i, a_i, k_i, ge_i = xs
        v_new = u_i - jnp.einsum('bhck,bhkv->bhcv', w_i, state)
        o_i = jnp.einsum('bhck,bhkv->bhcv', q_i, state) + jnp.einsum('bhcs,bhsv->bhcv', a_i, v_new)
        state = state * ge_i[..., None, None] + jnp.einsum('bhck,bhcv->bhkv', k_i, v_new)
        return state, o_i

    xs = tuple(jnp.moveaxis(t, 2, 0) for t in (u, w, q_dec, attn, k_dec, g_end))
    s0 = jnp.zeros((B_, H, Dk, Dv), f32)
    _, o = lax.scan(step, s0, xs)
    return o.transpose(1, 0, 3, 2, 4).reshape(B_, T, H, Dv)


def log_decay(a, a_log, dt_bias):
    f32 = jnp.float32
    return -jnp.exp(a_log.astype(f32)) * jax.nn.softplus(a.astype(f32) + dt_bias.astype(f32))


def gated_deltanet_mixer(q, k, v, g_out, beta_f, beta_b, a_f, a_b, conv_w,
                         a_log_f, a_log_b, dt_bias_f, dt_bias_b, o_gain):
    B_, T, _ = q.shape
    qkv = jax.nn.silu(centred_depthwise_conv(jnp.concatenate([q, k, v], axis=-1), conv_w))
    q, k, v = jnp.split(qkv, [H_C * DK_C, 2 * H_C * DK_C], axis=-1)
    q = l2_normalize(q.reshape(B_, T, H_C, DK_C)) * (DK_C ** -0.5)
    k = l2_normalize(k.reshape(B_, T, H_C, DK_C))
    v = v.reshape(B_, T, H_C, DV_C)
    o_fwd = chunk_gated_delta_rule(q, k, v, log_decay(a_f, a_log_f, dt_bias_f),
                                   jax.nn.sigmoid(beta_f.astype(jnp.float32)))
    rev = lambda t: jnp.flip(t, axis=1)
    o_bwd = rev(chunk_gated_delta_rule(rev(q), rev(k), rev(v), rev(log_decay(a_b, a_log_b, dt_bias_b)),
                                       rev(jax.nn.sigmoid(beta_b.astype(jnp.float32)))))
    o = rms_norm(o_fwd + o_bwd, o_gain) * jax.nn.silu(g_out.reshape(B_, T, H_C, DV_C).astype(jnp.float32))
    return o.astype(g_out.dtype).reshape(B_, T, W_C)


def expert_choice_ffn(h, w_router, w_gate, w_up, w_down):
    B_, T, _ = h.shape
    cap = CAPACITY_FACTOR * T // N_EXPERTS
    aff = jax.nn.softmax(jnp.einsum('btd,de->bte', h, w_router).astype(jnp.float32), axis=-1)
    weight, idx = lax.top_k(jnp.swapaxes(aff, 1, 2), cap)
    b_idx = jnp.arange(B_)[:, None, None]
    xs = h[b_idx, idx]
    hid = jax.nn.silu(jnp.einsum('becd,edf->becf', xs, w_gate)) * jnp.einsum('becd,edf->becf', xs, w_up)
    y = jnp.einsum('becf,efd->becd', hid, w_down) * weight[..., None].astype(h.dtype)
    return jnp.zeros_like(h).at[b_idx, idx].add(y)


def setup_inputs(seed: int = 0) -> dict:
    key = jax.random.key(seed)
    ks = iter(jax.random.split(key, 32))
    f32 = jnp.float32
    L, D = DEPTH, D_MODEL

    def normal(shape, scale):
        return scale * jax.random.normal(next(ks), shape, f32)

    def gain(shape):
        return 1.0 + normal(shape, 0.02)

    def dt_bias(shape):
        dt = jnp.exp(jax.random.uniform(next(ks), shape, f32, math.log(1e-3), math.log(1e-1)))
        return dt + jnp.log(-jnp.expm1(-dt))

    def a_log(shape):
        return jnp.log(jax.random.uniform(next(ks), shape, f32, 1.0, 16.0))

    x = normal((BATCH, SEQ, D), 1.0)
    positions = jnp.broadcast_to(jnp.arange(SEQ, dtype=jnp.int32), (BATCH, SEQ))
    return {
        'x': x,
        'positions': positions,
        'norm_mix': gain((L, D)),
        'w_in': normal((L, D, N_IN), D ** -0.5),
        'conv_a': normal((L, CONV_A, W_A), CONV_A ** -0.5),
        'q_norm': gain((L, DH_B)),
        'k_norm': gain((L, DH_B)),
        'lambda_q1': normal((L, DH_B), 0.1),
        'lambda_k1': normal((L, DH_B), 0.1),
        'lambda_q2': normal((L, DH_B), 0.1),
        'lambda_k2': normal((L, DH_B), 0.1),
        'subln': gain((L, DV_B)),
        'conv_c': normal((L, CONV_C, 2 * H_C * DK_C + W_C), CONV_C ** -0.5),
        'a_log_f': a_log((L, H_C)),
        'a_log_b': a_log((L, H_C)),
        'dt_bias_f': dt_bias((L, H_C)),
        'dt_bias_b': dt_bias((L, H_C)),
        'o_norm': gain((L, DV_C)),
        'w_out_a': normal((L, W_A, D), W_A ** -0.5),
        'w_out_b': normal((L, W_B, D), W_B ** -0.5),
        'w_out_c': normal((L, W_C, D), W_C ** -0.5),
        'w_gate': normal((L, D, N_BRANCH * D), D ** -0.5),
        'b_gate': normal((L, N_BRANCH * D), 0.02),
        'w_o': normal((L, D, D), D ** -0.5),
        'norm_ffn': gain((L, D)),
        'w_router': normal((L, D, N_EXPERTS), D ** -0.5),
        'w_e_gate': normal((L, N_EXPERTS, D, EXPERT_FF), D ** -0.5),
        'w_e_up': normal((L, N_EXPERTS, D, EXPERT_FF), D ** -0.5),
        'w_e_down': normal((L, N_EXPERTS, EXPERT_FF, D), EXPERT_FF ** -0.5),
    }


def reference(x, positions, norm_mix, w_in, conv_a, q_norm, k_norm, lambda_q1, lambda_k1,
              lambda_q2, lambda_k2, subln, conv_c, a_log_f, a_log_b, dt_bias_f, dt_bias_b,
              o_norm, w_out_a, w_out_b, w_out_c, w_gate, b_gate, w_o, norm_ffn, w_router,
              w_e_gate, w_e_up, w_e_down):
    inv_freq = ROPE_THETA ** (-jnp.arange(0, ROT_DIM, 2, dtype=jnp.float32) / ROT_DIM)
    ang = positions.astype(jnp.float32)[..., None] * inv_freq
    cos, sin = jnp.cos(ang), jnp.sin(ang)
    split_at = np.cumsum(SPLIT_SIZES)[:-1].tolist()
    for l in range(DEPTH):
        lam_init = 0.8 - 0.6 * math.exp(-0.3 * l)
        xn = rms_norm(x, norm_mix[l])
        (b_a, c_a, v_a, q_b, k_b, v_b, q_c, k_c, v_c, g_c,
         beta_f, beta_b, a_f, a_b) = jnp.split(xn @ w_in[l], split_at, axis=-1)
        y_a = short_conv_mixer(b_a, c_a, v_a, conv_a[l]) @ w_out_a[l]
        y_b = diff_attention_mixer(q_b, k_b, v_b, cos, sin, q_norm[l], k_norm[l],
                                   lambda_q1[l], lambda_k1[l], lambda_q2[l], lambda_k2[l],
                                   subln[l], lam_init) @ w_out_b[l]
        y_c = gated_deltanet_mixer(q_c, k_c, v_c, g_c, beta_f, beta_b, a_f, a_b, conv_c[l],
                                   a_log_f[l], a_log_b[l], dt_bias_f[l], dt_bias_b[l],
                                   o_norm[l]) @ w_out_c[l]
        g_a, g_b, g_cc = jnp.split(jax.nn.sigmoid(xn @ w_gate[l] + b_gate[l]), N_BRANCH, axis=-1)
        x = x + (g_a * y_a + g_b * y_b + g_cc * y_c) @ w_o[l]
        x = x + expert_choice_ffn(rms_norm(x, norm_ffn[l]), w_router[l],
                                  w_e_gate[l], w_e_up[l], w_e_down[l])
    return x
```

```python
import math
from contextlib import ExitStack
import numpy as np
import concourse.bass as bass
import concourse.mybir as mybir
from concourse.bass_utils import run_bass_kernel_spmd

F32 = mybir.dt.float32
BF16 = mybir.dt.bfloat16
I32 = mybir.dt.int32
AF = mybir.ActivationFunctionType
ALU = mybir.AluOpType
AX = mybir.AxisListType

D = 2048
NIN = 6936
NG = 6144
NE = 16
FF = 1024
EPS = 1e-6
NEG = -1.0e30
ENGS = ("pe", "act", "dve", "pool", "sp")
DQ = ("sp", "act", "pool")
NDSEM = 8


class Buf:
    __slots__ = ("name", "last_w", "readers")

    def __init__(self, name=""):
        self.name = name
        self.last_w = None
        self.readers = []


class Sched:
    def __init__(self, nc, stack):
        self.nc = nc
        self.q = {e: [] for e in ENGS}
        self.cnt = {e: 0 for e in ENGS}
        self.seen = {e: {} for e in ENGS}
        self.stack = stack
        self.epoch = {e: 0 for e in ENGS}
        self.esems = {(e, 0): stack.enter_context(nc.semaphore("s_" + e)) for e in ENGS}
        self.dsem = {e: [stack.enter_context(nc.semaphore("d_%s%d" % (e, i))) for i in range(NDSEM)]
                     for e in DQ}
        self.dcnt = {e: 0 for e in DQ}
        self.dlast = {e: [0] * NDSEM for e in DQ}
        self.nops = 0

    def _sem(self, key):
        return self.esems[(key[1], key[2])] if key[0] == "e" else self.dsem[key[1]][key[2]]

    def _ekey(self, e):
        return ("e", e, self.epoch[e])

    def _need(self, eng, tok, waits):
        if tok is None:
            return
        key, val = tok
        if key[0] == "e" and key[1] == "pe" and eng == "pe":
            return
        if self.seen[eng].get(key, 0) >= val:
            return
        if waits.get(key, 0) < val:
            waits[key] = val

    def _deps(self, eng, reads, writes):
        waits = {}
        for b in reads:
            self._need(eng, b.last_w, waits)
        for b in writes:
            self._need(eng, b.last_w, waits)
            for r in b.readers:
                self._need(eng, r, waits)
        return waits

    def _commit(self, tok, reads, writes):
        for b in reads:
            b.readers.append(tok)
            if len(b.readers) > 48:
                mx = {}
                for k, v in b.readers:
                    if mx.get(k, 0) < v:
                        mx[k] = v
                b.readers = list(mx.items())
        for b in writes:
            b.last_w = tok
            b.readers = []

    def op(self, eng, fn, reads=(), writes=()):
        waits = self._deps(eng, reads, writes)
        for key, val in waits.items():
            self.seen[eng][key] = val
        self.cnt[eng] += 1
        n = self.cnt[eng]
        sem = self.esems[(eng, self.epoch[eng])]
        wl = [(self._sem(k), v) for k, v in waits.items()]

        def emit(e):
            for s, v in wl:
                e.wait_ge(s, v)
            fn(e).then_inc(sem, 1)
        self.q[eng].append(emit)
        self.nops += 1
        tok = (self._ekey(eng), n)
        self._commit(tok, reads, writes)
        return tok

    def dma(self, eng, fn, reads=(), writes=()):
        waits = self._deps(eng, reads, writes)
        j = self.dcnt[eng]
        self.dcnt[eng] += 1
        i = j % NDSEM
        key = ("d", eng, i)
        prev = self.dlast[eng][i]
        if prev and self.seen[eng].get(key, 0) < prev:
            if waits.get(key, 0) < prev:
                waits[key] = prev
        for k, v in waits.items():
            self.seen[eng][k] = v
        val = prev + 16
        self.dlast[eng][i] = val
        sem = self.dsem[eng][i]
        wl = [(self._sem(k), v) for k, v in waits.items()]

        def emit(e):
            for s, v in wl:
                e.wait_ge(s, v)
            fn(e).then_inc(sem, 16)
        self.q[eng].append(emit)
        self.nops += 1
        tok = (key, val)
        self._commit(tok, reads, writes)
        return tok

    def barrier(self):
        for e in ENGS:
            waits = {}
            for e2 in ENGS:
                key = self._ekey(e2)
                if self.cnt[e2] > self.seen[e].get(key, 0):
                    waits[key] = self.cnt[e2]
            for q in DQ:
                for i in range(NDSEM):
                    key = ("d", q, i)
                    v = self.dlast[q][i]
                    if v > self.seen[e].get(key, 0):
                        waits[key] = v
            for k, v in waits.items():
                self.seen[e][k] = v
            wl = [(self._sem(k), v) for k, v in waits.items()]

            def emit(eh, wl=wl):
                for s, v in wl:
                    eh.wait_ge(s, v)
            self.q[e].append(emit)
        for e in ENGS:
            if self.cnt[e] > 16000:
                self.epoch[e] += 1
                self.cnt[e] = 0
                self.esems[(e, self.epoch[e])] = self.stack.enter_context(
                    self.nc.semaphore("s_%s_%d" % (e, self.epoch[e])))

    def finish(self):
        self.barrier()
        nc = self.nc
        with nc.Block() as block:
            @block.tensor
            def _(e):
                for f in self.q["pe"]:
                    f(e)

            @block.scalar
            def _(e):
                for f in self.q["act"]:
                    f(e)

            @block.vector
            def _(e):
                for f in self.q["dve"]:
                    f(e)

            @block.gpsimd
            def _(e):
                for f in self.q["pool"]:
                    f(e)

            @block.sync
            def _(e):
                for f in self.q["sp"]:
                    f(e)


C_IDENT, C_ONES, C_BD64, C_RMAT, C_CUMF, C_CUMB, C_NEGF, C_NEGB, C_STRF, C_STRB, \
    C_SELFA, C_SELFB, C_SELBA, C_SELBB, C_SLT = [i * 128 for i in range(15)]
C_LASTF = 15 * 128
C_LASTB = 16 * 128
C_INVF = 17 * 128
C_SGN = C_INVF + 1
C_IOTA = C_SGN + 1
NCST = C_IOTA + 128


def make_consts():
    c = np.zeros((128, NCST), np.float32)
    i = np.arange(128)[:, None]
    j = np.arange(128)[None, :]
    same = (i // 64) == (j // 64)
    c[:, C_IDENT:C_IDENT + 128] = (i == j)
    c[:, C_ONES:C_ONES + 128] = 1.0
    c[:, C_BD64:C_BD64 + 128] = same
    dd = np.arange(128) % 64
    r = np.zeros((128, 128), np.float32)
    for d in range(128):
        m = d % 64
        if m < 8:
            r[d + 8, d] = 1.0
        elif m < 16:
            r[d - 8, d] = 1.0
    c[:, C_RMAT:C_RMAT + 128] = r
    incl_f = same & (j <= i)
    incl_b = same & (j >= i)
    c[:, C_CUMF:C_CUMF + 128] = incl_f.T
    c[:, C_CUMB:C_CUMB + 128] = incl_b.T
    c[:, C_NEGF:C_NEGF + 128] = np.where(incl_f, 0.0, NEG)
    c[:, C_NEGB:C_NEGB + 128] = np.where(incl_b, 0.0, NEG)
    c[:, C_STRF:C_STRF + 128] = same & (j < i)
    c[:, C_STRB:C_STRB + 128] = same & (j > i)
    for off, row in ((C_SELFA, 63), (C_SELFB, 127), (C_SELBA, 0), (C_SELBB, 64)):
        c[row, off:off + 128] = 1.0
    c[:, C_LASTF:C_LASTF + 128] = (i == (j // 64) * 64 + 63)
    c[:, C_LASTB:C_LASTB + 128] = (i == (j // 64) * 64)
    c[:, C_SLT:C_SLT + 128] = (i < j)
    invf = np.where(dd < 16, 500000.0 ** (-((dd % 8).astype(np.float64)) / 8.0), 0.0)
    c[:, C_INVF] = invf
    c[:, C_SGN] = np.where(dd < 8, -1.0, np.where(dd < 16, 1.0, 0.0))
    c[:, C_IOTA:C_IOTA + 128] = np.arange(128)[None, :]
    return c


O_BA, O_CA, O_VA = 0, 512, 1024
O_QB, O_KB, O_VB = 1536, 2304, 3072
O_QC, O_KC, O_VC = 3840, 4608, 5376
O_TM = 6144
NTM = NIN - O_TM

W_NAMES = ["norm_mix", "w_in", "conv_a", "q_norm", "k_norm", "lambda_q1", "lambda_k1",
           "lambda_q2", "lambda_k2", "subln", "conv_c", "a_log_f", "a_log_b", "dt_bias_f",
           "dt_bias_b", "o_norm", "w_out_a", "w_out_b", "w_out_c", "w_gate", "b_gate", "w_o",
           "norm_ffn", "w_router", "w_e_gate", "w_e_up", "w_e_down"]
W_SHAPES = {
    "norm_mix": [D], "w_in": [D, NIN], "conv_a": [3, 512], "q_norm": [64], "k_norm": [64],
    "lambda_q1": [64], "lambda_k1": [64], "lambda_q2": [64], "lambda_k2": [64], "subln": [128],
    "conv_c": [3, 2304], "a_log_f": [6], "a_log_b": [6], "dt_bias_f": [6], "dt_bias_b": [6],
    "o_norm": [128], "w_out_a": [512, D], "w_out_b": [768, D], "w_out_c": [768, D],
    "w_gate": [D, NG], "b_gate": [NG], "w_o": [D, D], "norm_ffn": [D], "w_router": [D, NE],
    "w_e_gate": [NE, D, FF], "w_e_up": [NE, D, FF], "w_e_down": [NE, FF, D],
}


class LazyW(dict):
    def __init__(self, k):
        super().__init__()
        self.k = k

    def __missing__(self, n):
        v = self.k.nc.dram_tensor(n, [self.k.L] + W_SHAPES[n], F32, kind="ExternalInput").ap()
        self[n] = v
        return v


class KB:
    def __init__(self, T, L, dbg=(), stages=None):
        self.T, self.L = T, L
        self.NT = T // 128
        self.CAP = 2 * T // NE
        self.dbg = set(dbg)
        self.stages = stages
        self.nc = nc = bass.Bass("TRN2", target_bir_lowering=False)
        self.st = ExitStack()
        self.S = Sched(nc, self.st)
        self.sb_base = 16512
        self.sb_ptr = 16512
        self.nalloc = 0
        self.regs = {}
        self.x_in = nc.dram_tensor("x", [T, D], F32, kind="ExternalInput").ap()
        self.pos_in = nc.dram_tensor("pos", [1, T], I32, kind="ExternalInput").ap()
        self.cst_in = nc.dram_tensor("cst", [128, NCST], F32, kind="ExternalInput").ap()
        self.w = LazyW(self)
        self.y_out = nc.dram_tensor("y", [T, D], F32, kind="ExternalOutput").ap()
        self.ps = []
        for i in range(8):
            t = nc.alloc_psum_tensor("psb%d" % i, [128, 512], F32)
            self.ps.append((t, Buf("ps%d" % i)))

    def dram(self, name, shape, dtype):
        kind = "ExternalOutput" if name in self.dbg else "Internal"
        return self.nc.dram_tensor(name, shape, dtype, kind=kind).ap()

    def sb(self, name, shape, dtype, bufs=None):
        esz = 4 if dtype in (F32, I32) else 2
        n = 1
        for s in shape[1:]:
            n *= s
        nbytes = (n * esz + 31) // 32 * 32
        self.nalloc += 1
        t = self.nc.alloc_sbuf_tensor_at("%s_%d" % (name, self.nalloc), list(shape), dtype,
                                         offset=self.sb_ptr)
        self.sb_ptr += nbytes
        assert self.sb_ptr <= 229344, ("SBUF overflow", name, self.sb_ptr)
        return t, Buf(name)

    def stage_reset(self):
        self.S.barrier()
        self.sb_ptr = self.sb_base

    def R(self, *key):
        b = self.regs.get(key)
        if b is None:
            b = self.regs[key] = Buf(str(key))
        return b

    def MM(self, out, lhsT, rhs, start=True, stop=True, r=(), w=()):
        self.S.op("pe", lambda e: e.matmul(out, lhsT=lhsT, rhs=rhs, start=start, stop=stop), r, w)

    def TR(self, out, in_, ident, r=(), w=()):
        self.S.op("pe", lambda e: e.transpose(out, in_, ident), r, w)

    def ACT(self, out, in_, func, bias=0.0, scale=1.0, accum=None, r=(), w=()):
        if accum is None:
            self.S.op("act", lambda e: e.activation(out=out, in_=in_, func=func, bias=bias, scale=scale), r, w)
        else:
            self.S.op("act", lambda e: e.activation(out=out, in_=in_, func=func, bias=bias, scale=scale,
                                                    accum_out=accum), r, w)

    def _eng(self, name):
        return name

    def TT(self, eng, out, in0, in1, op, r=(), w=()):
        self.S.op(eng, lambda e: e.tensor_tensor(out=out, in0=in0, in1=in1, op=op), r, w)

    def TS(self, eng, out, in0, s1, s2, op0, op1=None, accum=None, r=(), w=()):
        def f(e):
            kw = {}
            if op1 is not None:
                kw["op1"] = op1
            if accum is not None:
                kw["accum_out"] = accum
            return e.tensor_scalar(out=out, in0=in0, scalar1=s1, scalar2=s2, op0=op0, **kw)
        self.S.op(eng, f, r, w)

    def STT(self, eng, out, in0, scalar, in1, op0, op1, r=(), w=()):
        self.S.op(eng, lambda e: e.scalar_tensor_tensor(out=out, in0=in0, scalar=scalar, in1=in1,
                                                        op0=op0, op1=op1), r, w)

    def CP(self, eng, out, in_, r=(), w=()):
        if eng == "act":
            self.S.op("act", lambda e: e.copy(out=out, in_=in_), r, w)
        else:
            self.S.op(eng, lambda e: e.tensor_copy(out=out, in_=in_), r, w)

    def RCP(self, out, in_, r=(), w=()):
        self.S.op("dve", lambda e: e.reciprocal(out=out, in_=in_), r, w)

    def MSET(self, eng, ap, val, r=(), w=()):
        self.S.op(eng, lambda e: e.memset(ap, val), r, w)

    def DMA(self, q, out, in_, r=(), w=(), slow=False):
        if slow:
            self.S.dma(q, lambda e: e.dma_start(out=out, in_=in_, allow_slow_non_contiguous=True), r, w)
        else:
            self.S.dma(q, lambda e: e.dma_start(out=out, in_=in_), r, w)

    def on(self, name):
        return self.stages is None or name in self.stages


def st_setup(k):
    cst, bc = k.sb("cst", [128, NCST], F32)
    k.cst, k.bcst = cst, bc
    k.DMA("sp", cst[:], k.cst_in[:, :], w=[bc])
    idb, bidb = k.sb("identb", [128, 128], BF16)
    k.CP("dve", idb[:], cst[:, C_IDENT:C_IDENT + 128], r=[bc], w=[bidb])
    k.identb, k.bidentb = idb, bidb
    k.sb_base = k.sb_ptr


def cs(k, off, n=128, p0=0, p1=128):
    return k.cst[p0:p1, off:off + n]


def rstd_from_ssq(k, rstd, ssq, n, brstd, bssq):
    k.ACT(rstd, ssq, AF.Sqrt, bias=k.epsc[0:rstd.shape[0], 0:1], scale=1.0 / n, r=[bssq, k.bepsc], w=[brstd])
    k.RCP(rstd, rstd, r=[brstd], w=[brstd])


def st_norm_T(k, src, gain_ap, dstT, bdst, t0, nt, tag):
    gbc, bg = k.sb("gbc", [128, D], F32)
    k.DMA("sp", gbc[:], gain_ap.partition_broadcast(128), w=[bg])
    xt = [k.sb("xt%d" % i, [128, D], F32) for i in range(2)]
    xs = [k.sb("xs%d" % i, [128, D], BF16) for i in range(2)]
    junk, bj = k.sb("junk", [128, D], BF16)
    ssq = [k.sb("ssq%d" % i, [128, 1], F32) for i in range(2)]
    rs = [k.sb("rs%d" % i, [128, 1], F32) for i in range(2)]
    for tt in range(nt):
        x_t, bx = xt[tt % 2]
        xs_t, bxs = xs[tt % 2]
        sq, bsq = ssq[tt % 2]
        r_, br = rs[tt % 2]
        row0 = (t0 + tt) * 128
        k.DMA("sp", x_t[:], src[row0:row0 + 128, :], r=[k.R(tag, t0 + tt)], w=[bx])
        k.ACT(junk[:], x_t[:], AF.Square, accum=sq[:], r=[bx], w=[bj, bsq])
        rstd_from_ssq(k, r_[:], sq[:], D, br, bsq)
        k.STT("dve", xs_t[:], x_t[:], r_[:, 0:1], gbc[:], ALU.mult, ALU.mult, r=[bx, br, bg], w=[bxs])
        for g4 in range(4):
            pt, bpt = k.ps[(tt * 4 + g4) % 4]
            ptb = pt[:].bitcast(BF16)
            for c in range(4):
                kc = g4 * 4 + c
                k.TR(ptb[:, c * 128:(c + 1) * 128], xs_t[:, kc * 128:(kc + 1) * 128], k.identb[:],
                     r=[bxs, k.bidentb], w=[bpt])
            eng = "act" if g4 % 2 == 0 else "dve"
            k.CP(eng, dstT[:, g4 * 4:(g4 + 1) * 4, tt * 128:(tt + 1) * 128],
                 ptb[:, 0:512].rearrange("p (c t) -> p c t", c=4), r=[bpt], w=[bdst])


def st_proj(k, l, xsrc, xtag):
    T = k.T
    TB = min(T, 2048)
    w_in, w_gate, b_gate = k.w["w_in"][l], k.w["w_gate"][l], k.w["b_gate"][l]
    for sbi in range(T // TB):
        k.stage_reset()
        xnT, bxn = k.sb("xnT", [128, 16, TB], BF16)
        mark = k.sb_ptr
        st_norm_T(k, xsrc, k.w["norm_mix"][l], xnT, bxn, sbi * TB // 128, TB // 128, xtag)
        k.S.barrier()
        k.sb_ptr = mark
        nblk = TB // 512 if TB >= 512 else 1
        bw = TB // nblk
        wf = [k.sb("wf%d" % i, [128, 16, 128], F32) for i in range(2)]
        wb = [k.sb("wb%d" % i, [128, 16, 128], BF16) for i in range(2)]
        stg = [k.sb("stg%d" % i, [128, TB], F32) for i in range(2)]
        stgb = [k.sb("stgb%d" % i, [128, TB], BF16) for i in range(2)]
        bgc, bbgc = k.sb("bgc", [128, 48], F32)
        k.DMA("sp", bgc[:], b_gate.rearrange("(c p) -> p c", p=128), w=[bbgc], slow=True)
        it = 0
        for kind, ncks in (("in", 48), ("gate", 48)):
            W = w_in if kind == "in" else w_gate
            for cc in range(ncks):
                wf_t, bwf = wf[it % 2]
                wb_t, bwb = wb[it % 2]
                c0 = cc * 128
                k.DMA("sp", wf_t[:], W[:, c0:c0 + 128].rearrange("(kc p) c -> p kc c", p=128), w=[bwf])
                k.CP("pool", wb_t[:], wf_t[:], r=[bwf], w=[bwb])
                if kind == "in":
                    so, bso = stg[it % 2]
                else:
                    so, bso = stgb[it % 2]
                for nb in range(nblk):
                    pt, bpt = k.ps[4 + (it * nblk + nb) % 4]
                    for kc in range(16):
                        k.MM(pt[:, 0:bw], wb_t[:, kc, :], xnT[:, kc, nb * bw:(nb + 1) * bw],
                             start=(kc == 0), stop=(kc == 15), r=[bwb, bxn], w=[bpt])
                    if kind == "in":
                        eng = "act" if nb % 2 == 0 else "dve"
                        k.CP(eng, so[:, nb * bw:(nb + 1) * bw], pt[:, 0:bw], r=[bpt], w=[bso])
                    else:
                        k.ACT(so[:, nb * bw:(nb + 1) * bw], pt[:, 0:bw], AF.Sigmoid, bias=bgc[:, cc:cc + 1],
                              r=[bpt, bbgc], w=[bso])
                if kind == "in":
                    k.DMA("act", k.pT[c0:c0 + 128, sbi * TB:(sbi + 1) * TB], so[:], r=[bso],
                          w=[k.R("pT", cc, sbi)])
                else:
                    k.DMA("act", k.gT[c0:c0 + 128, sbi * TB:(sbi + 1) * TB], so[:], r=[bso],
                          w=[k.R("gT", cc, sbi)])
                it += 1
        k.S.barrier()
        k.sb_ptr = mark
        wtf, bwtf = k.sb("wtf", [128, 4, NTM], F32)
        wtb, bwtb = k.sb("wtb", [128, 16, NTM], BF16)
        for q4 in range(4):
            k.DMA("sp", wtf[:], w_in[q4 * 512:(q4 + 1) * 512, O_TM:NIN].rearrange("(kc p) c -> p kc c", p=128),
                  r=[], w=[bwtf])
            k.CP("pool", wtb[:, q4 * 4:(q4 + 1) * 4, :], wtf[:], r=[bwtf], w=[bwtb])
        so2 = [k.sb("so2%d" % i, [128, NTM], F32) for i in range(2)]
        for tt in range(TB // 128):
            so, bso = so2[tt % 2]
            p0, bp0 = k.ps[(tt % 2) * 2]
            p1, bp1 = k.ps[(tt % 2) * 2 + 1]
            for kc in range(16):
                k.MM(p0[:, 0:512], xnT[:, kc, tt * 128:(tt + 1) * 128], wtb[:, kc, 0:512],
                     start=(kc == 0), stop=(kc == 15), r=[bwtb, bxn], w=[bp0])
            for kc in range(16):
                k.MM(p1[:, 0:NTM - 512], xnT[:, kc, tt * 128:(tt + 1) * 128], wtb[:, kc, 512:NTM],
                     start=(kc == 0), stop=(kc == 15), r=[bwtb, bxn], w=[bp1])
            k.CP("act", so[:, 0:512], p0[:, 0:512], r=[bp0], w=[bso])
            k.CP("dve", so[:, 512:NTM], p1[:, 0:NTM - 512], r=[bp1], w=[bso])
            row0 = sbi * TB + tt * 128
            k.DMA("act", k.tm[row0:row0 + 128, :], so[:], r=[bso], w=[k.R("tm", row0 // 128)])


def st_rot(k):
    T = k.T
    k.stage_reset()
    TWO_PI = 2.0 * math.pi
    posi, bpi = k.sb("posi", [128, T], I32)
    ang, ba = k.sb("ang", [128, T], F32)
    a2, ba2 = k.sb("a2", [128, T], F32)
    kq, bk = k.sb("kq", [128, T], F32)
    ki, bki = k.sb("ki", [128, T], I32)
    m, bm = k.sb("m", [128, T], F32)
    k.DMA("sp", posi[:], k.pos_in[0:1, :].partition_broadcast(128), w=[bpi])
    k.CP("dve", ang[:], posi[:], r=[bpi], w=[ba])
    k.TS("dve", ang[:], ang[:], k.cst[:, C_INVF:C_INVF + 1], None, ALU.mult, r=[ba, k.bcst], w=[ba])
    for which, dst in ((0, k.rotS), (1, k.rotC)):
        shift = 0.0 if which == 0 else math.pi / 2
        k.TS("dve", a2[:], ang[:], shift, None, ALU.add, r=[ba], w=[ba2])
        k.TS("dve", kq[:], a2[:], 1.0 / TWO_PI, None, ALU.mult, r=[ba2], w=[bk])
        k.CP("dve", ki[:], kq[:], r=[bk], w=[bki])
        k.CP("dve", kq[:], ki[:], r=[bki], w=[bk])
        k.STT("dve", a2[:], kq[:], -TWO_PI, a2[:], ALU.mult, ALU.add, r=[bk, ba2], w=[ba2])
        k.TS("dve", m[:], a2[:], math.pi, None, ALU.is_gt, r=[ba2], w=[bm])
        k.STT("dve", a2[:], m[:], -TWO_PI, a2[:], ALU.mult, ALU.add, r=[bm, ba2], w=[ba2])
        k.TS("dve", m[:], a2[:], -math.pi, None, ALU.is_lt, r=[ba2], w=[bm])
        k.STT("dve", a2[:], m[:], TWO_PI, a2[:], ALU.mult, ALU.add, r=[bm, ba2], w=[ba2])
        k.TS("dve", a2[:], a2[:], math.pi, -math.pi, ALU.min, ALU.max, r=[ba2], w=[ba2])
        k.ACT(kq[:], a2[:], AF.Sin, r=[ba2], w=[bk])
        if which == 0:
            k.TS("dve", kq[:], kq[:], k.cst[:, C_SGN:C_SGN + 1], None, ALU.mult, r=[bk, k.bcst], w=[bk])
        k.DMA("sp", dst[:, :], kq[:], r=[bk], w=[k.R("rot", which)])


def st_mixA(k, l):
    T = k.T
    k.stage_reset()
    cw, bcw = k.sb("cwa", [128, 3, 4], F32)
    for tap in range(3):
        k.DMA("sp", cw[:, tap, :], k.w["conv_a"][l][tap].rearrange("(c p) -> p c", p=128), w=[bcw], slow=True)
    b_, bb = k.sb("b_", [128, T], F32)
    c_, bc = k.sb("c_", [128, T], F32)
    v_, bv = k.sb("v_", [128, T], F32)
    acc, bacc = k.sb("acc", [128, T], F32)
    yb, byb = k.sb("yb", [128, T], BF16)
    nsb = max(1, T // 2048)
    for c4 in range(4):
        rd = [k.R("pT", (O_BA // 128) + c4, s) for s in range(nsb)]
        k.DMA("sp", b_[:], k.pT[O_BA + c4 * 128:O_BA + (c4 + 1) * 128, :], r=rd, w=[bb])
        rd = [k.R("pT", (O_CA // 128) + c4, s) for s in range(nsb)]
        k.DMA("sp", c_[:], k.pT[O_CA + c4 * 128:O_CA + (c4 + 1) * 128, :], r=rd, w=[bc])
        rd = [k.R("pT", (O_VA // 128) + c4, s) for s in range(nsb)]
        k.DMA("sp", v_[:], k.pT[O_VA + c4 * 128:O_VA + (c4 + 1) * 128, :], r=rd, w=[bv])
        k.TT("dve", c_[:], c_[:], v_[:], ALU.mult, r=[bc, bv], w=[bc])
        k.TS("pool", acc[:], c_[:], cw[:, 1, c4:c4 + 1], None, ALU.mult, r=[bc, bcw], w=[bacc])
        k.STT("dve", acc[:, 1:T], c_[:, 0:T - 1], cw[:, 0, c4:c4 + 1], acc[:, 1:T], ALU.mult, ALU.add,
              r=[bc, bcw, bacc], w=[bacc])
        k.STT("dve", acc[:, 0:T - 1], c_[:, 1:T], cw[:, 2, c4:c4 + 1], acc[:, 0:T - 1], ALU.mult, ALU.add,
              r=[bc, bcw, bacc], w=[bacc])
        k.TT("dve", yb[:], acc[:], b_[:], ALU.mult, r=[bacc, bb], w=[byb])
        k.DMA("act", k.oT[c4 * 128:(c4 + 1) * 128, :], yb[:], r=[byb], w=[k.R("oT", c4)])


def st_attn(k, l):
    T, NT = k.T, k.NT
    lam_init = 0.8 - 0.6 * math.exp(-0.3 * l)
    k.stage_reset()
    nsb = max(1, T // 2048)
    CT, bCT = k.sb("CT", [128, T], F32)
    SN, bSN = k.sb("SN", [128, T], F32)
    k.DMA("sp", CT[:], k.rotC[:, :], r=[k.R("rot", 1)], w=[bCT])
    k.DMA("sp", SN[:], k.rotS[:, :], r=[k.R("rot", 0)], w=[bSN])
    raw, braw = k.sb("raw", [128, T], F32)
    qT, bqT = k.sb("qTr", [128, T], BF16)
    kT, bkT = k.sb("kTr", [128, T], BF16)
    vext, bvx = k.sb("vext", [128, NT, 130], BF16)
    oBT, boBT = k.sb("oBT", [128, T], BF16)
    gq, bgq = k.sb("gq", [128, 1], F32)
    gk, bgk = k.sb("gk", [128, 1], F32)
    for half in range(2):
        k.DMA("sp", gq[half * 64:(half + 1) * 64, :], k.w["q_norm"][l].rearrange("(p o) -> p o", o=1), w=[bgq], slow=True)
        k.DMA("sp", gk[half * 64:(half + 1) * 64, :], k.w["k_norm"][l].rearrange("(p o) -> p o", o=1), w=[bgk], slow=True)
    sgc, bsgc = k.sb("sgc", [128, 1], F32)
    k.DMA("sp", sgc[:], k.w["subln"][l].rearrange("(p o) -> p o", o=1), w=[bsgc], slow=True)
    k.TS("dve", sgc[:], sgc[:], 1.0 - lam_init, None, ALU.mult, r=[bsgc], w=[bsgc])
    onesb, bonesb = k.sb("onesb", [128, 2], BF16)
    k.MSET("dve", onesb[:], 1.0, w=[bonesb])
    rden, brden = k.sb("rden", [1, 2, 512], F32)
    lv, blv = k.sb("lv", [1, 4, 64], F32)
    for i, n in enumerate(("lambda_q1", "lambda_k1", "lambda_q2", "lambda_k2")):
        k.DMA("sp", lv[0:1, i, :], k.w[n][l:l + 1, :], w=[blv])
    lp, blp = k.sb("lp", [1, 2, 64], F32)
    k.TT("dve", lp[0:1, 0, :], lv[0:1, 0, :], lv[0:1, 1, :], ALU.mult, r=[blv], w=[blp])
    k.TT("dve", lp[0:1, 1, :], lv[0:1, 2, :], lv[0:1, 3, :], ALU.mult, r=[blv], w=[blp])
    ls, bls = k.sb("ls", [1, 2], F32)
    k.S.op("dve", lambda e: e.reduce_sum(out=ls[0:1, 0:2], in_=lp[0:1, :, :], axis=AX.X), [blp], [bls])
    k.ACT(ls[0:1, 0:2], ls[0:1, 0:2], AF.Exp, r=[bls], w=[bls])
    nl, bnl = k.sb("nl", [1, 1], F32)
    k.TT("dve", nl[0:1, 0:1], ls[0:1, 1:2], ls[0:1, 0:1], ALU.subtract, r=[bls], w=[bnl])
    k.TS("dve", nl[0:1, 0:1], nl[0:1, 0:1], -lam_init, None, ALU.add, r=[bnl], w=[bnl])
    nlb, bnlb = k.sb("nlb", [128, 1], F32)
    p7, bp7 = k.ps[7]
    k.MM(p7[:, 0:1], k.cst[0:1, C_ONES:C_ONES + 128], nl[0:1, 0:1], r=[k.bcst, bnl], w=[bp7])
    k.CP("dve", nlb[:], p7[:, 0:1], r=[bp7], w=[bnlb])
    import os
    STOP = int(os.environ.get("ATTN_STOP", "99"))
    if STOP == 1:
        return
    tmp = [k.sb("atmp%d" % i, [128, 512], F32) for i in range(4)]
    PT = [k.sb("PT%d" % i, [128, 512], BF16) for i in range(3)]
    o1, bo1 = k.sb("o1", [128, 128], F32)
    o2, bo2 = k.sb("o2", [128, 128], F32)
    ob, bob = k.sb("ob", [128, 128], BF16)
    rd, brd = k.sb("rd", [128, 2], F32)
    ssq, bssq = k.sb("assq", [128, 1], F32)
    rsd, brsd = k.sb("arsd", [128, 1], F32)
    junk, bj = k.sb("ajunk", [128, 128], F32)
    k.MSET("pool", vext[:, :, 128:130], 1.0, w=[bvx])
    BW = min(512, T)
    for h in range(6):
        for which, dstT, bdst, gcol, bgc_, off in ((0, qT, bqT, gq, bgq, O_QB), (1, kT, bkT, gk, bgk, O_KB)):
            rdl = [k.R("pT", off // 128 + h, s) for s in range(nsb)]
            k.DMA("sp", raw[:], k.pT[off + h * 128:off + (h + 1) * 128, :], r=rdl, w=[braw])
            for blk in range(T // BW):
                sl = slice(blk * BW, (blk + 1) * BW)
                (t0, bt0), (t1, bt1), (t2, bt2), (t3, bt3) = tmp
                pa, bpa = k.ps[4 + blk % 2]
                pb, bpb = k.ps[6 + blk % 2]
                k.ACT(t0[:, 0:BW], raw[:, sl], AF.Square, r=[braw], w=[bt0])
                k.MM(pa[:, 0:BW], k.cst[:, C_BD64:C_BD64 + 128], t0[:, 0:BW], r=[k.bcst, bt0], w=[bpa])
                k.ACT(t1[:, 0:BW], pa[:, 0:BW], AF.Sqrt, bias=k.epsc[:, 0:1], scale=1.0 / 64, r=[bpa, k.bepsc], w=[bt1])
                k.RCP(t1[:, 0:BW], t1[:, 0:BW], r=[bt1], w=[bt1])
                k.STT("dve", t2[:, 0:BW], raw[:, sl], gcol[:, 0:1], t1[:, 0:BW], ALU.mult, ALU.mult,
                      r=[braw, bgc_, bt1], w=[bt2])
                k.MM(pb[:, 0:BW], k.cst[:, C_RMAT:C_RMAT + 128], t2[:, 0:BW], r=[k.bcst, bt2], w=[bpb])
                k.TT("pool", t3[:, 0:BW], t2[:, 0:BW], CT[:, sl], ALU.mult, r=[bt2, bCT], w=[bt3])
                k.TT("dve", t0[:, 0:BW], pb[:, 0:BW], SN[:, sl], ALU.mult, r=[bpb, bSN], w=[bt0])
                k.TT("dve", dstT[:, sl], t3[:, 0:BW], t0[:, 0:BW], ALU.add, r=[bt3, bt0], w=[bdst])
        if STOP == 2:
            return
        rdl = [k.R("pT", O_VB // 128 + h, s) for s in range(nsb)]
        k.DMA("sp", raw[:], k.pT[O_VB + h * 128:O_VB + (h + 1) * 128, :], r=rdl, w=[braw])
        for tt in range(NT):
            pa, bpa = k.ps[4 + tt % 4]
            k.TR(pa[:, 0:128], raw[:, tt * 128:(tt + 1) * 128], k.cst[:, C_IDENT:C_IDENT + 128], r=[braw, k.bcst], w=[bpa])
            k.CP("act" if tt % 2 else "dve", vext[:, tt, 0:128], pa[:, 0:128], r=[bpa], w=[bvx])
        if STOP == 3:
            return
        for qb in range(T // BW):
            steps = [(s_, kc) for s_ in range(2) for kc in range(NT)]
            qsl = slice(qb * BW, (qb + 1) * BW)

            def issue_S(i):
                s_, kc = steps[i]
                pst, bpst = k.ps[4 + i % 4]
                k.MM(pst[:, 0:BW], kT[s_ * 64:(s_ + 1) * 64, kc * 128:(kc + 1) * 128],
                     qT[s_ * 64:(s_ + 1) * 64, qsl], r=[bkT, bqT], w=[bpst])
            issue_S(0)
            for i, (s, kc) in enumerate(steps):
                if i + 1 < len(steps):
                    issue_S(i + 1)
                pst, bpst = k.ps[4 + i % 4]
                pt_, bpt_ = PT[i % 3]
                k.ACT(pt_[:, 0:BW], pst[:, 0:BW], AF.Exp, scale=0.125, r=[bpst], w=[bpt_])
                po, bpo = k.ps[s]
                pd, bpd = k.ps[2 + s]
                k.MM(po[:, 0:BW], vext[:, kc, 0:128], pt_[:, 0:BW], start=(kc == 0), stop=(kc == NT - 1),
                     r=[bpt_, bvx], w=[bpo])
                k.MM(pd[0:1, 0:BW], onesb[:, 0:1], pt_[:, 0:BW], start=(kc == 0), stop=(kc == NT - 1),
                     r=[bpt_, bonesb], w=[bpd])
            for s in range(2):
                k.RCP(rden[0:1, s, 0:BW], k.ps[2 + s][0][0:1, 0:BW], r=[k.ps[2 + s][1]], w=[brden])
            (t0, bt0), (t1, bt1), (t2, bt2), (t3, bt3) = tmp
            pb0, bpb0 = k.ps[4]
            pb1, bpb1 = k.ps[5]
            k.MM(pb0[:, 0:BW], k.cst[0:1, C_ONES:C_ONES + 128], rden[0:1, 0, 0:BW], r=[k.bcst, brden], w=[bpb0])
            k.MM(pb1[:, 0:BW], k.cst[0:1, C_ONES:C_ONES + 128], rden[0:1, 1, 0:BW], r=[k.bcst, brden], w=[bpb1])
            k.CP("act", t0[:, 0:BW], pb0[:, 0:BW], r=[bpb0], w=[bt0])
            k.ACT(t1[:, 0:BW], pb1[:, 0:BW], AF.Copy, scale=nlb[:, 0:1], r=[bpb1, bnlb], w=[bt1])
            k.TT("dve", t0[:, 0:BW], k.ps[0][0][:, 0:BW], t0[:, 0:BW], ALU.mult, r=[k.ps[0][1], bt0], w=[bt0])
            k.TT("dve", t1[:, 0:BW], k.ps[1][0][:, 0:BW], t1[:, 0:BW], ALU.mult, r=[k.ps[1][1], bt1], w=[bt1])
            k.TT("pool", t2[:, 0:BW], t0[:, 0:BW], t1[:, 0:BW], ALU.add, r=[bt0, bt1], w=[bt2])
            k.ACT(t3[:, 0:BW], t2[:, 0:BW], AF.Square, r=[bt2], w=[bt3])
            pss_, bpss = k.ps[6]
            k.MM(pss_[:, 0:BW], k.cst[:, C_ONES:C_ONES + 128], t3[:, 0:BW], r=[k.bcst, bt3], w=[bpss])
            k.ACT(t0[:, 0:BW], pss_[:, 0:BW], AF.Sqrt, bias=k.epsc[:, 0:1], scale=1.0 / 128, r=[bpss, k.bepsc], w=[bt0])
            k.RCP(t0[:, 0:BW], t0[:, 0:BW], r=[bt0], w=[bt0])
            k.STT("dve", oBT[:, qsl], t2[:, 0:BW], sgc[:, 0:1], t0[:, 0:BW], ALU.mult, ALU.mult, r=[bt2, bsgc, bt0], w=[boBT])
        k.DMA("act", k.oT[512 + h * 128:512 + (h + 1) * 128, :], oBT[:], r=[boBT], w=[k.R("oT", 4 + h)])
        k.S.barrier()
    if "dq" in k.dbg:
        dq = k.dram("dq", [128, T], BF16)
        dk = k.dram("dk", [128, T], BF16)
        dv = k.dram("dv", [128, NT * 130], BF16)
        k.DMA("sp", dq[:, :], qT[:], r=[bqT], w=[k.R("dq")])
        k.DMA("sp", dk[:, :], kT[:], r=[bkT], w=[k.R("dk")])
        k.DMA("sp", dv[:, :], vext[:].rearrange("p a b -> p (a b)"), r=[bvx], w=[k.R("dv")])


def st_merge(k, l, xsrc, xtag):
    T = k.T
    k.stage_reset()
    BW = min(512, T)
    nqt = BW // 128
    wabc = [(k.w["w_out_a"][l], 0, 4), (k.w["w_out_b"][l], 4, 6), (k.w["w_out_c"][l], 10, 6)]
    w_o = k.w["w_o"][l]
    WA, bWA = k.sb("mWA", [128, 16, D], BF16)
    WO, bWO = k.sb("mWO", [128, 16, D], BF16)
    wf = [k.sb("mwf%d" % i, [128, 16, 128], F32) for i in range(2)]
    oTb, boTb = k.sb("oTb", [128, 16, BW], BF16)
    mT, bmT = k.sb("mT", [128, 16, BW], BF16)
    gt = [k.sb("mgt%d" % i, [128, 3, BW], BF16) for i in range(2)]
    t1, bt1 = k.sb("mt1", [128, BW], F32)
    t2, bt2 = k.sb("mt2", [128, BW], F32)
    xt = [k.sb("mxt%d" % i, [128, 512], F32) for i in range(2)]
    ce = ("dve", "act", "pool")
    for oc in range(16):
        wf_t, bwf = wf[oc % 2]
        for (wap, c0, nk) in wabc:
            k.DMA("sp", wf_t[:, c0:c0 + nk, :], wap[:, oc * 128:(oc + 1) * 128].rearrange("(kc p) c -> p kc c", p=128),
                  w=[bwf])
        k.CP(ce[oc % 3], WA[:, :, oc * 128:(oc + 1) * 128], wf_t[:], r=[bwf], w=[bWA])
    for oc in range(16):
        wf_t, bwf = wf[oc % 2]
        k.DMA("sp", wf_t[:], w_o[:, oc * 128:(oc + 1) * 128].rearrange("(kc p) c -> p kc c", p=128), w=[bwf])
        k.CP(ce[(oc + 1) % 3], WO[:, :, oc * 128:(oc + 1) * 128], wf_t[:], r=[bwf], w=[bWO])
    it = 0
    nsb = max(1, T // 2048)
    for tb in range(T // BW):
        tsl = slice(tb * BW, (tb + 1) * BW)
        for c in range(16):
            k.DMA("sp", oTb[:, c, :], k.oT[c * 128:(c + 1) * 128, tsl], r=[k.R("oT", c)], w=[boTb])
        for oc in range(16):
            g_t, bg = gt[it % 2]
            it += 1
            for br in range(3):
                row = br * 2048 + oc * 128
                k.DMA("act", g_t[:, br, :], k.gT[row:row + 128, tsl],
                      r=[k.R("gT", row // 128, s_) for s_ in range(nsb)], w=[bg])
            pss = []
            for bi, (wap, c0, nk) in enumerate(wabc):
                pt, bpt = k.ps[bi + 3 * (oc % 2)]
                for kc in range(c0, c0 + nk):
                    k.MM(pt[:, 0:BW], WA[:, kc, oc * 128:(oc + 1) * 128], oTb[:, kc, :], start=(kc == c0),
                         stop=(kc == c0 + nk - 1), r=[bWA, boTb], w=[bpt])
                pss.append((pt, bpt))
            k.TT("dve", t1[:], pss[0][0][:, 0:BW], g_t[:, 0, :], ALU.mult, r=[pss[0][1], bg], w=[bt1])
            k.TT("dve", t2[:], pss[1][0][:, 0:BW], g_t[:, 1, :], ALU.mult, r=[pss[1][1], bg], w=[bt2])
            k.TT("pool", t1[:], t1[:], t2[:], ALU.add, r=[bt1, bt2], w=[bt1])
            k.TT("dve", t2[:], pss[2][0][:, 0:BW], g_t[:, 2, :], ALU.mult, r=[pss[2][1], bg], w=[bt2])
            k.TT("pool", mT[:, oc, :], t1[:], t2[:], ALU.add, r=[bt1, bt2], w=[bmT])
        iq = 0
        for cb in range(4):
            for qt in range(nqt):
                x_t, bx = xt[iq % 2]
                pt, bpt = k.ps[6 + iq % 2]
                iq += 1
                row0 = tb * BW + qt * 128
                k.DMA("act", x_t[:], xsrc[row0:row0 + 128, cb * 512:(cb + 1) * 512], r=[k.R(xtag, row0 // 128)], w=[bx])
                for kc in range(16):
                    k.MM(pt[:, 0:512], mT[:, kc, qt * 128:(qt + 1) * 128], WO[:, kc, cb * 512:(cb + 1) * 512],
                         start=(kc == 0), stop=(kc == 15), r=[bmT, bWO], w=[bpt])
                k.TT("dve", x_t[:], pt[:, 0:512], x_t[:], ALU.add, r=[bpt, bx], w=[bx])
                k.DMA("act", k.hbuf[row0:row0 + 128, cb * 512:(cb + 1) * 512], x_t[:], r=[bx], w=[k.R("h", row0 // 128, cb)])


def st_moe(k, l, xdst, xdtag):
    T, NT, CAP = k.T, k.NT, k.CAP
    SR = min(128, CAP)
    nst = (CAP + 127) // 128
    NSLOT = NE * CAP
    BIG = float(NSLOT + 1000)
    ident = k.cst[:, C_IDENT:C_IDENT + 128]

    def bndreg(eh):
        if getattr(k, "_bnd", None) is None:
            k._bnd = eh.alloc_register("bnd")
            eh.reg_mov(k._bnd, NSLOT - 1)
        return k._bnd
    k.stage_reset()
    mT, bmT = k.sb("mskT", [128, NT, 16], F32)
    wT, bwT = k.sb("wT", [128, NT, 16], F32)
    idxf, bxf = k.sb("idxf", [128, NT, 16], F32)
    idxi, bxi = k.sb("idxi", [128, NT, 16], I32)
    basee, bbe = k.sb("basee", [128, 16], F32)
    sm, bsm = k.sb("bis", [16, 8], F32)
    persist2 = k.sb_ptr
    lgT, blg = k.sb("lgT", [16, T], F32)
    persist = k.sb_ptr
    gbc, bg = k.sb("gbc2", [128, D], F32)
    k.DMA("sp", gbc[:], k.w["norm_ffn"][l].partition_broadcast(128), w=[bg])
    wr, bwr = k.sb("wr", [128, 16, NE], F32)
    k.DMA("sp", wr[:], k.w["w_router"][l].rearrange("(kc p) e -> p kc e", p=128), w=[bwr])
    xt = [k.sb("hx%d" % i, [128, D], F32) for i in range(2)]
    hnf = [k.sb("hnf%d" % i, [128, D], F32) for i in range(2)]
    hnb = [k.sb("hnb%d" % i, [128, D], BF16) for i in range(2)]
    hT = [k.sb("hT%d" % i, [128, 16, 128], F32) for i in range(2)]
    junk, bj = k.sb("mjunk", [128, D], BF16)
    ssq = [k.sb("mssq%d" % i, [128, 1], F32) for i in range(2)]
    rs = [k.sb("mrs%d" % i, [128, 1], F32) for i in range(2)]
    for tt in range(NT):
        x_t, bx = xt[tt % 2]
        f_t, bf = hnf[tt % 2]
        b_t, bb = hnb[tt % 2]
        h_t, bh = hT[tt % 2]
        sq, bsq = ssq[tt % 2]
        r_, br = rs[tt % 2]
        row0 = tt * 128
        k.DMA("sp", x_t[:], k.hbuf[row0:row0 + 128, :], r=[k.R("h", tt, cb) for cb in range(4)], w=[bx])
        k.ACT(junk[:], x_t[:], AF.Square, accum=sq[:], r=[bx], w=[bj, bsq])
        rstd_from_ssq(k, r_[:], sq[:], D, br, bsq)
        k.STT("dve", f_t[:], x_t[:], r_[:, 0:1], gbc[:], ALU.mult, ALU.mult, r=[bx, br, bg], w=[bf])
        k.CP("pool", b_t[:], f_t[:], r=[bf], w=[bb])
        k.DMA("act", k.hn[row0:row0 + 128, :], b_t[:], r=[bb], w=[k.R("hn", tt)])
        for g4 in range(4):
            pt, bpt = k.ps[g4]
            for c in range(4):
                kc = g4 * 4 + c
                k.TR(pt[:, c * 128:(c + 1) * 128], f_t[:, kc * 128:(kc + 1) * 128], ident, r=[bf, k.bcst], w=[bpt])
            k.CP("act" if g4 % 2 else "dve", h_t[:, g4 * 4:(g4 + 1) * 4, :],
                 pt[:, 0:512].rearrange("p (c t) -> p c t", c=4), r=[bpt], w=[bh])
        pl, bpl = k.ps[4 + tt % 2]
        for kc in range(16):
            k.MM(pl[0:16, 0:128], wr[:, kc, :], h_t[:, kc, :], start=(kc == 0), stop=(kc == 15), r=[bwr, bh], w=[bpl])
        k.CP("act", lgT[:, row0:row0 + 128], pl[0:16, 0:128], r=[bpl], w=[blg])
    k.S.barrier()
    k.sb_ptr = persist
    aff, baf = k.sb("aff", [16, T], F32)
    tmpA, btA = k.sb("tmpA", [16, T], F32)
    k.ACT(aff[:], lgT[:], AF.Exp, r=[blg], w=[baf])
    BW = min(512, T)
    for blk in range(T // BW):
        sl = slice(blk * BW, (blk + 1) * BW)
        pt, bpt = k.ps[blk % 2]
        k.MM(pt[0:16, 0:BW], k.cst[0:16, C_ONES:C_ONES + 16], aff[:, sl], r=[k.bcst, baf], w=[bpt])
        k.RCP(tmpA[:, sl], pt[0:16, 0:BW], r=[bpt], w=[btA])
    k.TT("dve", aff[:], aff[:], tmpA[:], ALU.mult, r=[baf, btA], w=[baf])
    lo, hi, mid, cnt, ge, d1 = [sm[:, i:i + 1] for i in range(6)]
    k.MSET("dve", sm[:], 0.0, w=[bsm])
    k.MSET("dve", hi, 1.0, w=[bsm])
    for itn in range(30):
        k.TT("dve", mid, lo, hi, ALU.add, r=[bsm], w=[bsm])
        k.TS("dve", mid, mid, 0.5, None, ALU.mult, r=[bsm], w=[bsm])
        k.TS("dve", tmpA[:], aff[:], mid, 0.0, ALU.is_ge, ALU.add, accum=cnt, r=[baf, bsm], w=[btA, bsm])
        k.TS("dve", ge, cnt, float(CAP), None, ALU.is_ge, r=[bsm], w=[bsm])
        k.TT("dve", d1, mid, lo, ALU.subtract, r=[bsm], w=[bsm])
        k.STT("dve", lo, d1, ge, lo, ALU.mult, ALU.add, r=[bsm], w=[bsm])
        k.TT("dve", d1, hi, mid, ALU.subtract, r=[bsm], w=[bsm])
        k.STT("dve", hi, d1, ge, mid, ALU.mult, ALU.add, r=[bsm], w=[bsm])
    msk, bmk = k.sb("msk", [16, T], F32)
    k.TS("dve", msk[:], aff[:], lo, None, ALU.is_ge, r=[baf, bsm], w=[bmk])
    k.TT("dve", aff[:], aff[:], msk[:], ALU.mult, r=[baf, bmk], w=[baf])
    k.TS("dve", basee[:], k.cst[:, C_IOTA:C_IOTA + 16], float(CAP), None, ALU.mult, r=[k.bcst], w=[bbe])
    for tt in range(NT):
        pt, bpt = k.ps[tt % 2]
        k.TR(pt[:, 0:16], msk[:, tt * 128:(tt + 1) * 128], k.cst[0:16, C_IDENT:C_IDENT + 16], r=[bmk, k.bcst], w=[bpt])
        k.TR(pt[:, 16:32], aff[:, tt * 128:(tt + 1) * 128], k.cst[0:16, C_IDENT:C_IDENT + 16], r=[baf, k.bcst], w=[bpt])
        k.CP("dve", mT[:, tt, :], pt[:, 0:16], r=[bpt], w=[bmT])
        k.CP("act", wT[:, tt, :], pt[:, 16:32], r=[bpt], w=[bwT])
    for tt in range(NT):
        pt, bpt = k.ps[2 + tt % 2]
        for t2 in range(tt):
            k.MM(pt[:, 0:16], k.cst[:, C_ONES:C_ONES + 128], mT[:, t2, :], start=(t2 == 0), stop=False,
                 r=[k.bcst, bmT], w=[bpt])
        k.MM(pt[:, 0:16], k.cst[:, C_SLT:C_SLT + 128], mT[:, tt, :], start=(tt == 0), stop=True, r=[k.bcst, bmT], w=[bpt])
        k.TT("dve", idxf[:, tt, :], pt[:, 0:16], basee[:], ALU.add, r=[bpt, bbe], w=[bxf])
    k.TS("dve", idxf[:], idxf[:], -BIG, None, ALU.add, r=[bxf], w=[bxf])
    k.TT("dve", idxf[:], idxf[:], mT[:], ALU.mult, r=[bxf, bmT], w=[bxf])
    k.TS("dve", idxf[:], idxf[:], BIG, None, ALU.add, r=[bxf], w=[bxf])
    k.CP("dve", idxi[:], idxf[:], r=[bxf], w=[bxi])
    if "dbg_idx" in k.dbg:
        di = k.dram("dbg_idx", [128, NT * 16], I32)
        k.DMA("sp", di[:, :], idxi[:].rearrange("p a b -> p (a b)"), r=[bxi], w=[k.R("dbgidx")])
        dw = k.dram("dbg_w", [128, NT * 16], F32)
        k.DMA("sp", dw[:, :], wT[:].rearrange("p a b -> p (a b)"), r=[bwT], w=[k.R("dbgw")])
    k.S.barrier()
    k.sb_ptr = persist2
    hb = [k.sb("dhb%d" % i, [128, D], BF16) for i in range(2)]
    bxs = k.R("xs")
    for tt in range(NT):
        h_t, bh = hb[tt % 2]
        k.DMA("sp", h_t[:], k.hn[tt * 128:(tt + 1) * 128, :], r=[k.R("hn", tt)], w=[bh])
        for e in range(NE):
            off = idxi[:, tt, e:e + 1]

            def f(eh, off=off, h_t=h_t):
                return eh.indirect_dma_start(out=k.xs[:, :], out_offset=bass.IndirectOffsetOnAxis(ap=off, axis=0),
                                             in_=h_t[:, :], in_offset=None, bounds_check=bndreg(eh), oob_is_err=False)
            k.S.dma("pool", f, [bh, bxi], [k.R("xsw", tt, e)])
    k.S.barrier()
    k.sb_ptr = persist2
    xr = [k.sb("xr%d" % i, [SR, D], BF16) for i in range(2)]
    xsT, bxT = k.sb("xsT", [128, 16, CAP], BF16)
    hidT, bhid = k.sb("hidT", [128, 8, CAP], BF16)
    wf = [k.sb("ewf%d" % i, [128, 16, 512], F32) for i in range(2)]
    wbf = [k.sb("ewb%d" % i, [128, 16, 512], BF16) for i in range(2)]
    wdf = [k.sb("ewdf%d" % i, [128, 8, 512], F32) for i in range(2)]
    wdb = [k.sb("ewdb%d" % i, [128, 8, 512], BF16) for i in range(2)]
    sg, bsg = k.sb("esg", [128, CAP], F32)
    yt = [k.sb("eyt%d" % i, [SR, 512], F32) for i in range(2)]
    iw = 0
    idw = 0
    iy = 0
    for e in range(NE):
        for stl in range(nst):
            x_r, bxr = xr[stl % 2]
            r0 = e * CAP + stl * SR
            k.DMA("sp", x_r[:], k.xs[r0:r0 + SR, :], r=[], w=[bxr])
            for g4 in range(4):
                pt, bpt = k.ps[g4]
                ptb = pt[:].bitcast(BF16)
                for c in range(4):
                    kc = g4 * 4 + c
                    k.TR(ptb[:, c * SR:(c + 1) * SR], x_r[:, kc * 128:(kc + 1) * 128], k.identb[0:SR, 0:SR],
                         r=[bxr, k.bidentb], w=[bpt])
                k.CP("act" if g4 % 2 else "dve", xsT[:, g4 * 4:(g4 + 1) * 4, stl * SR:(stl + 1) * SR],
                     ptb[:, 0:4 * SR].rearrange("p (c t) -> p c t", c=4), r=[bpt], w=[bxT])
        for fcg in range(2):
            grp = []
            for which, wn in ((0, "w_e_gate"), (1, "w_e_up")):
                wf_t, bwf = wf[which]
                wb_t, bwb = wbf[which]
                k.DMA("sp", wf_t[:], k.w[wn][l, e][:, fcg * 512:(fcg + 1) * 512].rearrange("(kc p) c -> p kc c", p=128), w=[bwf])
                for piece in range(4):
                    k.CP(("dve", "pool", "act", "dve")[(iw + piece) % 4], wb_t[:, :, piece * 128:(piece + 1) * 128],
                         wf_t[:, :, piece * 128:(piece + 1) * 128], r=[bwf], w=[bwb])
                iw += 1
                grp.append((wb_t, bwb))
            for f4 in range(4):
                fc = fcg * 4 + f4
                pg, bpg = k.ps[4 + (fc % 2) * 2]
                pu, bpu = k.ps[5 + (fc % 2) * 2]
                for (wb_t, bwb), pp, bpp in ((grp[0], pg, bpg), (grp[1], pu, bpu)):
                    for kc in range(16):
                        k.MM(pp[:, 0:CAP], wb_t[:, kc, f4 * 128:(f4 + 1) * 128], xsT[:, kc, :], start=(kc == 0), stop=(kc == 15),
                             r=[bwb, bxT], w=[bpp])
                k.ACT(sg[:], pg[:, 0:CAP], AF.Silu, r=[bpg], w=[bsg])
                k.TT("dve", hidT[:, fc, :], pu[:, 0:CAP], sg[:], ALU.mult, r=[bpu, bsg], w=[bhid])
        for cb in range(4):
            wd_f, bwdf = wdf[idw % 2]
            wd_b, bwdb = wdb[idw % 2]
            idw += 1
            k.DMA("sp", wd_f[:], k.w["w_e_down"][l, e][:, cb * 512:(cb + 1) * 512].rearrange("(fc p) c -> p fc c", p=128), w=[bwdf])
            k.CP("pool", wd_b[:, 0:4, :], wd_f[:, 0:4, :], r=[bwdf], w=[bwdb])
            k.CP("dve", wd_b[:, 4:8, :], wd_f[:, 4:8, :], r=[bwdf], w=[bwdb])
            for stl in range(nst):
                py, bpy = k.ps[iy % 4]
                y_t, by = yt[iy % 2]
                iy += 1
                for fc in range(8):
                    k.MM(py[0:SR, 0:512], hidT[:, fc, stl * SR:(stl + 1) * SR], wd_b[:, fc, :], start=(fc == 0), stop=(fc == 7),
                         r=[bhid, bwdb], w=[bpy])
                k.CP("act" if iy % 2 else "dve", y_t[:], py[0:SR, 0:512], r=[bpy], w=[by])
                r0 = e * CAP + stl * SR
                k.DMA("act", k.ys[r0:r0 + SR, cb * 512:(cb + 1) * 512], y_t[:], r=[by], w=[k.R("ysw", e, stl, cb)])
    k.S.barrier()
    k.sb_ptr = persist2
    acc = [k.sb("cacc%d" % i, [128, D], F32) for i in range(2)]
    gb = [k.sb("cgb%d" % i, [128, D], F32) for i in range(3)]
    for g_t, bgb in gb:
        k.MSET("pool", g_t[:], 0.0, w=[bgb])
    ig = 0
    for tt in range(NT):
        a_t, ba = acc[tt % 2]
        k.DMA("sp", a_t[:], k.hbuf[tt * 128:(tt + 1) * 128, :], r=[], w=[ba])
        for e in range(NE):
            g_t, bgb = gb[ig % 3]
            ig += 1
            off = idxi[:, tt, e:e + 1]

            def f(eh, off=off, g_t=g_t):
                return eh.indirect_dma_start(out=g_t[:, :], out_offset=None, in_=k.ys[:, :],
                                             in_offset=bass.IndirectOffsetOnAxis(ap=off, axis=0),
                                             bounds_check=bndreg(eh), oob_is_err=False)
            k.S.dma("pool", f, [bxi], [bgb])
            k.STT("dve", a_t[:], g_t[:], wT[:, tt, e:e + 1], a_t[:], ALU.mult, ALU.add, r=[bgb, bwT, ba], w=[ba])
        k.DMA("act", xdst[tt * 128:(tt + 1) * 128, :], a_t[:], r=[ba], w=[k.R(xdtag, tt)])


def st_delta(k, l):
    T, NT = k.T, k.NT
    k.stage_reset()
    nsb = max(1, T // 2048)
    ident = k.cst[:, C_IDENT:C_IDENT + 128]
    ones = k.cst[:, C_ONES:C_ONES + 128]
    bc = k.bcst
    rr = [0]

    def PR():
        c = rr[0]
        rr[0] += 1
        b, q = c % 8, (c // 8) % 4
        return k.ps[b][0][:, q * 128:(q + 1) * 128], k.ps[b][1]

    def PR2():
        c = rr[0]
        rr[0] += 1
        b, q = c % 8, 2 * ((c // 8) % 2)
        return k.ps[b][0][:, q * 128:(q + 2) * 128], k.ps[b][1], k.ps[b][1]

    regs = [(k.ps[i % 8][0][:, (i // 8) * 128:(i // 8 + 1) * 128], k.ps[i % 8][1]) for i in range(32)]
    prm, bprm = k.sb("dprm", [128, 24], F32)
    for i, n in enumerate(("dt_bias_f", "dt_bias_b", "a_log_f", "a_log_b")):
        k.DMA("sp", prm[:, i * 6:(i + 1) * 6], k.w[n][l].partition_broadcast(128), w=[bprm])
    negA, bnA = k.sb("negA", [128, 12], F32)
    k.ACT(negA[:], prm[:, 12:24], AF.Exp, r=[bprm], w=[bnA])
    k.TS("dve", negA[:], negA[:], -1.0, None, ALU.mult, r=[bnA], w=[bnA])
    smA, bsmA = k.sb("smA", [128, NT, 24], F32)
    for tt in range(NT):
        k.DMA("sp", smA[:, tt, :], k.tm[tt * 128:(tt + 1) * 128, 768:792], r=[k.R("tm", tt)], w=[bsmA])
    beta, bbeta = k.sb("dbeta", [128, NT, 12], F32)
    gg, bgg = k.sb("dg", [128, NT, 12], F32)
    gc, bgc = k.sb("dgc", [128, NT, 12], F32)
    eg, beg = k.sb("deg", [128, NT, 12], F32)
    kd, bkd = k.sb("dkd", [128, NT, 12], F32)
    gend, bgend = k.sb("dgend", [128, NT, 24], F32)
    k.ACT(beta[:], smA[:, :, 0:12], AF.Sigmoid, r=[bsmA], w=[bbeta])
    for tt in range(NT):
        k.TT("dve", gg[:, tt, :], smA[:, tt, 12:24], prm[:, 0:12], ALU.add, r=[bsmA, bprm], w=[bgg])
    k.ACT(gg[:], gg[:], AF.Exp, r=[bgg], w=[bgg])
    k.ACT(gg[:], gg[:], AF.Ln, bias=1.0, r=[bgg], w=[bgg])
    for tt in range(NT):
        k.TT("dve", gg[:, tt, :], gg[:, tt, :], negA[:], ALU.mult, r=[bgg, bnA], w=[bgg])
    for tt in range(NT):
        p, bp = PR()
        k.MM(p[:, 0:6], k.cst[:, C_CUMF:C_CUMF + 128], gg[:, tt, 0:6], r=[bc, bgg], w=[bp])
        k.MM(p[:, 6:12], k.cst[:, C_CUMB:C_CUMB + 128], gg[:, tt, 6:12], r=[bc, bgg], w=[bp])
        k.CP("dve", gc[:, tt, :], p[:, 0:12], r=[bp], w=[bgc])
        p2, bp2 = PR()
        k.MM(p2[:, 0:6], k.cst[:, C_LASTF:C_LASTF + 128], gc[:, tt, 0:6], r=[bc, bgc], w=[bp2])
        k.MM(p2[:, 6:12], k.cst[:, C_LASTB:C_LASTB + 128], gc[:, tt, 6:12], r=[bc, bgc], w=[bp2])
        k.TT("dve", kd[:, tt, :], p2[:, 0:12], gc[:, tt, :], ALU.subtract, r=[bp2, bgc], w=[bkd])
        p3, bp3 = PR()
        for ci, (cf, cb_) in enumerate(((C_SELFA, C_SELBA), (C_SELFB, C_SELBB))):
            k.MM(p3[:, ci * 12:ci * 12 + 6], k.cst[:, cf:cf + 128], gc[:, tt, 0:6], r=[bc, bgc], w=[bp3])
            k.MM(p3[:, ci * 12 + 6:ci * 12 + 12], k.cst[:, cb_:cb_ + 128], gc[:, tt, 6:12], r=[bc, bgc], w=[bp3])
        k.CP("dve", gend[:, tt, :], p3[:, 0:24], r=[bp3], w=[bgend])
    k.ACT(eg[:], gc[:], AF.Exp, r=[bgc], w=[beg])
    k.ACT(kd[:], kd[:], AF.Exp, r=[bkd], w=[bkd])
    k.ACT(gend[:], gend[:], AF.Exp, r=[bgend], w=[bgend])
    ogb, bogb = k.sb("ogb", [128, 128], F32)
    k.DMA("sp", ogb[:], k.w["o_norm"][l].partition_broadcast(128), w=[bogb])
    cwc, bcwc = k.sb("cwc", [128, 3, 18], F32)
    for tap in range(3):
        k.DMA("sp", cwc[:, tap, :], k.w["conv_c"][l][tap].rearrange("(c p) -> p c", p=128), w=[bcwc], slow=True)
    qT, bqT = k.sb("dqT", [128, T], F32)
    kT, bkT = k.sb("dkT", [128, T], F32)
    Kt, bKt = k.sb("dKt", [128, NT, 128], F32)
    Vt, bVt = k.sb("dVt", [128, NT, 128], F32)
    of_, bof = k.sb("dof", [128, NT, 128], F32)
    ob_, bob = k.sb("dob", [128, NT, 128], F32)
    oCT, boCT = k.sb("oCT", [128, T], BF16)
    raw = of_[:].rearrange("p a b -> p (a b)")
    acc = ob_[:].rearrange("p a b -> p (a b)")
    BW = min(512, T)
    tmpb = [k.sb("dtb%d" % i, [128, BW], F32) for i in range(2)]

    BUFS = [[None, None], [None, None]]
    for d_ in range(2):
        for par_ in range(2):
            B = {}
            for nm in ("DG", "DEC", "LM", "LT", "ATT", "ATTT", "PA", "PAT", "PB", "PBT", "WT", "KD"):
                B[nm] = k.sb("%s%d%d" % (nm, d_, par_), [128, 128], F32)
            for nm in ("RHS", "XX"):
                B[nm] = k.sb("%s%d%d" % (nm, d_, par_), [128, 256], F32)
            BUFS[d_][par_] = B
    VN = [k.sb("VN%d" % d_, [128, 128], F32) for d_ in range(2)]
    O1 = [k.sb("O1%d" % d_, [128, 128], F32) for d_ in range(2)]
    SS = [[k.sb("S%d_%d" % (d, i), [128, 128], F32) for i in range(2)] for d in range(2)]
    gout, bgout = k.sb("gout", [128, 128], F32)
    osum, bosum = k.sb("osum", [128, 128], F32)
    onb, bonb = k.sb("onb", [128, 128], BF16)
    fssq, bfssq = k.sb("fssq", [128, 1], F32)
    frs, bfrs = k.sb("frs", [128, 1], F32)
    fj, bfj = k.sb("fj", [128, 128], F32)
    masks = ((C_NEGF, C_STRF), (C_NEGB, C_STRB))

    for h in range(6):
        for which, off, dst, bdst in ((0, O_QC, qT, bqT), (1, O_KC, kT, bkT), (2, O_VC, None, None)):
            ch = which * 6 + h
            rdl = [k.R("pT", off // 128 + h, s) for s in range(nsb)]
            k.DMA("sp", raw, k.pT[off + h * 128:off + (h + 1) * 128, :], r=rdl, w=[bof])
            k.TS("pool", acc, raw, cwc[:, 1, ch:ch + 1], None, ALU.mult, r=[bof, bcwc], w=[bob])
            k.STT("dve", acc[:, 1:T], raw[:, 0:T - 1], cwc[:, 0, ch:ch + 1], acc[:, 1:T], ALU.mult, ALU.add,
                  r=[bof, bcwc, bob], w=[bob])
            k.STT("dve", acc[:, 0:T - 1], raw[:, 1:T], cwc[:, 2, ch:ch + 1], acc[:, 0:T - 1], ALU.mult, ALU.add,
                  r=[bof, bcwc, bob], w=[bob])
            k.ACT(acc, acc, AF.Silu, r=[bob], w=[bob])
            if which < 2:
                for blk in range(T // BW):
                    sl = slice(blk * BW, (blk + 1) * BW)
                    (t0, bt0), (t1, bt1) = tmpb
                    k.ACT(t0[:], acc[:, sl], AF.Square, r=[bob], w=[bt0])
                    pb4 = k.ps[blk % 2]
                    k.MM(pb4[0][:, 0:BW], ones, t0[:], r=[bc, bt0], w=[pb4[1]])
                    k.ACT(t1[:], pb4[0][:, 0:BW], AF.Sqrt, bias=k.epsc[:, 0:1], r=[pb4[1], k.bepsc], w=[bt1])
                    k.RCP(t1[:], t1[:], r=[bt1], w=[bt1])
                    if which == 0:
                        k.STT("dve", dst[:, sl], acc[:, sl], 128.0 ** -0.5, t1[:], ALU.mult, ALU.mult, r=[bob, bt1], w=[bdst])
                    else:
                        k.TT("dve", dst[:, sl], acc[:, sl], t1[:], ALU.mult, r=[bob, bt1], w=[bdst])
            else:
                for tt in range(NT):
                    p, bp = regs[8 + tt % 8]
                    k.TR(p, acc[:, tt * 128:(tt + 1) * 128], ident, r=[bob, bc], w=[bp])
                    k.CP("act" if tt % 2 else "dve", Vt[:, tt, :], p, r=[bp], w=[bVt])
        for tt in range(NT):
            p, bp = regs[8 + tt % 8]
            k.TR(p, kT[:, tt * 128:(tt + 1) * 128], ident, r=[bkT, bc], w=[bp])
            k.CP("act" if tt % 2 else "dve", Kt[:, tt, :], p, r=[bp], w=[bKt])
        k.S.barrier()
        rr[0] = 0
        for d in range(2):
            k.MSET("dve", SS[d][0][0][:], 0.0, w=[SS[d][0][1]])
        scur = [0, 0]

        def intra(d, it):
            par = it % 2
            tt = it if d == 0 else NT - 1 - it
            col = d * 6 + h
            tsl = slice(tt * 128, (tt + 1) * 128)
            cneg, cstr = masks[d]
            bsc = beta[:, tt, col:col + 1]
            gsc = gc[:, tt, col:col + 1]
            esc = eg[:, tt, col:col + 1]
            B = BUFS[d][par]
            (dg, bdg), (dec, bdec), (lm, blm), (lt, blt) = B["DG"], B["DEC"], B["LM"], B["LT"]
            (att, batt), (attT, battT), (rhs, brhs), (xx, bxx) = B["ATT"], B["ATTT"], B["RHS"], B["XX"]
            (wt, bwt), (kdt, bkdt) = B["WT"], B["KD"]
            pkk, bpkk = PR()
            k.MM(pkk, kT[:, tsl], kT[:, tsl], r=[bkT], w=[bpkk])
            k.TS("pool", dg[:], ident, gsc, None, ALU.mult, r=[bc, bgc], w=[bdg])
            yield
            pg, bpg = PR()
            k.MM(pg, ones, dg[:], r=[bc, bdg], w=[bpg])
            k.STT("dve", dec[:], pg, -1.0, k.cst[:, cneg:cneg + 128], ALU.mult, ALU.add, r=[bpg, bc], w=[bdec])
            yield
            k.ACT(dec[:], dec[:], AF.Exp, bias=gsc, r=[bdec, bgc], w=[bdec])
            k.TS("pool", rhs[:, 0:128], Vt[:, tt, :], bsc, None, ALU.mult, r=[bVt, bbeta], w=[brhs])
            k.TS("pool", rhs[:, 128:256], Kt[:, tt, :], bsc, esc, ALU.mult, ALU.mult, r=[bKt, bbeta, beg], w=[brhs])
            yield
            k.STT("dve", lm[:], pkk, bsc, dec[:], ALU.mult, ALU.mult, r=[bpkk, bbeta, bdec], w=[blm])
            k.TT("pool", lm[:], lm[:], k.cst[:, cstr:cstr + 128], ALU.mult, r=[blm, bc], w=[blm])
            yield
            p1, bp1 = PR()
            k.TR(p1, lm[:], ident, r=[blm, bc], w=[bp1])
            k.CP("act", lt[:], p1, r=[bp1], w=[blt])
            yield
            pqk, bpqk = PR()
            k.MM(pqk, qT[:, tsl], kT[:, tsl], r=[bqT, bkT], w=[bpqk])
            k.TT("dve", att[:], pqk, dec[:], ALU.mult, r=[bpqk, bdec], w=[batt])
            k.TS("pool", kdt[:], Kt[:, tt, :], kd[:, tt, col:col + 1], None, ALU.mult, r=[bKt, bkd], w=[bkdt])
            yield
            px, bpxa, bpxb = PR2()
            k.MM(px, lt[:], rhs[:], r=[blt, brhs], w=[bpxa, bpxb])
            k.TT("dve", xx[:], rhs[:], px, ALU.subtract, r=[brhs, bpxa, bpxb], w=[bxx])
            yield
            p2, bp2 = PR()
            k.TR(p2, att[:], ident, r=[batt, bc], w=[bp2])
            k.CP("act", attT[:], p2, r=[bp2], w=[battT])
            yield
            P, bP = lm, blm
            PT_, bPT = lt, blt
            nxt = [(B["PA"], B["PAT"]), (B["PB"], B["PBT"])]
            for lvl in range(5):
                (np_, bnp), (npt, bnpt) = nxt[lvl % 2]
                pt2, bpt2 = PR()
                k.MM(pt2, P[:], PT_[:], r=[bP, bPT], w=[bpt2])
                k.CP("act", npt[:], pt2, r=[bpt2], w=[bnpt])
                if lvl < 4:
                    pp2, bpp2 = PR()
                    k.MM(pp2, PT_[:], P[:], r=[bP, bPT], w=[bpp2])
                    k.CP("pool" if False else "dve", np_[:], pp2, r=[bpp2], w=[bnp])
                yield
                px, bpxa, bpxb = PR2()
                k.MM(px, npt[:], xx[:], r=[bnpt, bxx], w=[bpxa, bpxb])
                k.TT("dve", xx[:], xx[:], px, ALU.add, r=[bxx, bpxa, bpxb], w=[bxx])
                P, bP, PT_, bPT = np_, bnp, npt, bnpt
                yield
            p3, bp3 = PR()
            k.TR(p3, xx[:, 128:256], ident, r=[bxx, bc], w=[bp3])
            k.CP("act", wt[:], p3, r=[bp3], w=[bwt])
            yield

        def rec(d, it):
            par = it % 2
            tt = it if d == 0 else NT - 1 - it
            col = d * 6 + h
            tsl = slice(tt * 128, (tt + 1) * 128)
            B = BUFS[d][par]
            (attT, battT), (xx, bxx), (wt, bwt), (kdt, bkdt) = B["ATTT"], B["XX"], B["WT"], B["KD"]
            (vn, bvn), (o1, bo1) = VN[d], O1[d]
            odst, bodst = (of_, bof) if d == 0 else (ob_, bob)
            for step in range(2):
                ci = step if d == 0 else 1 - step
                rows = slice(ci * 64, (ci + 1) * 64)
                S_, bS = SS[d][scur[d]]
                Sn, bSn = SS[d][1 - scur[d]]
                scur[d] = 1 - scur[d]
                pv, bpv = PR()
                k.MM(pv, wt[:], S_[:], r=[bwt, bS], w=[bpv])
                po1, bpo1 = PR()
                k.MM(po1, qT[:, tsl], S_[:], r=[bqT, bS], w=[bpo1])
                k.TT("dve", vn[rows, :], xx[rows, 0:128], pv[rows, :], ALU.subtract, r=[bxx, bpv], w=[bvn])
                k.ACT(o1[rows, :], po1[rows, :], AF.Copy, scale=eg[rows, tt, col:col + 1], r=[bpo1, beg], w=[bo1])
                yield
                pS, bpS = PR()
                k.MM(pS, kdt[rows, :], vn[rows, :], r=[bkdt, bvn], w=[bpS])
                gcol = ci * 12 + col
                k.STT("dve", Sn[:], S_[:], gend[:, tt, gcol:gcol + 1], pS, ALU.mult, ALU.add, r=[bS, bgend, bpS], w=[bSn])
                po2, bpo2 = PR()
                k.MM(po2, attT[rows, :], vn[rows, :], r=[battT, bvn], w=[bpo2])
                k.TT("pool" if False else "dve", odst[rows, tt, :], o1[rows, :], po2[rows, :], ALU.add, r=[bo1, bpo2], w=[bodst])
                yield

        def run_rr(gens):
            gens = list(gens)
            while gens:
                for g in list(gens):
                    try:
                        next(g)
                    except StopIteration:
                        gens.remove(g)

        run_rr([intra(0, 0), intra(1, 0)])
        for it in range(NT):
            gl = [rec(0, it), rec(1, it)]
            if it + 1 < NT:
                gl += [intra(0, it + 1), intra(1, it + 1)]
            run_rr(gl)
        for tt in range(NT):
            k.DMA("sp", gout[:], k.tm[tt * 128:(tt + 1) * 128, h * 128:(h + 1) * 128], r=[k.R("tm", tt)], w=[bgout])
            k.ACT(gout[:], gout[:], AF.Silu, r=[bgout], w=[bgout])
            k.TT("dve", osum[:], of_[:, tt, :], ob_[:, tt, :], ALU.add, r=[bof, bob], w=[bosum])
            k.ACT(fj[:], osum[:], AF.Square, accum=fssq[:], r=[bosum], w=[bfj, bfssq])
            rstd_from_ssq(k, frs[:], fssq[:], 128, bfrs, bfssq)
            k.STT("dve", osum[:], osum[:], frs[:, 0:1], ogb[:], ALU.mult, ALU.mult, r=[bosum, bfrs, bogb], w=[bosum])
            k.TT("dve", onb[:], osum[:], gout[:], ALU.mult, r=[bosum, bgout], w=[bonb])
            pz, bpz = k.ps[tt % 2]
            pzb = pz[:].bitcast(BF16)
            k.TR(pzb[:, 0:128], onb[:], k.identb[:], r=[bonb, k.bidentb], w=[bpz])
            k.CP("act", oCT[:, tt * 128:(tt + 1) * 128], pzb[:, 0:128], r=[bpz], w=[boCT])
        k.DMA("act", k.oT[1280 + h * 128:1280 + (h + 1) * 128, :], oCT[:], r=[boCT], w=[k.R("oT", 10 + h)])
        k.S.barrier()


def build(T, L, dbg=(), stages=None):
    k = KB(T, L, dbg, stages)
    nc = k.nc
    k.pT = k.dram("pT", [6144, T], F32)
    k.tm = k.dram("tm", [T, NTM], F32)
    k.gT = k.dram("gT", [6144, T], BF16)
    k.oT = k.dram("oT", [D, T], BF16)
    k.hbuf = k.dram("hbuf", [T, D], F32)
    k.xbuf = k.dram("xbuf", [T, D], F32)
    k.rotC = k.dram("rotC", [128, T], F32)
    k.hn = k.dram("hn", [T, D], BF16)
    k.xs = k.dram("xs", [NE * (2 * T // NE), D], BF16)
    k.ys = k.dram("ys", [NE * (2 * T // NE), D], F32)
    k.rotS = k.dram("rotS", [128, T], F32)
    st_setup(k)
    epsc, bepsc = k.sb("epsc", [128, 1], F32)
    k.MSET("dve", epsc[:], EPS, w=[bepsc])
    k.epsc, k.bepsc = epsc, bepsc
    k.sb_base = k.sb_ptr
    for l in range(L):
        xsrc = k.x_in if l == 0 else k.xbuf
        xdst = k.y_out if l == L - 1 else k.xbuf
        if k.on("proj"):
            st_proj(k, l, xsrc, "xres")
        if k.on("mixA"):
            st_mixA(k, l)
        if k.on("rot") and l == 0:
            st_rot(k)
        if k.on("attn"):
            st_attn(k, l)
        if k.on("delta"):
            st_delta(k, l)
        if k.stages is not None and "zeroC" in k.stages:
            k.stage_reset()
            zt, bz = k.sb("zt", [128, T], BF16)
            k.MSET("dve", zt[:], 0.0, w=[bz])
            for c in range(10, 16):
                k.DMA("sp", k.oT[c * 128:(c + 1) * 128, :], zt[:], r=[bz], w=[k.R("oT", c)])
        if k.on("merge"):
            st_merge(k, l, xsrc, "xres")
        if k.stages is not None and "copyh" in k.stages:
            k.stage_reset()
            ct, bct = k.sb("ct", [128, D], F32)
            for tt in range(T // 128):
                k.DMA("sp", ct[:], k.x_in[tt * 128:(tt + 1) * 128, :], w=[bct])
                for cb in range(4):
                    k.DMA("sp", k.hbuf[tt * 128:(tt + 1) * 128, cb * 512:(cb + 1) * 512], ct[:, cb * 512:(cb + 1) * 512],
                          r=[bct], w=[k.R("h", tt, cb)])
        if k.on("moe"):
            st_moe(k, l, xdst, "xres")
    k.S.finish()
    return k


T_FULL = 4096
L_FULL = 2
N_CORES = 4
_CACHE = {}


def kernel(**inputs):
    x = np.ascontiguousarray(inputs["x"], dtype=np.float32)
    pos = np.ascontiguousarray(inputs["positions"]).astype(np.int32)
    B = x.shape[0]
    if "k" not in _CACHE:
        _CACHE["k"] = build(T_FULL, L_FULL)
    k = _CACHE["k"]
    cst = make_consts()
    in_maps = []
    for c in range(N_CORES):
        b = c % B
        m = {"x": x[b], "pos": pos[b:b + 1], "cst": cst}
        for n in k.w:
            m[n] = np.ascontiguousarray(inputs[n], dtype=np.float32)
        in_maps.append(m)
    res = run_bass_kernel_spmd(k.nc, in_maps, core_ids=list(range(N_CORES)))
    out = np.stack([res.results[b]["y"] for b in range(B)], axis=0)
    return out.astype(np.float32)
```

```python
import math
from contextlib import ExitStack
import numpy as np
import concourse.bass as bass
import concourse.mybir as mybir
from concourse.bass_utils import run_bass_kernel_spmd

F32 = mybir.dt.float32
BF16 = mybir.dt.bfloat16
I32 = mybir.dt.int32
AF = mybir.ActivationFunctionType
ALU = mybir.AluOpType
AX = mybir.AxisListType

D = 2048
NIN = 6936
NG = 6144
NE = 16
FF = 1024
EPS = 1e-6
NEG = -1.0e30
ENGS = ("pe", "act", "dve", "pool", "sp")
DQ = ("sp", "act", "pool")
NDSEM = 8


class Buf:
    __slots__ = ("name", "last_w", "readers")

    def __init__(self, name=""):
        self.name = name
        self.last_w = None
        self.readers = []


class Sched:
    def __init__(self, nc, stack):
        self.nc = nc
        self.q = {e: [] for e in ENGS}
        self.cnt = {e: 0 for e in ENGS}
        self.seen = {e: {} for e in ENGS}
        self.stack = stack
        self.epoch = {e: 0 for e in ENGS}
        self.esems = {(e, 0): stack.enter_context(nc.semaphore("s_" + e)) for e in ENGS}
        self.dsem = {e: [stack.enter_context(nc.semaphore("d_%s%d" % (e, i))) for i in range(NDSEM)]
                     for e in DQ}
        self.dcnt = {e: 0 for e in DQ}
        self.dlast = {e: [0] * NDSEM for e in DQ}
        self.nops = 0

    def _sem(self, key):
        return self.esems[(key[1], key[2])] if key[0] == "e" else self.dsem[key[1]][key[2]]

    def _ekey(self, e):
        return ("e", e, self.epoch[e])

    def _need(self, eng, tok, waits):
        if tok is None:
            return
        key, val = tok
        if key[0] == "e" and key[1] == "pe" and eng == "pe":
            return
        if self.seen[eng].get(key, 0) >= val:
            return
        if waits.get(key, 0) < val:
            waits[key] = val

    def _deps(self, eng, reads, writes):
        waits = {}
        for b in reads:
            self._need(eng, b.last_w, waits)
        for b in writes:
            self._need(eng, b.last_w, waits)
            for r in b.readers:
                self._need(eng, r, waits)
        return waits

    def _commit(self, tok, reads, writes):
        for b in reads:
            b.readers.append(tok)
            if len(b.readers) > 48:
                mx = {}
                for k, v in b.readers:
                    if mx.get(k, 0) < v:
                        mx[k] = v
                b.readers = list(mx.items())
        for b in writes:
            b.last_w = tok
            b.readers = []

    def op(self, eng, fn, reads=(), writes=()):
        waits = self._deps(eng, reads, writes)
        for key, val in waits.items():
            self.seen[eng][key] = val
        self.cnt[eng] += 1
        n = self.cnt[eng]
        sem = self.esems[(eng, self.epoch[eng])]
        wl = [(self._sem(k), v) for k, v in waits.items()]

        def emit(e):
            for s, v in wl:
                e.wait_ge(s, v)
            fn(e).then_inc(sem, 1)
        self.q[eng].append(emit)
        self.nops += 1
        tok = (self._ekey(eng), n)
        self._commit(tok, reads, writes)
        return tok

    def dma(self, eng, fn, reads=(), writes=()):
        waits = self._deps(eng, reads, writes)
        j = self.dcnt[eng]
        self.dcnt[eng] += 1
        i = j % NDSEM
        key = ("d", eng, i)
        prev = self.dlast[eng][i]
        if prev and self.seen[eng].get(key, 0) < prev:
            if waits.get(key, 0) < prev:
                waits[key] = prev
        for k, v in waits.items():
            self.seen[eng][k] = v
        val = prev + 16
        self.dlast[eng][i] = val
        sem = self.dsem[eng][i]
        wl = [(self._sem(k), v) for k, v in waits.items()]

        def emit(e):
            for s, v in wl:
                e.wait_ge(s, v)
            fn(e).then_inc(sem, 16)
        self.q[eng].append(emit)
        self.nops += 1
        tok = (key, val)
        self._commit(tok, reads, writes)
        return tok

    def barrier(self):
        for e in ENGS:
            waits = {}
            for e2 in ENGS:
                key = self._ekey(e2)
                if self.cnt[e2] > self.seen[e].get(key, 0):
                    waits[key] = self.cnt[e2]
            for q in DQ:
                for i in range(NDSEM):
                    key = ("d", q, i)
                    v = self.dlast[q][i]
                    if v > self.seen[e].get(key, 0):
                        waits[key] = v
            for k, v in waits.items():
                self.seen[e][k] = v
            wl = [(self._sem(k), v) for k, v in waits.items()]

            def emit(eh, wl=wl):
                for s, v in wl:
                    eh.wait_ge(s, v)
            self.q[e].append(emit)
        for e in ENGS:
            if self.cnt[e] > 16000:
                self.epoch[e] += 1
                self.cnt[e] = 0
                self.esems[(e, self.epoch[e])] = self.stack.enter_context(
                    self.nc.semaphore("s_%s_%d" % (e, self.epoch[e])))

    def finish(self):
        self.barrier()
        nc = self.nc
        with nc.Block() as block:
            @block.tensor
            def _(e):
                for f in self.q["pe"]:
                    f(e)

            @block.scalar
            def _(e):
                for f in self.q["act"]:
                    f(e)

            @block.vector
            def _(e):
                for f in self.q["dve"]:
                    f(e)

            @block.gpsimd
            def _(e):
                for f in self.q["pool"]:
                    f(e)

            @block.sync
            def _(e):
                for f in self.q["sp"]:
                    f(e)


C_IDENT, C_ONES, C_BD64, C_RMAT, C_CUMF, C_CUMB, C_NEGF, C_NEGB, C_STRF, C_STRB, \
    C_SELFA, C_SELFB, C_SELBA, C_SELBB, C_SLT = [i * 128 for i in range(15)]
C_LASTF = 15 * 128
C_LASTB = 16 * 128
C_INVF = 17 * 128
C_SGN = C_INVF + 1
C_IOTA = C_SGN + 1
NCST = C_IOTA + 128


def make_consts():
    c = np.zeros((128, NCST), np.float32)
    i = np.arange(128)[:, None]
    j = np.arange(128)[None, :]
    same = (i // 64) == (j // 64)
    c[:, C_IDENT:C_IDENT + 128] = (i == j)
    c[:, C_ONES:C_ONES + 128] = 1.0
    c[:, C_BD64:C_BD64 + 128] = same
    dd = np.arange(128) % 64
    r = np.zeros((128, 128), np.float32)
    for d in range(128):
        m = d % 64
        if m < 8:
            r[d + 8, d] = 1.0
        elif m < 16:
            r[d - 8, d] = 1.0
    c[:, C_RMAT:C_RMAT + 128] = r
    incl_f = same & (j <= i)
    incl_b = same & (j >= i)
    c[:, C_CUMF:C_CUMF + 128] = incl_f.T
    c[:, C_CUMB:C_CUMB + 128] = incl_b.T
    c[:, C_NEGF:C_NEGF + 128] = np.where(incl_f, 0.0, NEG)
    c[:, C_NEGB:C_NEGB + 128] = np.where(incl_b, 0.0, NEG)
    c[:, C_STRF:C_STRF + 128] = same & (j < i)
    c[:, C_STRB:C_STRB + 128] = same & (j > i)
    for off, row in ((C_SELFA, 63), (C_SELFB, 127), (C_SELBA, 0), (C_SELBB, 64)):
        c[row, off:off + 128] = 1.0
    c[:, C_LASTF:C_LASTF + 128] = (i == (j // 64) * 64 + 63)
    c[:, C_LASTB:C_LASTB + 128] = (i == (j // 64) * 64)
    c[:, C_SLT:C_SLT + 128] = (i < j)
    invf = np.where(dd < 16, 500000.0 ** (-((dd % 8).astype(np.float64)) / 8.0), 0.0)
    c[:, C_INVF] = invf
    c[:, C_SGN] = np.where(dd < 8, -1.0, np.where(dd < 16, 1.0, 0.0))
    c[:, C_IOTA:C_IOTA + 128] = np.arange(128)[None, :]
    return c


O_BA, O_CA, O_VA = 0, 512, 1024
O_QB, O_KB, O_VB = 1536, 2304, 3072
O_QC, O_KC, O_VC = 3840, 4608, 5376
O_TM = 6144
NTM = NIN - O_TM

W_NAMES = ["norm_mix", "w_in", "conv_a", "q_norm", "k_norm", "lambda_q1", "lambda_k1",
           "lambda_q2", "lambda_k2", "subln", "conv_c", "a_log_f", "a_log_b", "dt_bias_f",
           "dt_bias_b", "o_norm", "w_out_a", "w_out_b", "w_out_c", "w_gate", "b_gate", "w_o",
           "norm_ffn", "w_router", "w_e_gate", "w_e_up", "w_e_down"]
W_SHAPES = {
    "norm_mix": [D], "w_in": [D, NIN], "conv_a": [3, 512], "q_norm": [64], "k_norm": [64],
    "lambda_q1": [64], "lambda_k1": [64], "lambda_q2": [64], "lambda_k2": [64], "subln": [128],
    "conv_c": [3, 2304], "a_log_f": [6], "a_log_b": [6], "dt_bias_f": [6], "dt_bias_b": [6],
    "o_norm": [128], "w_out_a": [512, D], "w_out_b": [768, D], "w_out_c": [768, D],
    "w_gate": [D, NG], "b_gate": [NG], "w_o": [D, D], "norm_ffn": [D], "w_router": [D, NE],
    "w_e_gate": [NE, D, FF], "w_e_up": [NE, D, FF], "w_e_down": [NE, FF, D],
}


class LazyW(dict):
    def __init__(self, k):
        super().__init__()
        self.k = k

    def __missing__(self, n):
        v = self.k.nc.dram_tensor(n, [self.k.L] + W_SHAPES[n], F32, kind="ExternalInput").ap()
        self[n] = v
        return v


class KB:
    def __init__(self, T, L, dbg=(), stages=None):
        self.T, self.L = T, L
        self.NT = T // 128
        self.CAP = 2 * T // NE
        self.dbg = set(dbg)
        self.stages = stages
        self.nc = nc = bass.Bass("TRN2", target_bir_lowering=False)
        self.st = ExitStack()
        self.S = Sched(nc, self.st)
        self.sb_base = 16512
        self.sb_ptr = 16512
        self.nalloc = 0
        self.regs = {}
        self.x_in = nc.dram_tensor("x", [T, D], F32, kind="ExternalInput").ap()
        self.pos_in = nc.dram_tensor("pos", [1, T], I32, kind="ExternalInput").ap()
        self.cst_in = nc.dram_tensor("cst", [128, NCST], F32, kind="ExternalInput").ap()
        self.w = LazyW(self)
        self.y_out = nc.dram_tensor("y", [T, D], F32, kind="ExternalOutput").ap()
        self.ps = []
        for i in range(8):
            t = nc.alloc_psum_tensor("psb%d" % i, [128, 512], F32)
            self.ps.append((t, Buf("ps%d" % i)))

    def dram(self, name, shape, dtype):
        kind = "ExternalOutput" if name in self.dbg else "Internal"
        return self.nc.dram_tensor(name, shape, dtype, kind=kind).ap()

    def sb(self, name, shape, dtype, bufs=None):
        esz = 4 if dtype in (F32, I32) else 2
        n = 1
        for s in shape[1:]:
            n *= s
        nbytes = (n * esz + 31) // 32 * 32
        self.nalloc += 1
        t = self.nc.alloc_sbuf_tensor_at("%s_%d" % (name, self.nalloc), list(shape), dtype,
                                         offset=self.sb_ptr)
        self.sb_ptr += nbytes
        assert self.sb_ptr <= 229344, ("SBUF overflow", name, self.sb_ptr)
        return t, Buf(name)

    def stage_reset(self):
        self.S.barrier()
        self.sb_ptr = self.sb_base

    def R(self, *key):
        b = self.regs.get(key)
        if b is None:
            b = self.regs[key] = Buf(str(key))
        return b

    def MM(self, out, lhsT, rhs, start=True, stop=True, r=(), w=()):
        self.S.op("pe", lambda e: e.matmul(out, lhsT=lhsT, rhs=rhs, start=start, stop=stop), r, w)

    def TR(self, out, in_, ident, r=(), w=()):
        self.S.op("pe", lambda e: e.transpose(out, in_, ident), r, w)

    def ACT(self, out, in_, func, bias=0.0, scale=1.0, accum=None, r=(), w=()):
        if accum is None:
            self.S.op("act", lambda e: e.activation(out=out, in_=in_, func=func, bias=bias, scale=scale), r, w)
        else:
            self.S.op("act", lambda e: e.activation(out=out, in_=in_, func=func, bias=bias, scale=scale,
                                                    accum_out=accum), r, w)

    def _eng(self, name):
        return name

    def TT(self, eng, out, in0, in1, op, r=(), w=()):
        self.S.op(eng, lambda e: e.tensor_tensor(out=out, in0=in0, in1=in1, op=op), r, w)

    def TS(self, eng, out, in0, s1, s2, op0, op1=None, accum=None, r=(), w=()):
        def f(e):
            kw = {}
            if op1 is not None:
                kw["op1"] = op1
            if accum is not None:
                kw["accum_out"] = accum
            return e.tensor_scalar(out=out, in0=in0, scalar1=s1, scalar2=s2, op0=op0, **kw)
        self.S.op(eng, f, r, w)

    def STT(self, eng, out, in0, scalar, in1, op0, op1, r=(), w=()):
        self.S.op(eng, lambda e: e.scalar_tensor_tensor(out=out, in0=in0, scalar=scalar, in1=in1,
                                                        op0=op0, op1=op1), r, w)

    def CP(self, eng, out, in_, r=(), w=()):
        if eng == "act":
            self.S.op("act", lambda e: e.copy(out=out, in_=in_), r, w)
        else:
            self.S.op(eng, lambda e: e.tensor_copy(out=out, in_=in_), r, w)

    def RCP(self, out, in_, r=(), w=()):
        self.S.op("dve", lambda e: e.reciprocal(out=out, in_=in_), r, w)

    def MSET(self, eng, ap, val, r=(), w=()):
        self.S.op(eng, lambda e: e.memset(ap, val), r, w)

    def DMA(self, q, out, in_, r=(), w=(), slow=False):
        if slow:
            self.S.dma(q, lambda e: e.dma_start(out=out, in_=in_, allow_slow_non_contiguous=True), r, w)
        else:
            self.S.dma(q, lambda e: e.dma_start(out=out, in_=in_), r, w)

    def on(self, name):
        return self.stages is None or name in self.stages


def st_setup(k):
    cst, bc = k.sb("cst", [128, NCST], F32)
    k.cst, k.bcst = cst, bc
    k.DMA("sp", cst[:], k.cst_in[:, :], w=[bc])
    idb, bidb = k.sb("identb", [128, 128], BF16)
    k.CP("dve", idb[:], cst[:, C_IDENT:C_IDENT + 128], r=[bc], w=[bidb])
    k.identb, k.bidentb = idb, bidb
    k.sb_base = k.sb_ptr


def cs(k, off, n=128, p0=0, p1=128):
    return k.cst[p0:p1, off:off + n]


def rstd_from_ssq(k, rstd, ssq, n, brstd, bssq):
    k.ACT(rstd, ssq, AF.Sqrt, bias=k.epsc[0:rstd.shape[0], 0:1], scale=1.0 / n, r=[bssq, k.bepsc], w=[brstd])
    k.RCP(rstd, rstd, r=[brstd], w=[brstd])


def st_norm_T(k, src, gain_ap, dstT, bdst, t0, nt, tag):
    gbc, bg = k.sb("gbc", [128, D], F32)
    k.DMA("sp", gbc[:], gain_ap.partition_broadcast(128), w=[bg])
    xt = [k.sb("xt%d" % i, [128, D], F32) for i in range(2)]
    xs = [k.sb("xs%d" % i, [128, D], BF16) for i in range(2)]
    junk, bj = k.sb("junk", [128, D], BF16)
    ssq = [k.sb("ssq%d" % i, [128, 1], F32) for i in range(2)]
    rs = [k.sb("rs%d" % i, [128, 1], F32) for i in range(2)]
    for tt in range(nt):
        x_t, bx = xt[tt % 2]
        xs_t, bxs = xs[tt % 2]
        sq, bsq = ssq[tt % 2]
        r_, br = rs[tt % 2]
        row0 = (t0 + tt) * 128
        k.DMA("sp", x_t[:], src[row0:row0 + 128, :], r=[k.R(tag, t0 + tt)], w=[bx])
        k.ACT(junk[:], x_t[:], AF.Square, accum=sq[:], r=[bx], w=[bj, bsq])
        rstd_from_ssq(k, r_[:], sq[:], D, br, bsq)
        k.STT("dve", xs_t[:], x_t[:], r_[:, 0:1], gbc[:], ALU.mult, ALU.mult, r=[bx, br, bg], w=[bxs])
        for g4 in range(4):
            pt, bpt = k.ps[(tt * 4 + g4) % 4]
            ptb = pt[:].bitcast(BF16)
            for c in range(4):
                kc = g4 * 4 + c
                k.TR(ptb[:, c * 128:(c + 1) * 128], xs_t[:, kc * 128:(kc + 1) * 128], k.identb[:],
                     r=[bxs, k.bidentb], w=[bpt])
            eng = "act" if g4 % 2 == 0 else "dve"
            k.CP(eng, dstT[:, g4 * 4:(g4 + 1) * 4, tt * 128:(tt + 1) * 128],
                 ptb[:, 0:512].rearrange("p (c t) -> p c t", c=4), r=[bpt], w=[bdst])


def st_proj(k, l, xsrc, xtag):
    T = k.T
    TB = min(T, 2048)
    w_in, w_gate, b_gate = k.w["w_in"][l], k.w["w_gate"][l], k.w["b_gate"][l]
    for sbi in range(T // TB):
        k.stage_reset()
        xnT, bxn = k.sb("xnT", [128, 16, TB], BF16)
        mark = k.sb_ptr
        st_norm_T(k, xsrc, k.w["norm_mix"][l], xnT, bxn, sbi * TB // 128, TB // 128, xtag)
        k.S.barrier()
        k.sb_ptr = mark
        nblk = TB // 512 if TB >= 512 else 1
        bw = TB // nblk
        wf = [k.sb("wf%d" % i, [128, 16, 128], F32) for i in range(2)]
        wb = [k.sb("wb%d" % i, [128, 16, 128], BF16) for i in range(2)]
        stg = [k.sb("stg%d" % i, [128, TB], F32) for i in range(2)]
        stgb = [k.sb("stgb%d" % i, [128, TB], BF16) for i in range(2)]
        bgc, bbgc = k.sb("bgc", [128, 48], F32)
        k.DMA("sp", bgc[:], b_gate.rearrange("(c p) -> p c", p=128), w=[bbgc], slow=True)
        it = 0
        for kind, ncks in (("in", 48), ("gate", 48)):
            W = w_in if kind == "in" else w_gate
            for cc in range(ncks):
                wf_t, bwf = wf[it % 2]
                wb_t, bwb = wb[it % 2]
                c0 = cc * 128
                k.DMA("sp", wf_t[:], W[:, c0:c0 + 128].rearrange("(kc p) c -> p kc c", p=128), w=[bwf])
                k.CP("pool", wb_t[:], wf_t[:], r=[bwf], w=[bwb])
                if kind == "in":
                    so, bso = stg[it % 2]
                else:
                    so, bso = stgb[it % 2]
                for nb in range(nblk):
                    pt, bpt = k.ps[4 + (it * nblk + nb) % 4]
                    for kc in range(16):
                        k.MM(pt[:, 0:bw], wb_t[:, kc, :], xnT[:, kc, nb * bw:(nb + 1) * bw],
                             start=(kc == 0), stop=(kc == 15), r=[bwb, bxn], w=[bpt])
                    if kind == "in":
                        eng = "act" if nb % 2 == 0 else "dve"
                        k.CP(eng, so[:, nb * bw:(nb + 1) * bw], pt[:, 0:bw], r=[bpt], w=[bso])
                    else:
                        k.ACT(so[:, nb * bw:(nb + 1) * bw], pt[:, 0:bw], AF.Sigmoid, bias=bgc[:, cc:cc + 1],
                              r=[bpt, bbgc], w=[bso])
                if kind == "in":
                    k.DMA("act", k.pT[c0:c0 + 128, sbi * TB:(sbi + 1) * TB], so[:], r=[bso],
                          w=[k.R("pT", cc, sbi)])
                else:
                    k.DMA("act", k.gT[c0:c0 + 128, sbi * TB:(sbi + 1) * TB], so[:], r=[bso],
                          w=[k.R("gT", cc, sbi)])
                it += 1
        k.S.barrier()
        k.sb_ptr = mark
        wtf, bwtf = k.sb("wtf", [128, 4, NTM], F32)
        wtb, bwtb = k.sb("wtb", [128, 16, NTM], BF16)
        for q4 in range(4):
            k.DMA("sp", wtf[:], w_in[q4 * 512:(q4 + 1) * 512, O_TM:NIN].rearrange("(kc p) c -> p kc c", p=128),
                  r=[], w=[bwtf])
            k.CP("pool", wtb[:, q4 * 4:(q4 + 1) * 4, :], wtf[:], r=[bwtf], w=[bwtb])
        so2 = [k.sb("so2%d" % i, [128, NTM], F32) for i in range(2)]
        for tt in range(TB // 128):
            so, bso = so2[tt % 2]
            p0, bp0 = k.ps[(tt % 2) * 2]
            p1, bp1 = k.ps[(tt % 2) * 2 + 1]
            for kc in range(16):
                k.MM(p0[:, 0:512], xnT[:, kc, tt * 128:(tt + 1) * 128], wtb[:, kc, 0:512],
                     start=(kc == 0), stop=(kc == 15), r=[bwtb, bxn], w=[bp0])
            for kc in range(16):
                k.MM(p1[:, 0:NTM - 512], xnT[:, kc, tt * 128:(tt + 1) * 128], wtb[:, kc, 512:NTM],
                     start=(kc == 0), stop=(kc == 15), r=[bwtb, bxn], w=[bp1])
            k.CP("act", so[:, 0:512], p0[:, 0:512], r=[bp0], w=[bso])
            k.CP("dve", so[:, 512:NTM], p1[:, 0:NTM - 512], r=[bp1], w=[bso])
            row0 = sbi * TB + tt * 128
            k.DMA("act", k.tm[row0:row0 + 128, :], so[:], r=[bso], w=[k.R("tm", row0 // 128)])


def st_rot(k):
    T = k.T
    k.stage_reset()
    TWO_PI = 2.0 * math.pi
    posi, bpi = k.sb("posi", [128, T], I32)
    ang, ba = k.sb("ang", [128, T], F32)
    a2, ba2 = k.sb("a2", [128, T], F32)
    kq, bk = k.sb("kq", [128, T], F32)
    ki, bki = k.sb("ki", [128, T], I32)
    m, bm = k.sb("m", [128, T], F32)
    k.DMA("sp", posi[:], k.pos_in[0:1, :].partition_broadcast(128), w=[bpi])
    k.CP("dve", ang[:], posi[:], r=[bpi], w=[ba])
    k.TS("dve", ang[:], ang[:], k.cst[:, C_INVF:C_INVF + 1], None, ALU.mult, r=[ba, k.bcst], w=[ba])
    for which, dst in ((0, k.rotS), (1, k.rotC)):
        shift = 0.0 if which == 0 else math.pi / 2
        k.TS("dve", a2[:], ang[:], shift, None, ALU.add, r=[ba], w=[ba2])
        k.TS("dve", kq[:], a2[:], 1.0 / TWO_PI, None, ALU.mult, r=[ba2], w=[bk])
        k.CP("dve", ki[:], kq[:], r=[bk], w=[bki])
        k.CP("dve", kq[:], ki[:], r=[bki], w=[bk])
        k.STT("dve", a2[:], kq[:], -TWO_PI, a2[:], ALU.mult, ALU.add, r=[bk, ba2], w=[ba2])
        k.TS("dve", m[:], a2[:], math.pi, None, ALU.is_gt, r=[ba2], w=[bm])
        k.STT("dve", a2[:], m[:], -TWO_PI, a2[:], ALU.mult, ALU.add, r=[bm, ba2], w=[ba2])
        k.TS("dve", m[:], a2[:], -math.pi, None, ALU.is_lt, r=[ba2], w=[bm])
        k.STT("dve", a2[:], m[:], TWO_PI, a2[:], ALU.mult, ALU.add, r=[bm, ba2], w=[ba2])
        k.TS("dve", a2[:], a2[:], math.pi, -math.pi, ALU.min, ALU.max, r=[ba2], w=[ba2])
        k.ACT(kq[:], a2[:], AF.Sin, r=[ba2], w=[bk])
        if which == 0:
            k.TS("dve", kq[:], kq[:], k.cst[:, C_SGN:C_SGN + 1], None, ALU.mult, r=[bk, k.bcst], w=[bk])
        k.DMA("sp", dst[:, :], kq[:], r=[bk], w=[k.R("rot", which)])


def st_mixA(k, l):
    T = k.T
    k.stage_reset()
    cw, bcw = k.sb("cwa", [128, 3, 4], F32)
    for tap in range(3):
        k.DMA("sp", cw[:, tap, :], k.w["conv_a"][l][tap].rearrange("(c p) -> p c", p=128), w=[bcw], slow=True)
    b_, bb = k.sb("b_", [128, T], F32)
    c_, bc = k.sb("c_", [128, T], F32)
    v_, bv = k.sb("v_", [128, T], F32)
    acc, bacc = k.sb("acc", [128, T], F32)
    yb, byb = k.sb("yb", [128, T], BF16)
    nsb = max(1, T // 2048)
    for c4 in range(4):
        rd = [k.R("pT", (O_BA // 128) + c4, s) for s in range(nsb)]
        k.DMA("sp", b_[:], k.pT[O_BA + c4 * 128:O_BA + (c4 + 1) * 128, :], r=rd, w=[bb])
        rd = [k.R("pT", (O_CA // 128) + c4, s) for s in range(nsb)]
        k.DMA("sp", c_[:], k.pT[O_CA + c4 * 128:O_CA + (c4 + 1) * 128, :], r=rd, w=[bc])
        rd = [k.R("pT", (O_VA // 128) + c4, s) for s in range(nsb)]
        k.DMA("sp", v_[:], k.pT[O_VA + c4 * 128:O_VA + (c4 + 1) * 128, :], r=rd, w=[bv])
        k.TT("dve", c_[:], c_[:], v_[:], ALU.mult, r=[bc, bv], w=[bc])
        k.ACT(acc[:], c_[:], AF.Copy, scale=cw[:, 1, c4:c4 + 1], r=[bc, bcw], w=[bacc])
        k.STT("dve", acc[:, 1:T], c_[:, 0:T - 1], cw[:, 0, c4:c4 + 1], acc[:, 1:T], ALU.mult, ALU.add,
              r=[bc, bcw, bacc], w=[bacc])
        k.STT("dve", acc[:, 0:T - 1], c_[:, 1:T], cw[:, 2, c4:c4 + 1], acc[:, 0:T - 1], ALU.mult, ALU.add,
              r=[bc, bcw, bacc], w=[bacc])
        k.TT("dve", yb[:], acc[:], b_[:], ALU.mult, r=[bacc, bb], w=[byb])
        k.DMA("act", k.oT[c4 * 128:(c4 + 1) * 128, :], yb[:], r=[byb], w=[k.R("oT", c4)])


def st_attn(k, l):
    T, NT = k.T, k.NT
    lam_init = 0.8 - 0.6 * math.exp(-0.3 * l)
    k.stage_reset()
    nsb = max(1, T // 2048)
    CT, bCT = k.sb("CT", [128, T], F32)
    SN, bSN = k.sb("SN", [128, T], F32)
    k.DMA("sp", CT[:], k.rotC[:, :], r=[k.R("rot", 1)], w=[bCT])
    k.DMA("sp", SN[:], k.rotS[:, :], r=[k.R("rot", 0)], w=[bSN])
    raw, braw = k.sb("raw", [128, T], F32)
    qT, bqT = k.sb("qTr", [128, T], BF16)
    kT, bkT = k.sb("kTr", [128, T], BF16)
    vext, bvx = k.sb("vext", [128, NT, 130], BF16)
    oBT, boBT = k.sb("oBT", [128, T], BF16)
    gq, bgq = k.sb("gq", [128, 1], F32)
    gk, bgk = k.sb("gk", [128, 1], F32)
    for half in range(2):
        k.DMA("sp", gq[half * 64:(half + 1) * 64, :], k.w["q_norm"][l].rearrange("(p o) -> p o", o=1), w=[bgq], slow=True)
        k.DMA("sp", gk[half * 64:(half + 1) * 64, :], k.w["k_norm"][l].rearrange("(p o) -> p o", o=1), w=[bgk], slow=True)
    sgc, bsgc = k.sb("sgc", [128, 1], F32)
    k.DMA("sp", sgc[:], k.w["subln"][l].rearrange("(p o) -> p o", o=1), w=[bsgc], slow=True)
    k.TS("dve", sgc[:], sgc[:], 1.0 - lam_init, None, ALU.mult, r=[bsgc], w=[bsgc])
    onesb, bonesb = k.sb("onesb", [128, 128], BF16)
    k.MSET("dve", onesb[:], 1.0, w=[bonesb])
    rden, brden = k.sb("rden", [1, 2, 512], F32)
    lv, blv = k.sb("lv", [1, 4, 64], F32)
    for i, n in enumerate(("lambda_q1", "lambda_k1", "lambda_q2", "lambda_k2")):
        k.DMA("sp", lv[0:1, i, :], k.w[n][l:l + 1, :], w=[blv])
    lp, blp = k.sb("lp", [1, 2, 64], F32)
    k.TT("dve", lp[0:1, 0, :], lv[0:1, 0, :], lv[0:1, 1, :], ALU.mult, r=[blv], w=[blp])
    k.TT("dve", lp[0:1, 1, :], lv[0:1, 2, :], lv[0:1, 3, :], ALU.mult, r=[blv], w=[blp])
    ls, bls = k.sb("ls", [1, 2], F32)
    k.S.op("dve", lambda e: e.reduce_sum(out=ls[0:1, 0:2], in_=lp[0:1, :, :], axis=AX.X), [blp], [bls])
    k.ACT(ls[0:1, 0:2], ls[0:1, 0:2], AF.Exp, r=[bls], w=[bls])
    nl, bnl = k.sb("nl", [1, 1], F32)
    k.TT("dve", nl[0:1, 0:1], ls[0:1, 1:2], ls[0:1, 0:1], ALU.subtract, r=[bls], w=[bnl])
    k.TS("dve", nl[0:1, 0:1], nl[0:1, 0:1], -lam_init, None, ALU.add, r=[bnl], w=[bnl])
    nlb, bnlb = k.sb("nlb", [128, 1], F32)
    p7, bp7 = k.ps[7]
    k.MM(p7[:, 0:1], k.cst[0:1, C_ONES:C_ONES + 128], nl[0:1, 0:1], r=[k.bcst, bnl], w=[bp7])
    k.CP("dve", nlb[:], p7[:, 0:1], r=[bp7], w=[bnlb])
    import os
    STOP = int(os.environ.get("ATTN_STOP", "99"))
    if STOP == 1:
        return
    tmp = [k.sb("atmp%d" % i, [128, 512], F32) for i in range(4)]
    PT = [k.sb("PT%d" % i, [128, 512], BF16) for i in range(4)]
    o1, bo1 = k.sb("o1", [128, 128], F32)
    o2, bo2 = k.sb("o2", [128, 128], F32)
    ob, bob = k.sb("ob", [128, 128], BF16)
    rd, brd = k.sb("rd", [128, 2], F32)
    ssq, bssq = k.sb("assq", [128, 1], F32)
    rsd, brsd = k.sb("arsd", [128, 1], F32)
    junk, bj = k.sb("ajunk", [128, 128], F32)
    k.MSET("pool", vext[:, :, 128:130], 1.0, w=[bvx])
    BW = min(512, T)
    for h in range(6):
        for which, dstT, bdst, gcol, bgc_, off in ((0, qT, bqT, gq, bgq, O_QB), (1, kT, bkT, gk, bgk, O_KB)):
            rdl = [k.R("pT", off // 128 + h, s) for s in range(nsb)]
            k.DMA("sp", raw[:], k.pT[off + h * 128:off + (h + 1) * 128, :], r=rdl, w=[braw])
            for blk in range(T // BW):
                sl = slice(blk * BW, (blk + 1) * BW)
                (t0, bt0), (t1, bt1), (t2, bt2), (t3, bt3) = tmp
                pa, bpa = k.ps[4 + blk % 2]
                pb, bpb = k.ps[6 + blk % 2]
                k.ACT(t0[:, 0:BW], raw[:, sl], AF.Square, r=[braw], w=[bt0])
                k.MM(pa[:, 0:BW], k.cst[:, C_BD64:C_BD64 + 128], t0[:, 0:BW], r=[k.bcst, bt0], w=[bpa])
                k.ACT(t1[:, 0:BW], pa[:, 0:BW], AF.Sqrt, bias=k.epsc[:, 0:1], scale=1.0 / 64, r=[bpa, k.bepsc], w=[bt1])
                k.RCP(t1[:, 0:BW], t1[:, 0:BW], r=[bt1], w=[bt1])
                k.STT("dve", t2[:, 0:BW], raw[:, sl], gcol[:, 0:1], t1[:, 0:BW], ALU.mult, ALU.mult,
                      r=[braw, bgc_, bt1], w=[bt2])
                k.MM(pb[:, 0:BW], k.cst[:, C_RMAT:C_RMAT + 128], t2[:, 0:BW], r=[k.bcst, bt2], w=[bpb])
                k.TT("dve", t3[:, 0:BW], t2[:, 0:BW], CT[:, sl], ALU.mult, r=[bt2, bCT], w=[bt3])
                k.TT("dve", t0[:, 0:BW], pb[:, 0:BW], SN[:, sl], ALU.mult, r=[bpb, bSN], w=[bt0])
                k.TT("dve", dstT[:, sl], t3[:, 0:BW], t0[:, 0:BW], ALU.add, r=[bt3, bt0], w=[bdst])
        if STOP == 2:
            return
        rdl = [k.R("pT", O_VB // 128 + h, s) for s in range(nsb)]
        k.DMA("sp", raw[:], k.pT[O_VB + h * 128:O_VB + (h + 1) * 128, :], r=rdl, w=[braw])
        for tt in range(NT):
            pa, bpa = k.ps[4 + tt % 4]
            k.TR(pa[:, 0:128], raw[:, tt * 128:(tt + 1) * 128], k.cst[:, C_IDENT:C_IDENT + 128], r=[braw, k.bcst], w=[bpa])
            k.CP("act" if tt % 2 else "dve", vext[:, tt, 0:128], pa[:, 0:128], r=[bpa], w=[bvx])
        if STOP == 3:
            return
        for qb in range(T // BW):
            steps = [(s_, kc) for s_ in range(2) for kc in range(NT)]
            qsl = slice(qb * BW, (qb + 1) * BW)

            def issue_S(i):
                s_, kc = steps[i]
                pst, bpst = k.ps[4 + i % 4]
                k.MM(pst[:, 0:BW], kT[s_ * 64:(s_ + 1) * 64, kc * 128:(kc + 1) * 128],
                     qT[s_ * 64:(s_ + 1) * 64, qsl], r=[bkT, bqT], w=[bpst])
            LA = 3
            for i0_ in range(min(LA, len(steps))):
                issue_S(i0_)
            for i, (s, kc) in enumerate(steps):
                if i + LA < len(steps):
                    issue_S(i + LA)
                pst, bpst = k.ps[4 + i % 4]
                pt_, bpt_ = PT[i % 4]
                k.ACT(pt_[:, 0:BW], pst[:, 0:BW], AF.Exp, scale=0.125, r=[bpst], w=[bpt_])
                po, bpo = k.ps[s]
                pd, bpd = k.ps[2 + s]
                k.MM(po[:, 0:BW], vext[:, kc, 0:128], pt_[:, 0:BW], start=(kc == 0), stop=(kc == NT - 1),
                     r=[bpt_, bvx], w=[bpo])
                k.MM(pd[:, 0:BW], onesb[:, 0:128], pt_[:, 0:BW], start=(kc == 0), stop=(kc == NT - 1),
                     r=[bpt_, bonesb], w=[bpd])
            (t0, bt0), (t1, bt1), (t2, bt2), (t3, bt3) = tmp
            k.RCP(t0[:, 0:BW], k.ps[2][0][:, 0:BW], r=[k.ps[2][1]], w=[bt0])
            k.RCP(t1[:, 0:BW], k.ps[3][0][:, 0:BW], r=[k.ps[3][1]], w=[bt1])
            k.TT("dve", t0[:, 0:BW], k.ps[0][0][:, 0:BW], t0[:, 0:BW], ALU.mult, r=[k.ps[0][1], bt0], w=[bt0])
            k.STT("dve", t1[:, 0:BW], k.ps[1][0][:, 0:BW], nlb[:, 0:1], t1[:, 0:BW], ALU.mult, ALU.mult,
                  r=[k.ps[1][1], bnlb, bt1], w=[bt1])
            k.TT("dve", t2[:, 0:BW], t0[:, 0:BW], t1[:, 0:BW], ALU.add, r=[bt0, bt1], w=[bt2])
            k.ACT(t3[:, 0:BW], t2[:, 0:BW], AF.Square, r=[bt2], w=[bt3])
            pss_, bpss = k.ps[6]
            k.MM(pss_[:, 0:BW], k.cst[:, C_ONES:C_ONES + 128], t3[:, 0:BW], r=[k.bcst, bt3], w=[bpss])
            k.ACT(t0[:, 0:BW], pss_[:, 0:BW], AF.Sqrt, bias=k.epsc[:, 0:1], scale=1.0 / 128, r=[bpss, k.bepsc], w=[bt0])
            k.RCP(t0[:, 0:BW], t0[:, 0:BW], r=[bt0], w=[bt0])
            k.STT("dve", oBT[:, qsl], t2[:, 0:BW], sgc[:, 0:1], t0[:, 0:BW], ALU.mult, ALU.mult, r=[bt2, bsgc, bt0], w=[boBT])
        k.DMA("act", k.oT[512 + h * 128:512 + (h + 1) * 128, :], oBT[:], r=[boBT], w=[k.R("oT", 4 + h)])
        k.S.barrier()
    if "dq" in k.dbg:
        dq = k.dram("dq", [128, T], BF16)
        dk = k.dram("dk", [128, T], BF16)
        dv = k.dram("dv", [128, NT * 130], BF16)
        k.DMA("sp", dq[:, :], qT[:], r=[bqT], w=[k.R("dq")])
        k.DMA("sp", dk[:, :], kT[:], r=[bkT], w=[k.R("dk")])
        k.DMA("sp", dv[:, :], vext[:].rearrange("p a b -> p (a b)"), r=[bvx], w=[k.R("dv")])


def st_merge(k, l, xsrc, xtag):
    T = k.T
    k.stage_reset()
    BW = min(512, T)
    nqt = BW // 128
    wabc = [(k.w["w_out_a"][l], 0, 4), (k.w["w_out_b"][l], 4, 6), (k.w["w_out_c"][l], 10, 6)]
    w_o = k.w["w_o"][l]
    WA, bWA = k.sb("mWA", [128, 16, D], BF16)
    WO, bWO = k.sb("mWO", [128, 16, D], BF16)
    wf = [k.sb("mwf%d" % i, [128, 16, 128], F32) for i in range(2)]
    oTb, boTb = k.sb("oTb", [128, 16, BW], BF16)
    mT, bmT = k.sb("mT", [128, 16, BW], BF16)
    gt = [k.sb("mgt%d" % i, [128, 3, BW], BF16) for i in range(2)]
    t1, bt1 = k.sb("mt1", [128, BW], F32)
    t2, bt2 = k.sb("mt2", [128, BW], F32)
    xt = [k.sb("mxt%d" % i, [128, 512], F32) for i in range(2)]
    ce = ("dve", "act", "pool")
    for oc in range(16):
        wf_t, bwf = wf[oc % 2]
        for (wap, c0, nk) in wabc:
            k.DMA("sp", wf_t[:, c0:c0 + nk, :], wap[:, oc * 128:(oc + 1) * 128].rearrange("(kc p) c -> p kc c", p=128),
                  w=[bwf])
        k.CP(ce[oc % 3], WA[:, :, oc * 128:(oc + 1) * 128], wf_t[:], r=[bwf], w=[bWA])
    for oc in range(16):
        wf_t, bwf = wf[oc % 2]
        k.DMA("sp", wf_t[:], w_o[:, oc * 128:(oc + 1) * 128].rearrange("(kc p) c -> p kc c", p=128), w=[bwf])
        k.CP(ce[(oc + 1) % 3], WO[:, :, oc * 128:(oc + 1) * 128], wf_t[:], r=[bwf], w=[bWO])
    it = 0
    nsb = max(1, T // 2048)
    for tb in range(T // BW):
        tsl = slice(tb * BW, (tb + 1) * BW)
        for c in range(16):
            k.DMA("sp", oTb[:, c, :], k.oT[c * 128:(c + 1) * 128, tsl], r=[k.R("oT", c)], w=[boTb])
        for oc in range(16):
            g_t, bg = gt[it % 2]
            it += 1
            for br in range(3):
                row = br * 2048 + oc * 128
                k.DMA("act", g_t[:, br, :], k.gT[row:row + 128, tsl],
                      r=[k.R("gT", row // 128, s_) for s_ in range(nsb)], w=[bg])
            pss = []
            for bi, (wap, c0, nk) in enumerate(wabc):
                pt, bpt = k.ps[bi + 3 * (oc % 2)]
                for kc in range(c0, c0 + nk):
                    k.MM(pt[:, 0:BW], WA[:, kc, oc * 128:(oc + 1) * 128], oTb[:, kc, :], start=(kc == c0),
                         stop=(kc == c0 + nk - 1), r=[bWA, boTb], w=[bpt])
                pss.append((pt, bpt))
            k.TT("dve", t1[:], pss[0][0][:, 0:BW], g_t[:, 0, :], ALU.mult, r=[pss[0][1], bg], w=[bt1])
            k.TT("dve", t2[:], pss[1][0][:, 0:BW], g_t[:, 1, :], ALU.mult, r=[pss[1][1], bg], w=[bt2])
            k.TT("dve", t1[:], t1[:], t2[:], ALU.add, r=[bt1, bt2], w=[bt1])
            k.TT("dve", t2[:], pss[2][0][:, 0:BW], g_t[:, 2, :], ALU.mult, r=[pss[2][1], bg], w=[bt2])
            k.TT("dve", mT[:, oc, :], t1[:], t2[:], ALU.add, r=[bt1, bt2], w=[bmT])
        iq = 0
        for cb in range(4):
            for qt in range(nqt):
                x_t, bx = xt[iq % 2]
                pt, bpt = k.ps[6 + iq % 2]
                iq += 1
                row0 = tb * BW + qt * 128
                k.DMA("act", x_t[:], xsrc[row0:row0 + 128, cb * 512:(cb + 1) * 512], r=[k.R(xtag, row0 // 128)], w=[bx])
                for kc in range(16):
                    k.MM(pt[:, 0:512], mT[:, kc, qt * 128:(qt + 1) * 128], WO[:, kc, cb * 512:(cb + 1) * 512],
                         start=(kc == 0), stop=(kc == 15), r=[bmT, bWO], w=[bpt])
                k.TT("dve", x_t[:], pt[:, 0:512], x_t[:], ALU.add, r=[bpt, bx], w=[bx])
                k.DMA("act", k.hbuf[row0:row0 + 128, cb * 512:(cb + 1) * 512], x_t[:], r=[bx], w=[k.R("h", row0 // 128, cb)])


def st_moe(k, l, xdst, xdtag):
    T, NT, CAP = k.T, k.NT, k.CAP
    SR = min(128, CAP)
    nst = (CAP + 127) // 128
    NSLOT = NE * CAP
    BIG = float(NSLOT + 1000)
    ident = k.cst[:, C_IDENT:C_IDENT + 128]

    def bndreg(eh):
        if getattr(k, "_bnd", None) is None:
            k._bnd = eh.alloc_register("bnd")
            eh.reg_mov(k._bnd, NSLOT - 1)
        return k._bnd
    k.stage_reset()
    mT, bmT = k.sb("mskT", [128, NT, 16], F32)
    wT, bwT = k.sb("wT", [128, NT, 16], F32)
    idxf, bxf = k.sb("idxf", [128, NT, 16], F32)
    idxi, bxi = k.sb("idxi", [128, NT, 16], I32)
    basee, bbe = k.sb("basee", [128, 16], F32)
    sm, bsm = k.sb("bis", [16, 8], F32)
    persist2 = k.sb_ptr
    lgT, blg = k.sb("lgT", [16, T], F32)
    persist = k.sb_ptr
    gbc, bg = k.sb("gbc2", [128, D], F32)
    k.DMA("sp", gbc[:], k.w["norm_ffn"][l].partition_broadcast(128), w=[bg])
    wr, bwr = k.sb("wr", [128, 16, NE], F32)
    k.DMA("sp", wr[:], k.w["w_router"][l].rearrange("(kc p) e -> p kc e", p=128), w=[bwr])
    xt = [k.sb("hx%d" % i, [128, D], F32) for i in range(2)]
    hnf = [k.sb("hnf%d" % i, [128, D], F32) for i in range(2)]
    hnb = [k.sb("hnb%d" % i, [128, D], BF16) for i in range(2)]
    hT = [k.sb("hT%d" % i, [128, 16, 128], F32) for i in range(2)]
    junk, bj = k.sb("mjunk", [128, D], BF16)
    ssq = [k.sb("mssq%d" % i, [128, 1], F32) for i in range(2)]
    rs = [k.sb("mrs%d" % i, [128, 1], F32) for i in range(2)]
    for tt in range(NT):
        x_t, bx = xt[tt % 2]
        f_t, bf = hnf[tt % 2]
        b_t, bb = hnb[tt % 2]
        h_t, bh = hT[tt % 2]
        sq, bsq = ssq[tt % 2]
        r_, br = rs[tt % 2]
        row0 = tt * 128
        k.DMA("sp", x_t[:], k.hbuf[row0:row0 + 128, :], r=[k.R("h", tt, cb) for cb in range(4)], w=[bx])
        k.ACT(junk[:], x_t[:], AF.Square, accum=sq[:], r=[bx], w=[bj, bsq])
        rstd_from_ssq(k, r_[:], sq[:], D, br, bsq)
        k.STT("dve", f_t[:], x_t[:], r_[:, 0:1], gbc[:], ALU.mult, ALU.mult, r=[bx, br, bg], w=[bf])
        k.CP("act", b_t[:], f_t[:], r=[bf], w=[bb])
        k.DMA("act", k.hn[row0:row0 + 128, :], b_t[:], r=[bb], w=[k.R("hn", tt)])
        for g4 in range(4):
            pt, bpt = k.ps[g4]
            for c in range(4):
                kc = g4 * 4 + c
                k.TR(pt[:, c * 128:(c + 1) * 128], f_t[:, kc * 128:(kc + 1) * 128], ident, r=[bf, k.bcst], w=[bpt])
            k.CP("act" if g4 % 2 else "dve", h_t[:, g4 * 4:(g4 + 1) * 4, :],
                 pt[:, 0:512].rearrange("p (c t) -> p c t", c=4), r=[bpt], w=[bh])
        pl, bpl = k.ps[4 + tt % 2]
        for kc in range(16):
            k.MM(pl[0:16, 0:128], wr[:, kc, :], h_t[:, kc, :], start=(kc == 0), stop=(kc == 15), r=[bwr, bh], w=[bpl])
        k.CP("act", lgT[:, row0:row0 + 128], pl[0:16, 0:128], r=[bpl], w=[blg])
    k.S.barrier()
    k.sb_ptr = persist
    aff, baf = k.sb("aff", [16, T], F32)
    tmpA, btA = k.sb("tmpA", [16, T], F32)
    k.ACT(aff[:], lgT[:], AF.Exp, r=[blg], w=[baf])
    BW = min(512, T)
    for blk in range(T // BW):
        sl = slice(blk * BW, (blk + 1) * BW)
        pt, bpt = k.ps[blk % 2]
        k.MM(pt[0:16, 0:BW], k.cst[0:16, C_ONES:C_ONES + 16], aff[:, sl], r=[k.bcst, baf], w=[bpt])
        k.RCP(tmpA[:, sl], pt[0:16, 0:BW], r=[bpt], w=[btA])
    k.TT("dve", aff[:], aff[:], tmpA[:], ALU.mult, r=[baf, btA], w=[baf])
    lo, hi, mid, cnt, ge, d1 = [sm[:, i:i + 1] for i in range(6)]
    k.MSET("dve", sm[:], 0.0, w=[bsm])
    k.MSET("dve", hi, 1.0, w=[bsm])
    for itn in range(30):
        k.TT("dve", mid, lo, hi, ALU.add, r=[bsm], w=[bsm])
        k.TS("dve", mid, mid, 0.5, None, ALU.mult, r=[bsm], w=[bsm])
        k.TS("dve", tmpA[:], aff[:], mid, 0.0, ALU.is_ge, ALU.add, accum=cnt, r=[baf, bsm], w=[btA, bsm])
        k.TS("dve", ge, cnt, float(CAP), None, ALU.is_ge, r=[bsm], w=[bsm])
        k.TT("dve", d1, mid, lo, ALU.subtract, r=[bsm], w=[bsm])
        k.STT("dve", lo, d1, ge, lo, ALU.mult, ALU.add, r=[bsm], w=[bsm])
        k.TT("dve", d1, hi, mid, ALU.subtract, r=[bsm], w=[bsm])
        k.STT("dve", hi, d1, ge, mid, ALU.mult, ALU.add, r=[bsm], w=[bsm])
    msk, bmk = k.sb("msk", [16, T], F32)
    k.TS("dve", msk[:], aff[:], lo, None, ALU.is_ge, r=[baf, bsm], w=[bmk])
    k.TT("dve", aff[:], aff[:], msk[:], ALU.mult, r=[baf, bmk], w=[baf])
    k.TS("dve", basee[:], k.cst[:, C_IOTA:C_IOTA + 16], float(CAP), None, ALU.mult, r=[k.bcst], w=[bbe])
    for tt in range(NT):
        pt, bpt = k.ps[tt % 2]
        k.TR(pt[:, 0:16], msk[:, tt * 128:(tt + 1) * 128], k.cst[0:16, C_IDENT:C_IDENT + 16], r=[bmk, k.bcst], w=[bpt])
        k.TR(pt[:, 16:32], aff[:, tt * 128:(tt + 1) * 128], k.cst[0:16, C_IDENT:C_IDENT + 16], r=[baf, k.bcst], w=[bpt])
        k.CP("dve", mT[:, tt, :], pt[:, 0:16], r=[bpt], w=[bmT])
        k.CP("act", wT[:, tt, :], pt[:, 16:32], r=[bpt], w=[bwT])
    for tt in range(NT):
        pt, bpt = k.ps[2 + tt % 2]
        for t2 in range(tt):
            k.MM(pt[:, 0:16], k.cst[:, C_ONES:C_ONES + 128], mT[:, t2, :], start=(t2 == 0), stop=False,
                 r=[k.bcst, bmT], w=[bpt])
        k.MM(pt[:, 0:16], k.cst[:, C_SLT:C_SLT + 128], mT[:, tt, :], start=(tt == 0), stop=True, r=[k.bcst, bmT], w=[bpt])
        k.TT("dve", idxf[:, tt, :], pt[:, 0:16], basee[:], ALU.add, r=[bpt, bbe], w=[bxf])
    k.TS("dve", idxf[:], idxf[:], -BIG, None, ALU.add, r=[bxf], w=[bxf])
    k.TT("dve", idxf[:], idxf[:], mT[:], ALU.mult, r=[bxf, bmT], w=[bxf])
    k.TS("dve", idxf[:], idxf[:], BIG, None, ALU.add, r=[bxf], w=[bxf])
    k.CP("dve", idxi[:], idxf[:], r=[bxf], w=[bxi])
    if "dbg_idx" in k.dbg:
        di = k.dram("dbg_idx", [128, NT * 16], I32)
        k.DMA("sp", di[:, :], idxi[:].rearrange("p a b -> p (a b)"), r=[bxi], w=[k.R("dbgidx")])
        dw = k.dram("dbg_w", [128, NT * 16], F32)
        k.DMA("sp", dw[:, :], wT[:].rearrange("p a b -> p (a b)"), r=[bwT], w=[k.R("dbgw")])
    k.S.barrier()
    k.sb_ptr = persist2
    hb = [k.sb("dhb%d" % i, [128, D], BF16) for i in range(2)]
    bxs = k.R("xs")
    for tt in range(NT):
        h_t, bh = hb[tt % 2]
        k.DMA("sp", h_t[:], k.hn[tt * 128:(tt + 1) * 128, :], r=[k.R("hn", tt)], w=[bh])
        for e in range(NE):
            off = idxi[:, tt, e:e + 1]

            def f(eh, off=off, h_t=h_t):
                return eh.indirect_dma_start(out=k.xs[:, :], out_offset=bass.IndirectOffsetOnAxis(ap=off, axis=0),
                                             in_=h_t[:, :], in_offset=None, bounds_check=bndreg(eh), oob_is_err=False)
            k.S.dma("pool", f, [bh, bxi], [k.R("xsw", tt, e)])
    k.S.barrier()
    k.sb_ptr = persist2
    xr = [k.sb("xr%d" % i, [SR, D], BF16) for i in range(2)]
    xsT, bxT = k.sb("xsT", [128, 16, CAP], BF16)
    hidT, bhid = k.sb("hidT", [128, 8, CAP], BF16)
    wf = [k.sb("ewf%d" % i, [128, 16, 512], F32) for i in range(2)]
    wbf = [k.sb("ewb%d" % i, [128, 16, 512], BF16) for i in range(2)]
    wdf = [k.sb("ewdf%d" % i, [128, 8, 512], F32) for i in range(2)]
    wdb = [k.sb("ewdb%d" % i, [128, 8, 512], BF16) for i in range(2)]
    sg, bsg = k.sb("esg", [128, CAP], F32)
    yt = [k.sb("eyt%d" % i, [SR, 512], F32) for i in range(2)]
    iw = 0
    idw = 0
    iy = 0
    for e in range(NE):
        for stl in range(nst):
            x_r, bxr = xr[stl % 2]
            r0 = e * CAP + stl * SR
            k.DMA("sp", x_r[:], k.xs[r0:r0 + SR, :], r=[], w=[bxr])
            for g4 in range(4):
                pt, bpt = k.ps[g4]
                ptb = pt[:].bitcast(BF16)
                for c in range(4):
                    kc = g4 * 4 + c
                    k.TR(ptb[:, c * SR:(c + 1) * SR], x_r[:, kc * 128:(kc + 1) * 128], k.identb[0:SR, 0:SR],
                         r=[bxr, k.bidentb], w=[bpt])
                k.CP("act" if g4 % 2 else "dve", xsT[:, g4 * 4:(g4 + 1) * 4, stl * SR:(stl + 1) * SR],
                     ptb[:, 0:4 * SR].rearrange("p (c t) -> p c t", c=4), r=[bpt], w=[bxT])
        for fcg in range(2):
            grp = []
            for which, wn in ((0, "w_e_gate"), (1, "w_e_up")):
                wf_t, bwf = wf[which]
                wb_t, bwb = wbf[which]
                k.DMA("sp", wf_t[:], k.w[wn][l, e][:, fcg * 512:(fcg + 1) * 512].rearrange("(kc p) c -> p kc c", p=128), w=[bwf])
                for piece in range(4):
                    k.CP(("dve", "pool", "act", "dve")[(iw + piece) % 4], wb_t[:, :, piece * 128:(piece + 1) * 128],
                         wf_t[:, :, piece * 128:(piece + 1) * 128], r=[bwf], w=[bwb])
                iw += 1
                grp.append((wb_t, bwb))
            for f4 in range(4):
                fc = fcg * 4 + f4
                pg, bpg = k.ps[4 + (fc % 2) * 2]
                pu, bpu = k.ps[5 + (fc % 2) * 2]
                for (wb_t, bwb), pp, bpp in ((grp[0], pg, bpg), (grp[1], pu, bpu)):
                    for kc in range(16):
                        k.MM(pp[:, 0:CAP], wb_t[:, kc, f4 * 128:(f4 + 1) * 128], xsT[:, kc, :], start=(kc == 0), stop=(kc == 15),
                             r=[bwb, bxT], w=[bpp])
                k.ACT(sg[:], pg[:, 0:CAP], AF.Silu, r=[bpg], w=[bsg])
                k.TT("dve", hidT[:, fc, :], pu[:, 0:CAP], sg[:], ALU.mult, r=[bpu, bsg], w=[bhid])
        for cb in range(4):
            wd_f, bwdf = wdf[idw % 2]
            wd_b, bwdb = wdb[idw % 2]
            idw += 1
            k.DMA("sp", wd_f[:], k.w["w_e_down"][l, e][:, cb * 512:(cb + 1) * 512].rearrange("(fc p) c -> p fc c", p=128), w=[bwdf])
            k.CP("pool", wd_b[:, 0:4, :], wd_f[:, 0:4, :], r=[bwdf], w=[bwdb])
            k.CP("dve", wd_b[:, 4:8, :], wd_f[:, 4:8, :], r=[bwdf], w=[bwdb])
            for stl in range(nst):
                py, bpy = k.ps[iy % 4]
                y_t, by = yt[iy % 2]
                iy += 1
                for fc in range(8):
                    k.MM(py[0:SR, 0:512], hidT[:, fc, stl * SR:(stl + 1) * SR], wd_b[:, fc, :], start=(fc == 0), stop=(fc == 7),
                         r=[bhid, bwdb], w=[bpy])
                k.CP("act" if iy % 2 else "dve", y_t[:], py[0:SR, 0:512], r=[bpy], w=[by])
                r0 = e * CAP + stl * SR
                k.DMA("act", k.ys[r0:r0 + SR, cb * 512:(cb + 1) * 512], y_t[:], r=[by], w=[k.R("ysw", e, stl, cb)])
    k.S.barrier()
    k.sb_ptr = persist2
    acc = [k.sb("cacc%d" % i, [128, D], F32) for i in range(2)]
    gb = [k.sb("cgb%d" % i, [128, D], F32) for i in range(3)]
    for g_t, bgb in gb:
        k.MSET("pool", g_t[:], 0.0, w=[bgb])
    ig = 0
    for tt in range(NT):
        a_t, ba = acc[tt % 2]
        k.DMA("sp", a_t[:], k.hbuf[tt * 128:(tt + 1) * 128, :], r=[], w=[ba])
        for e in range(NE):
            g_t, bgb = gb[ig % 3]
            ig += 1
            off = idxi[:, tt, e:e + 1]

            def f(eh, off=off, g_t=g_t):
                return eh.indirect_dma_start(out=g_t[:, :], out_offset=None, in_=k.ys[:, :],
                                             in_offset=bass.IndirectOffsetOnAxis(ap=off, axis=0),
                                             bounds_check=bndreg(eh), oob_is_err=False)
            k.S.dma("pool", f, [bxi], [bgb])
            k.STT("dve", a_t[:], g_t[:], wT[:, tt, e:e + 1], a_t[:], ALU.mult, ALU.add, r=[bgb, bwT, ba], w=[ba])
        k.DMA("act", xdst[tt * 128:(tt + 1) * 128, :], a_t[:], r=[ba], w=[k.R(xdtag, tt)])


def st_delta(k, l):
    T, NT = k.T, k.NT
    k.stage_reset()
    nsb = max(1, T // 2048)
    ident = k.cst[:, C_IDENT:C_IDENT + 128]
    ones = k.cst[:, C_ONES:C_ONES + 128]
    bc = k.bcst
    rr = [0]

    def PR():
        c = rr[0]
        rr[0] += 1
        b, q = c % 8, (c // 8) % 4
        return k.ps[b][0][:, q * 128:(q + 1) * 128], k.ps[b][1]

    def PR2():
        c = rr[0]
        rr[0] += 1
        b, q = c % 8, 2 * ((c // 8) % 2)
        return k.ps[b][0][:, q * 128:(q + 2) * 128], k.ps[b][1], k.ps[b][1]

    regs = [(k.ps[i % 8][0][:, (i // 8) * 128:(i // 8 + 1) * 128], k.ps[i % 8][1]) for i in range(32)]
    prm, bprm = k.sb("dprm", [128, 24], F32)
    for i, n in enumerate(("dt_bias_f", "dt_bias_b", "a_log_f", "a_log_b")):
        k.DMA("sp", prm[:, i * 6:(i + 1) * 6], k.w[n][l].partition_broadcast(128), w=[bprm])
    negA, bnA = k.sb("negA", [128, 12], F32)
    k.ACT(negA[:], prm[:, 12:24], AF.Exp, r=[bprm], w=[bnA])
    k.TS("dve", negA[:], negA[:], -1.0, None, ALU.mult, r=[bnA], w=[bnA])
    smA, bsmA = k.sb("smA", [128, NT, 24], F32)
    for tt in range(NT):
        k.DMA("sp", smA[:, tt, :], k.tm[tt * 128:(tt + 1) * 128, 768:792], r=[k.R("tm", tt)], w=[bsmA])
    beta, bbeta = k.sb("dbeta", [128, NT, 12], F32)
    gg, bgg = k.sb("dg", [128, NT, 12], F32)
    gc, bgc = k.sb("dgc", [128, NT, 12], F32)
    eg, beg = k.sb("deg", [128, NT, 12], F32)
    kd, bkd = k.sb("dkd", [128, NT, 12], F32)
    gend, bgend = k.sb("dgend", [128, NT, 24], F32)
    k.ACT(beta[:], smA[:, :, 0:12], AF.Sigmoid, r=[bsmA], w=[bbeta])
    for tt in range(NT):
        k.TT("dve", gg[:, tt, :], smA[:, tt, 12:24], prm[:, 0:12], ALU.add, r=[bsmA, bprm], w=[bgg])
    k.ACT(gg[:], gg[:], AF.Exp, r=[bgg], w=[bgg])
    k.ACT(gg[:], gg[:], AF.Ln, bias=1.0, r=[bgg], w=[bgg])
    for tt in range(NT):
        k.TT("dve", gg[:, tt, :], gg[:, tt, :], negA[:], ALU.mult, r=[bgg, bnA], w=[bgg])
    for tt in range(NT):
        p, bp = PR()
        k.MM(p[:, 0:6], k.cst[:, C_CUMF:C_CUMF + 128], gg[:, tt, 0:6], r=[bc, bgg], w=[bp])
        k.MM(p[:, 6:12], k.cst[:, C_CUMB:C_CUMB + 128], gg[:, tt, 6:12], r=[bc, bgg], w=[bp])
        k.CP("dve", gc[:, tt, :], p[:, 0:12], r=[bp], w=[bgc])
        p2, bp2 = PR()
        k.MM(p2[:, 0:6], k.cst[:, C_LASTF:C_LASTF + 128], gc[:, tt, 0:6], r=[bc, bgc], w=[bp2])
        k.MM(p2[:, 6:12], k.cst[:, C_LASTB:C_LASTB + 128], gc[:, tt, 6:12], r=[bc, bgc], w=[bp2])
        k.TT("dve", kd[:, tt, :], p2[:, 0:12], gc[:, tt, :], ALU.subtract, r=[bp2, bgc], w=[bkd])
        p3, bp3 = PR()
        for ci, (cf, cb_) in enumerate(((C_SELFA, C_SELBA), (C_SELFB, C_SELBB))):
            k.MM(p3[:, ci * 12:ci * 12 + 6], k.cst[:, cf:cf + 128], gc[:, tt, 0:6], r=[bc, bgc], w=[bp3])
            k.MM(p3[:, ci * 12 + 6:ci * 12 + 12], k.cst[:, cb_:cb_ + 128], gc[:, tt, 6:12], r=[bc, bgc], w=[bp3])
        k.CP("dve", gend[:, tt, :], p3[:, 0:24], r=[bp3], w=[bgend])
    k.ACT(eg[:], gc[:], AF.Exp, r=[bgc], w=[beg])
    k.ACT(kd[:], kd[:], AF.Exp, r=[bkd], w=[bkd])
    k.ACT(gend[:], gend[:], AF.Exp, r=[bgend], w=[bgend])
    ogb, bogb = k.sb("ogb", [128, 128], F32)
    k.DMA("sp", ogb[:], k.w["o_norm"][l].partition_broadcast(128), w=[bogb])
    cwc, bcwc = k.sb("cwc", [128, 3, 18], F32)
    for tap in range(3):
        k.DMA("sp", cwc[:, tap, :], k.w["conv_c"][l][tap].rearrange("(c p) -> p c", p=128), w=[bcwc], slow=True)
    qT, bqT = k.sb("dqT", [128, T], F32)
    kT, bkT = k.sb("dkT", [128, T], F32)
    Kt, bKt = k.sb("dKt", [128, NT, 128], F32)
    Vt, bVt = k.sb("dVt", [128, NT, 128], F32)
    of_, bof = k.sb("dof", [128, NT, 128], F32)
    ob_, bob = k.sb("dob", [128, NT, 128], F32)
    oCT, boCT = k.sb("oCT", [128, T], BF16)
    raw = of_[:].rearrange("p a b -> p (a b)")
    acc = ob_[:].rearrange("p a b -> p (a b)")
    BW = min(512, T)
    tmpb = [k.sb("dtb%d" % i, [128, BW], F32) for i in range(2)]

    BUFS = [[None, None], [None, None]]
    for d_ in range(2):
        for par_ in range(2):
            B = {}
            for nm in ("DG", "DEC", "LM", "LT", "ATT", "ATTT", "PA", "PAT", "PB", "PBT", "WT", "KD"):
                B[nm] = k.sb("%s%d%d" % (nm, d_, par_), [128, 128], F32)
            for nm in ("RHS", "XX"):
                B[nm] = k.sb("%s%d%d" % (nm, d_, par_), [128, 256], F32)
            BUFS[d_][par_] = B
    VN = [k.sb("VN%d" % d_, [128, 128], F32) for d_ in range(2)]
    O1 = [k.sb("O1%d" % d_, [128, 128], F32) for d_ in range(2)]
    SS = [[k.sb("S%d_%d" % (d, i), [128, 128], F32) for i in range(2)] for d in range(2)]
    gout, bgout = k.sb("gout", [128, 128], F32)
    osum, bosum = k.sb("osum", [128, 128], F32)
    onb, bonb = k.sb("onb", [128, 128], BF16)
    fssq, bfssq = k.sb("fssq", [128, 1], F32)
    frs, bfrs = k.sb("frs", [128, 1], F32)
    fj, bfj = k.sb("fj", [128, 128], F32)
    masks = ((C_NEGF, C_STRF), (C_NEGB, C_STRB))

    for h in range(6):
        for which, off, dst, bdst in ((0, O_QC, qT, bqT), (1, O_KC, kT, bkT), (2, O_VC, None, None)):
            ch = which * 6 + h
            rdl = [k.R("pT", off // 128 + h, s) for s in range(nsb)]
            k.DMA("sp", raw, k.pT[off + h * 128:off + (h + 1) * 128, :], r=rdl, w=[bof])
            k.ACT(acc, raw, AF.Copy, scale=cwc[:, 1, ch:ch + 1], r=[bof, bcwc], w=[bob])
            k.STT("dve", acc[:, 1:T], raw[:, 0:T - 1], cwc[:, 0, ch:ch + 1], acc[:, 1:T], ALU.mult, ALU.add,
                  r=[bof, bcwc, bob], w=[bob])
            k.STT("dve", acc[:, 0:T - 1], raw[:, 1:T], cwc[:, 2, ch:ch + 1], acc[:, 0:T - 1], ALU.mult, ALU.add,
                  r=[bof, bcwc, bob], w=[bob])
            k.ACT(acc, acc, AF.Silu, r=[bob], w=[bob])
            if which < 2:
                for blk in range(T // BW):
                    sl = slice(blk * BW, (blk + 1) * BW)
                    (t0, bt0), (t1, bt1) = tmpb
                    k.ACT(t0[:], acc[:, sl], AF.Square, r=[bob], w=[bt0])
                    pb4 = k.ps[blk % 2]
                    k.MM(pb4[0][:, 0:BW], ones, t0[:], r=[bc, bt0], w=[pb4[1]])
                    k.ACT(t1[:], pb4[0][:, 0:BW], AF.Sqrt, bias=k.epsc[:, 0:1], r=[pb4[1], k.bepsc], w=[bt1])
                    k.RCP(t1[:], t1[:], r=[bt1], w=[bt1])
                    if which == 0:
                        k.STT("dve", dst[:, sl], acc[:, sl], 128.0 ** -0.5, t1[:], ALU.mult, ALU.mult, r=[bob, bt1], w=[bdst])
                    else:
                        k.TT("dve", dst[:, sl], acc[:, sl], t1[:], ALU.mult, r=[bob, bt1], w=[bdst])
            else:
                for tt in range(NT):
                    p, bp = regs[8 + tt % 8]
                    k.TR(p, acc[:, tt * 128:(tt + 1) * 128], ident, r=[bob, bc], w=[bp])
                    k.CP("act" if tt % 2 else "dve", Vt[:, tt, :], p, r=[bp], w=[bVt])
        for tt in range(NT):
            p, bp = regs[8 + tt % 8]
            k.TR(p, kT[:, tt * 128:(tt + 1) * 128], ident, r=[bkT, bc], w=[bp])
            k.CP("act" if tt % 2 else "dve", Kt[:, tt, :], p, r=[bp], w=[bKt])
        k.S.barrier()
        rr[0] = 0
        for d in range(2):
            k.MSET("dve", SS[d][0][0][:], 0.0, w=[SS[d][0][1]])
        scur = [0, 0]

        def intra(d, it):
            par = it % 2
            tt = it if d == 0 else NT - 1 - it
            col = d * 6 + h
            tsl = slice(tt * 128, (tt + 1) * 128)
            cneg, cstr = masks[d]
            bsc = beta[:, tt, col:col + 1]
            gsc = gc[:, tt, col:col + 1]
            esc = eg[:, tt, col:col + 1]
            B = BUFS[d][par]
            (dg, bdg), (dec, bdec), (lm, blm), (lt, blt) = B["DG"], B["DEC"], B["LM"], B["LT"]
            (att, batt), (attT, battT), (rhs, brhs), (xx, bxx) = B["ATT"], B["ATTT"], B["RHS"], B["XX"]
            (wt, bwt), (kdt, bkdt) = B["WT"], B["KD"]
            pkk, bpkk = PR()
            k.MM(pkk, kT[:, tsl], kT[:, tsl], r=[bkT], w=[bpkk])
            k.ACT(dg[:], ident, AF.Copy, scale=gsc, r=[bc, bgc], w=[bdg])
            yield
            pg, bpg = PR()
            k.MM(pg, ones, dg[:], r=[bc, bdg], w=[bpg])
            k.STT("dve", dec[:], pg, -1.0, k.cst[:, cneg:cneg + 128], ALU.mult, ALU.add, r=[bpg, bc], w=[bdec])
            yield
            k.ACT(dec[:], dec[:], AF.Exp, bias=gsc, r=[bdec, bgc], w=[bdec])
            k.ACT(rhs[:, 0:128], Vt[:, tt, :], AF.Copy, scale=bsc, r=[bVt, bbeta], w=[brhs])
            k.TS("dve", rhs[:, 128:256], Kt[:, tt, :], bsc, esc, ALU.mult, ALU.mult, r=[bKt, bbeta, beg], w=[brhs])
            yield
            k.STT("dve", lm[:], pkk, bsc, dec[:], ALU.mult, ALU.mult, r=[bpkk, bbeta, bdec], w=[blm])
            k.TT("dve", lm[:], lm[:], k.cst[:, cstr:cstr + 128], ALU.mult, r=[blm, bc], w=[blm])
            yield
            p1, bp1 = PR()
            k.TR(p1, lm[:], ident, r=[blm, bc], w=[bp1])
            k.CP("act", lt[:], p1, r=[bp1], w=[blt])
            yield
            pqk, bpqk = PR()
            k.MM(pqk, qT[:, tsl], kT[:, tsl], r=[bqT, bkT], w=[bpqk])
            k.TT("dve", att[:], pqk, dec[:], ALU.mult, r=[bpqk, bdec], w=[batt])
            k.ACT(kdt[:], Kt[:, tt, :], AF.Copy, scale=kd[:, tt, col:col + 1], r=[bKt, bkd], w=[bkdt])
            yield
            px, bpxa, bpxb = PR2()
            k.MM(px, lt[:], rhs[:], r=[blt, brhs], w=[bpxa, bpxb])
            k.TT("dve", xx[:], rhs[:], px, ALU.subtract, r=[brhs, bpxa, bpxb], w=[bxx])
            yield
            p2, bp2 = PR()
            k.TR(p2, att[:], ident, r=[batt, bc], w=[bp2])
            k.CP("act", attT[:], p2, r=[bp2], w=[battT])
            yield
            P, bP = lm, blm
            PT_, bPT = lt, blt
            nxt = [(B["PA"], B["PAT"]), (B["PB"], B["PBT"])]
            for lvl in range(5):
                (np_, bnp), (npt, bnpt) = nxt[lvl % 2]
                pt2, bpt2 = PR()
                k.MM(pt2, P[:], PT_[:], r=[bP, bPT], w=[bpt2])
                k.CP("act", npt[:], pt2, r=[bpt2], w=[bnpt])
                if lvl < 4:
                    pp2, bpp2 = PR()
                    k.MM(pp2, PT_[:], P[:], r=[bP, bPT], w=[bpp2])
                    k.CP("pool" if False else "dve", np_[:], pp2, r=[bpp2], w=[bnp])
                yield
                px, bpxa, bpxb = PR2()
                k.MM(px, npt[:], xx[:], r=[bnpt, bxx], w=[bpxa, bpxb])
                k.TT("dve", xx[:], xx[:], px, ALU.add, r=[bxx, bpxa, bpxb], w=[bxx])
                P, bP, PT_, bPT = np_, bnp, npt, bnpt
                yield
            p3, bp3 = PR()
            k.TR(p3, xx[:, 128:256], ident, r=[bxx, bc], w=[bp3])
            k.CP("act", wt[:], p3, r=[bp3], w=[bwt])
            yield

        def rec(d, it):
            par = it % 2
            tt = it if d == 0 else NT - 1 - it
            col = d * 6 + h
            tsl = slice(tt * 128, (tt + 1) * 128)
            B = BUFS[d][par]
            (attT, battT), (xx, bxx), (wt, bwt), (kdt, bkdt) = B["ATTT"], B["XX"], B["WT"], B["KD"]
            (vn, bvn), (o1, bo1) = VN[d], O1[d]
            odst, bodst = (of_, bof) if d == 0 else (ob_, bob)
            for step in range(2):
                ci = step if d == 0 else 1 - step
                rows = slice(ci * 64, (ci + 1) * 64)
                S_, bS = SS[d][scur[d]]
                Sn, bSn = SS[d][1 - scur[d]]
                scur[d] = 1 - scur[d]
                pv, bpv = PR()
                k.MM(pv, wt[:], S_[:], r=[bwt, bS], w=[bpv])
                po1, bpo1 = PR()
                k.MM(po1, qT[:, tsl], S_[:], r=[bqT, bS], w=[bpo1])
                k.TT("dve", vn[rows, :], xx[rows, 0:128], pv[rows, :], ALU.subtract, r=[bxx, bpv], w=[bvn])
                k.ACT(o1[rows, :], po1[rows, :], AF.Copy, scale=eg[rows, tt, col:col + 1], r=[bpo1, beg], w=[bo1])
                yield
                pS, bpS = PR()
                k.MM(pS, kdt[rows, :], vn[rows, :], r=[bkdt, bvn], w=[bpS])
                gcol = ci * 12 + col
                k.STT("dve", Sn[:], S_[:], gend[:, tt, gcol:gcol + 1], pS, ALU.mult, ALU.add, r=[bS, bgend, bpS], w=[bSn])
                po2, bpo2 = PR()
                k.MM(po2, attT[rows, :], vn[rows, :], r=[battT, bvn], w=[bpo2])
                k.TT("pool" if False else "dve", odst[rows, tt, :], o1[rows, :], po2[rows, :], ALU.add, r=[bo1, bpo2], w=[bodst])
                yield

        def run_rr(gens):
            gens = list(gens)
            while gens:
                for g in list(gens):
                    try:
                        next(g)
                    except StopIteration:
                        gens.remove(g)

        run_rr([intra(0, 0), intra(1, 0)])
        for it in range(NT):
            gl = [rec(0, it), rec(1, it)]
            if it + 1 < NT:
                gl += [intra(0, it + 1), intra(1, it + 1)]
            run_rr(gl)
        for tt in range(NT):
            k.DMA("sp", gout[:], k.tm[tt * 128:(tt + 1) * 128, h * 128:(h + 1) * 128], r=[k.R("tm", tt)], w=[bgout])
            k.ACT(gout[:], gout[:], AF.Silu, r=[bgout], w=[bgout])
            k.TT("dve", osum[:], of_[:, tt, :], ob_[:, tt, :], ALU.add, r=[bof, bob], w=[bosum])
            k.ACT(fj[:], osum[:], AF.Square, accum=fssq[:], r=[bosum], w=[bfj, bfssq])
            rstd_from_ssq(k, frs[:], fssq[:], 128, bfrs, bfssq)
            k.STT("dve", osum[:], osum[:], frs[:, 0:1], ogb[:], ALU.mult, ALU.mult, r=[bosum, bfrs, bogb], w=[bosum])
            k.TT("dve", onb[:], osum[:], gout[:], ALU.mult, r=[bosum, bgout], w=[bonb])
            pz, bpz = k.ps[tt % 2]
            pzb = pz[:].bitcast(BF16)
            k.TR(pzb[:, 0:128], onb[:], k.identb[:], r=[bonb, k.bidentb], w=[bpz])
            k.CP("act", oCT[:, tt * 128:(tt + 1) * 128], pzb[:, 0:128], r=[bpz], w=[boCT])
        k.DMA("act", k.oT[1280 + h * 128:1280 + (h + 1) * 128, :], oCT[:], r=[boCT], w=[k.R("oT", 10 + h)])
        k.S.barrier()


def build(T, L, dbg=(), stages=None):
    k = KB(T, L, dbg, stages)
    nc = k.nc
    k.pT = k.dram("pT", [6144, T], F32)
    k.tm = k.dram("tm", [T, NTM], F32)
    k.gT = k.dram("gT", [6144, T], BF16)
    k.oT = k.dram("oT", [D, T], BF16)
    k.hbuf = k.dram("hbuf", [T, D], F32)
    k.xbuf = k.dram("xbuf", [T, D], F32)
    k.rotC = k.dram("rotC", [128, T], F32)
    k.hn = k.dram("hn", [T, D], BF16)
    k.xs = k.dram("xs", [NE * (2 * T // NE), D], BF16)
    k.ys = k.dram("ys", [NE * (2 * T // NE), D], F32)
    k.rotS = k.dram("rotS", [128, T], F32)
    st_setup(k)
    epsc, bepsc = k.sb("epsc", [128, 1], F32)
    k.MSET("dve", epsc[:], EPS, w=[bepsc])
    k.epsc, k.bepsc = epsc, bepsc
    k.sb_base = k.sb_ptr
    for l in range(L):
        xsrc = k.x_in if l == 0 else k.xbuf
        xdst = k.y_out if l == L - 1 else k.xbuf
        if k.on("proj"):
            st_proj(k, l, xsrc, "xres")
        if k.on("mixA"):
            st_mixA(k, l)
        if k.on("rot") and l == 0:
            st_rot(k)
        if k.on("attn"):
            st_attn(k, l)
        if k.on("delta"):
            st_delta(k, l)
        if k.stages is not None and "zeroC" in k.stages:
            k.stage_reset()
            zt, bz = k.sb("zt", [128, T], BF16)
            k.MSET("dve", zt[:], 0.0, w=[bz])
            for c in range(10, 16):
                k.DMA("sp", k.oT[c * 128:(c + 1) * 128, :], zt[:], r=[bz], w=[k.R("oT", c)])
        if k.on("merge"):
            st_merge(k, l, xsrc, "xres")
        if k.stages is not None and "copyh" in k.stages:
            k.stage_reset()
            ct, bct = k.sb("ct", [128, D], F32)
            for tt in range(T // 128):
                k.DMA("sp", ct[:], k.x_in[tt * 128:(tt + 1) * 128, :], w=[bct])
                for cb in range(4):
                    k.DMA("sp", k.hbuf[tt * 128:(tt + 1) * 128, cb * 512:(cb + 1) * 512], ct[:, cb * 512:(cb + 1) * 512],
                          r=[bct], w=[k.R("h", tt, cb)])
        if k.on("moe"):
            st_moe(k, l, xdst, "xres")
    k.S.finish()
    return k


T_FULL = 4096
L_FULL = 2
N_CORES = 4
_CACHE = {}


def kernel(**inputs):
    x = np.ascontiguousarray(inputs["x"], dtype=np.float32)
    pos = np.ascontiguousarray(inputs["positions"]).astype(np.int32)
    B = x.shape[0]
    if "k" not in _CACHE:
        _CACHE["k"] = build(T_FULL, L_FULL)
    k = _CACHE["k"]
    cst = make_consts()
    in_maps = []
    for c in range(N_CORES):
        b = c % B
        m = {"x": x[b], "pos": pos[b:b + 1], "cst": cst}
        for n in k.w:
            m[n] = np.ascontiguousarray(inputs[n], dtype=np.float32)
        in_maps.append(m)
    res = run_bass_kernel_spmd(k.nc, in_maps, core_ids=list(range(N_CORES)))
    out = np.stack([res.results[b]["y"] for b in range(B)], axis=0)
    return out.astype(np.float32)
```

```python
import math
from contextlib import ExitStack
import numpy as np
import concourse.bass as bass
import concourse.mybir as mybir
from concourse.bass_utils import run_bass_kernel_spmd

F32 = mybir.dt.float32
BF16 = mybir.dt.bfloat16
I32 = mybir.dt.int32
AF = mybir.ActivationFunctionType
ALU = mybir.AluOpType
AX = mybir.AxisListType

D = 2048
NIN = 6936
NG = 6144
NE = 16
FF = 1024
EPS = 1e-6
NEG = -1.0e30
ENGS = ("pe", "act", "dve", "pool", "sp")
DQ = ("sp", "act", "pool")
NDSEM = 8


class Buf:
    __slots__ = ("name", "last_w", "readers")

    def __init__(self, name=""):
        self.name = name
        self.last_w = None
        self.readers = []


class Sched:
    def __init__(self, nc, stack):
        self.nc = nc
        self.q = {e: [] for e in ENGS}
        self.cnt = {e: 0 for e in ENGS}
        self.seen = {e: {} for e in ENGS}
        self.stack = stack
        self.epoch = {e: 0 for e in ENGS}
        self.esems = {(e, 0): stack.enter_context(nc.semaphore("s_" + e)) for e in ENGS}
        self.dsem = {e: [stack.enter_context(nc.semaphore("d_%s%d" % (e, i))) for i in range(NDSEM)]
                     for e in DQ}
        self.dcnt = {e: 0 for e in DQ}
        self.dlast = {e: [0] * NDSEM for e in DQ}
        self.nops = 0

    def _sem(self, key):
        return self.esems[(key[1], key[2])] if key[0] == "e" else self.dsem[key[1]][key[2]]

    def _ekey(self, e):
        return ("e", e, self.epoch[e])

    def _need(self, eng, tok, waits):
        if tok is None:
            return
        key, val = tok
        if key[0] == "e" and key[1] == "pe" and eng == "pe":
            return
        if self.seen[eng].get(key, 0) >= val:
            return
        if waits.get(key, 0) < val:
            waits[key] = val

    def _deps(self, eng, reads, writes):
        waits = {}
        for b in reads:
            self._need(eng, b.last_w, waits)
        for b in writes:
            self._need(eng, b.last_w, waits)
            for r in b.readers:
                self._need(eng, r, waits)
        return waits

    def _commit(self, tok, reads, writes):
        for b in reads:
            b.readers.append(tok)
            if len(b.readers) > 48:
                mx = {}
                for k, v in b.readers:
                    if mx.get(k, 0) < v:
                        mx[k] = v
                b.readers = list(mx.items())
        for b in writes:
            b.last_w = tok
            b.readers = []

    def op(self, eng, fn, reads=(), writes=()):
        waits = self._deps(eng, reads, writes)
        for key, val in waits.items():
            self.seen[eng][key] = val
        self.cnt[eng] += 1
        n = self.cnt[eng]
        sem = self.esems[(eng, self.epoch[eng])]
        wl = [(self._sem(k), v) for k, v in waits.items()]

        def emit(e):
            for s, v in wl:
                e.wait_ge(s, v)
            fn(e).then_inc(sem, 1)
        self.q[eng].append(emit)
        self.nops += 1
        tok = (self._ekey(eng), n)
        self._commit(tok, reads, writes)
        return tok

    def dma(self, eng, fn, reads=(), writes=()):
        waits = self._deps(eng, reads, writes)
        j = self.dcnt[eng]
        self.dcnt[eng] += 1
        i = j % NDSEM
        key = ("d", eng, i)
        prev = self.dlast[eng][i]
        if prev and self.seen[eng].get(key, 0) < prev:
            if waits.get(key, 0) < prev:
                waits[key] = prev
        for k, v in waits.items():
            self.seen[eng][k] = v
        val = prev + 16
        self.dlast[eng][i] = val
        sem = self.dsem[eng][i]
        wl = [(self._sem(k), v) for k, v in waits.items()]

        def emit(e):
            for s, v in wl:
                e.wait_ge(s, v)
            fn(e).then_inc(sem, 16)
        self.q[eng].append(emit)
        self.nops += 1
        tok = (key, val)
        self._commit(tok, reads, writes)
        return tok

    def barrier(self):
        for e in ENGS:
            waits = {}
            for e2 in ENGS:
                key = self._ekey(e2)
                if self.cnt[e2] > self.seen[e].get(key, 0):
                    waits[key] = self.cnt[e2]
            for q in DQ:
                for i in range(NDSEM):
                    key = ("d", q, i)
                    v = self.dlast[q][i]
                    if v > self.seen[e].get(key, 0):
                        waits[key] = v
            for k, v in waits.items():
                self.seen[e][k] = v
            wl = [(self._sem(k), v) for k, v in waits.items()]

            def emit(eh, wl=wl):
                for s, v in wl:
                    eh.wait_ge(s, v)
            self.q[e].append(emit)
        for e in ENGS:
            if self.cnt[e] > 16000:
                self.epoch[e] += 1
                self.cnt[e] = 0
                self.esems[(e, self.epoch[e])] = self.stack.enter_context(
                    self.nc.semaphore("s_%s_%d" % (e, self.epoch[e])))

    def finish(self):
        self.barrier()
        nc = self.nc
        with nc.Block() as block:
            @block.tensor
            def _(e):
                for f in self.q["pe"]:
                    f(e)

            @block.scalar
            def _(e):
                for f in self.q["act"]:
                    f(e)

            @block.vector
            def _(e):
                for f in self.q["dve"]:
                    f(e)

            @block.gpsimd
            def _(e):
                for f in self.q["pool"]:
                    f(e)

            @block.sync
            def _(e):
                for f in self.q["sp"]:
                    f(e)


C_IDENT, C_ONES, C_BD64, C_RMAT, C_CUMF, C_CUMB, C_NEGF, C_NEGB, C_STRF, C_STRB, \
    C_SELFA, C_SELFB, C_SELBA, C_SELBB, C_SLT = [i * 128 for i in range(15)]
C_LASTF = 15 * 128
C_LASTB = 16 * 128
C_INVF = 17 * 128
C_SGN = C_INVF + 1
C_IOTA = C_SGN + 1
NCST = C_IOTA + 128


def make_consts():
    c = np.zeros((128, NCST), np.float32)
    i = np.arange(128)[:, None]
    j = np.arange(128)[None, :]
    same = (i // 64) == (j // 64)
    c[:, C_IDENT:C_IDENT + 128] = (i == j)
    c[:, C_ONES:C_ONES + 128] = 1.0
    c[:, C_BD64:C_BD64 + 128] = same
    dd = np.arange(128) % 64
    r = np.zeros((128, 128), np.float32)
    for d in range(128):
        m = d % 64
        if m < 8:
            r[d + 8, d] = 1.0
        elif m < 16:
            r[d - 8, d] = 1.0
    c[:, C_RMAT:C_RMAT + 128] = r
    incl_f = same & (j <= i)
    incl_b = same & (j >= i)
    c[:, C_CUMF:C_CUMF + 128] = incl_f.T
    c[:, C_CUMB:C_CUMB + 128] = incl_b.T
    c[:, C_NEGF:C_NEGF + 128] = np.where(incl_f, 0.0, NEG)
    c[:, C_NEGB:C_NEGB + 128] = np.where(incl_b, 0.0, NEG)
    c[:, C_STRF:C_STRF + 128] = same & (j < i)
    c[:, C_STRB:C_STRB + 128] = same & (j > i)
    for off, row in ((C_SELFA, 63), (C_SELFB, 127), (C_SELBA, 0), (C_SELBB, 64)):
        c[row, off:off + 128] = 1.0
    c[:, C_LASTF:C_LASTF + 128] = (i == (j // 64) * 64 + 63)
    c[:, C_LASTB:C_LASTB + 128] = (i == (j // 64) * 64)
    c[:, C_SLT:C_SLT + 128] = (i < j)
    invf = np.where(dd < 16, 500000.0 ** (-((dd % 8).astype(np.float64)) / 8.0), 0.0)
    c[:, C_INVF] = invf
    c[:, C_SGN] = np.where(dd < 8, -1.0, np.where(dd < 16, 1.0, 0.0))
    c[:, C_IOTA:C_IOTA + 128] = np.arange(128)[None, :]
    return c


O_BA, O_CA, O_VA = 0, 512, 1024
O_QB, O_KB, O_VB = 1536, 2304, 3072
O_QC, O_KC, O_VC = 3840, 4608, 5376
O_TM = 6144
NTM = NIN - O_TM

W_NAMES = ["norm_mix", "w_in", "conv_a", "q_norm", "k_norm", "lambda_q1", "lambda_k1",
           "lambda_q2", "lambda_k2", "subln", "conv_c", "a_log_f", "a_log_b", "dt_bias_f",
           "dt_bias_b", "o_norm", "w_out_a", "w_out_b", "w_out_c", "w_gate", "b_gate", "w_o",
           "norm_ffn", "w_router", "w_e_gate", "w_e_up", "w_e_down"]
W_SHAPES = {
    "norm_mix": [D], "w_in": [D, NIN], "conv_a": [3, 512], "q_norm": [64], "k_norm": [64],
    "lambda_q1": [64], "lambda_k1": [64], "lambda_q2": [64], "lambda_k2": [64], "subln": [128],
    "conv_c": [3, 2304], "a_log_f": [6], "a_log_b": [6], "dt_bias_f": [6], "dt_bias_b": [6],
    "o_norm": [128], "w_out_a": [512, D], "w_out_b": [768, D], "w_out_c": [768, D],
    "w_gate": [D, NG], "b_gate": [NG], "w_o": [D, D], "norm_ffn": [D], "w_router": [D, NE],
    "w_e_gate": [NE, D, FF], "w_e_up": [NE, D, FF], "w_e_down": [NE, FF, D],
}


class LazyW(dict):
    def __init__(self, k):
        super().__init__()
        self.k = k

    def __missing__(self, n):
        v = self.k.nc.dram_tensor(n, [self.k.L] + W_SHAPES[n], F32, kind="ExternalInput").ap()
        self[n] = v
        return v


class KB:
    def __init__(self, T, L, dbg=(), stages=None):
        self.T, self.L = T, L
        self.NT = T // 128
        self.CAP = 2 * T // NE
        self.dbg = set(dbg)
        self.stages = stages
        self.nc = nc = bass.Bass("TRN2", target_bir_lowering=False)
        self.st = ExitStack()
        self.S = Sched(nc, self.st)
        self.sb_base = 16512
        self.sb_ptr = 16512
        self.nalloc = 0
        self.regs = {}
        self.x_in = nc.dram_tensor("x", [T, D], F32, kind="ExternalInput").ap()
        self.pos_in = nc.dram_tensor("pos", [1, T], I32, kind="ExternalInput").ap()
        self.cst_in = nc.dram_tensor("cst", [128, NCST], F32, kind="ExternalInput").ap()
        self.w = LazyW(self)
        self.y_out = nc.dram_tensor("y", [T, D], F32, kind="ExternalOutput").ap()
        self.ps = []
        for i in range(8):
            t = nc.alloc_psum_tensor("psb%d" % i, [128, 512], F32)
            self.ps.append((t, Buf("ps%d" % i)))

    def dram(self, name, shape, dtype):
        kind = "ExternalOutput" if name in self.dbg else "Internal"
        return self.nc.dram_tensor(name, shape, dtype, kind=kind).ap()

    def sb(self, name, shape, dtype, bufs=None):
        esz = 4 if dtype in (F32, I32) else 2
        n = 1
        for s in shape[1:]:
            n *= s
        nbytes = (n * esz + 31) // 32 * 32
        self.nalloc += 1
        t = self.nc.alloc_sbuf_tensor_at("%s_%d" % (name, self.nalloc), list(shape), dtype,
                                         offset=self.sb_ptr)
        self.sb_ptr += nbytes
        assert self.sb_ptr <= 229344, ("SBUF overflow", name, self.sb_ptr)
        return t, Buf(name)

    def stage_reset(self):
        self.S.barrier()
        self.sb_ptr = self.sb_base

    def R(self, *key):
        b = self.regs.get(key)
        if b is None:
            b = self.regs[key] = Buf(str(key))
        return b

    def MM(self, out, lhsT, rhs, start=True, stop=True, r=(), w=()):
        self.S.op("pe", lambda e: e.matmul(out, lhsT=lhsT, rhs=rhs, start=start, stop=stop), r, w)

    def TR(self, out, in_, ident, r=(), w=()):
        self.S.op("pe", lambda e: e.transpose(out, in_, ident), r, w)

    def ACT(self, out, in_, func, bias=0.0, scale=1.0, accum=None, r=(), w=()):
        if accum is None:
            self.S.op("act", lambda e: e.activation(out=out, in_=in_, func=func, bias=bias, scale=scale), r, w)
        else:
            self.S.op("act", lambda e: e.activation(out=out, in_=in_, func=func, bias=bias, scale=scale,
                                                    accum_out=accum), r, w)

    def _eng(self, name):
        return name

    def TT(self, eng, out, in0, in1, op, r=(), w=()):
        self.S.op(eng, lambda e: e.tensor_tensor(out=out, in0=in0, in1=in1, op=op), r, w)

    def TS(self, eng, out, in0, s1, s2, op0, op1=None, accum=None, r=(), w=()):
        def f(e):
            kw = {}
            if op1 is not None:
                kw["op1"] = op1
            if accum is not None:
                kw["accum_out"] = accum
            return e.tensor_scalar(out=out, in0=in0, scalar1=s1, scalar2=s2, op0=op0, **kw)
        self.S.op(eng, f, r, w)

    def STT(self, eng, out, in0, scalar, in1, op0, op1, r=(), w=()):
        self.S.op(eng, lambda e: e.scalar_tensor_tensor(out=out, in0=in0, scalar=scalar, in1=in1,
                                                        op0=op0, op1=op1), r, w)

    def CP(self, eng, out, in_, r=(), w=()):
        if eng == "act":
            self.S.op("act", lambda e: e.copy(out=out, in_=in_), r, w)
        else:
            self.S.op(eng, lambda e: e.tensor_copy(out=out, in_=in_), r, w)

    def RCP(self, out, in_, r=(), w=()):
        self.S.op("dve", lambda e: e.reciprocal(out=out, in_=in_), r, w)

    def MSET(self, eng, ap, val, r=(), w=()):
        self.S.op(eng, lambda e: e.memset(ap, val), r, w)

    def DMA(self, q, out, in_, r=(), w=(), slow=False):
        if slow:
            self.S.dma(q, lambda e: e.dma_start(out=out, in_=in_, allow_slow_non_contiguous=True), r, w)
        else:
            self.S.dma(q, lambda e: e.dma_start(out=out, in_=in_), r, w)

    def on(self, name):
        return self.stages is None or name in self.stages


def st_setup(k):
    cst, bc = k.sb("cst", [128, NCST], F32)
    k.cst, k.bcst = cst, bc
    k.DMA("sp", cst[:], k.cst_in[:, :], w=[bc])
    idb, bidb = k.sb("identb", [128, 128], BF16)
    k.CP("dve", idb[:], cst[:, C_IDENT:C_IDENT + 128], r=[bc], w=[bidb])
    k.identb, k.bidentb = idb, bidb
    jb, bjb = k.sb("junkb", [128, 512], BF16)
    k.MSET("dve", jb[:], 0.0, w=[bjb])
    k.junkb, k.bjunkb = jb, bjb
    k.sb_base = k.sb_ptr


def pe_warm(k, n=16, bank=7):
    pt, bpt = k.ps[bank]
    for i in range(n):
        k.MM(pt[:, 0:512], k.identb[:], k.junkb[:], start=True, stop=True, r=[k.bidentb, k.bjunkb], w=[bpt])


def cs(k, off, n=128, p0=0, p1=128):
    return k.cst[p0:p1, off:off + n]


def rstd_from_ssq(k, rstd, ssq, n, brstd, bssq):
    k.ACT(rstd, ssq, AF.Sqrt, bias=k.epsc[0:rstd.shape[0], 0:1], scale=1.0 / n, r=[bssq, k.bepsc], w=[brstd])
    k.RCP(rstd, rstd, r=[brstd], w=[brstd])


def st_norm_T(k, src, gain_ap, dstT, bdst, t0, nt, tag):
    gbc, bg = k.sb("gbc", [128, D], F32)
    k.DMA("sp", gbc[:], gain_ap.partition_broadcast(128), w=[bg])
    xt = [k.sb("xt%d" % i, [128, D], F32) for i in range(2)]
    xs = [k.sb("xs%d" % i, [128, D], BF16) for i in range(2)]
    junk, bj = k.sb("junk", [128, D], BF16)
    ssq = [k.sb("ssq%d" % i, [128, 1], F32) for i in range(2)]
    rs = [k.sb("rs%d" % i, [128, 1], F32) for i in range(2)]
    for tt in range(nt):
        x_t, bx = xt[tt % 2]
        xs_t, bxs = xs[tt % 2]
        sq, bsq = ssq[tt % 2]
        r_, br = rs[tt % 2]
        row0 = (t0 + tt) * 128
        k.DMA("sp", x_t[:], src[row0:row0 + 128, :], r=[k.R(tag, t0 + tt)], w=[bx])
        k.ACT(junk[:], x_t[:], AF.Square, accum=sq[:], r=[bx], w=[bj, bsq])
        rstd_from_ssq(k, r_[:], sq[:], D, br, bsq)
        k.STT("dve", xs_t[:], x_t[:], r_[:, 0:1], gbc[:], ALU.mult, ALU.mult, r=[bx, br, bg], w=[bxs])
        for g4 in range(4):
            pt, bpt = k.ps[(tt * 4 + g4) % 4]
            ptb = pt[:].bitcast(BF16)
            for c in range(4):
                kc = g4 * 4 + c
                k.TR(ptb[:, c * 128:(c + 1) * 128], xs_t[:, kc * 128:(kc + 1) * 128], k.identb[:],
                     r=[bxs, k.bidentb], w=[bpt])
            eng = "act" if g4 % 2 == 0 else "dve"
            k.CP(eng, dstT[:, g4 * 4:(g4 + 1) * 4, tt * 128:(tt + 1) * 128],
                 ptb[:, 0:512].rearrange("p (c t) -> p c t", c=4), r=[bpt], w=[bdst])


def st_proj(k, l, xsrc, xtag):
    T = k.T
    TB = min(T, 2048)
    w_in, w_gate, b_gate = k.w["w_in"][l], k.w["w_gate"][l], k.w["b_gate"][l]
    for sbi in range(T // TB):
        k.stage_reset()
        xnT, bxn = k.sb("xnT", [128, 16, TB], BF16)
        mark = k.sb_ptr
        st_norm_T(k, xsrc, k.w["norm_mix"][l], xnT, bxn, sbi * TB // 128, TB // 128, xtag)
        k.S.barrier()
        k.sb_ptr = mark
        nblk = TB // 512 if TB >= 512 else 1
        bw = TB // nblk
        wf = [k.sb("wf%d" % i, [128, 16, 128], F32) for i in range(2)]
        wb = [k.sb("wb%d" % i, [128, 16, 128], BF16) for i in range(2)]
        stg = [k.sb("stg%d" % i, [128, TB], F32) for i in range(2)]
        stgb = [k.sb("stgb%d" % i, [128, TB], BF16) for i in range(2)]
        bgc, bbgc = k.sb("bgc", [128, 48], F32)
        k.DMA("sp", bgc[:], b_gate.rearrange("(c p) -> p c", p=128), w=[bbgc], slow=True)
        it = 0
        for kind, ncks in (("in", 48), ("gate", 48)):
            W = w_in if kind == "in" else w_gate
            for cc in range(ncks):
                wf_t, bwf = wf[it % 2]
                wb_t, bwb = wb[it % 2]
                c0 = cc * 128
                k.DMA("sp", wf_t[:], W[:, c0:c0 + 128].rearrange("(kc p) c -> p kc c", p=128), w=[bwf])
                k.CP("pool", wb_t[:], wf_t[:], r=[bwf], w=[bwb])
                if kind == "in":
                    so, bso = stg[it % 2]
                else:
                    so, bso = stgb[it % 2]
                for nb in range(nblk):
                    pt, bpt = k.ps[4 + (it * nblk + nb) % 4]
                    for kc in range(16):
                        k.MM(pt[:, 0:bw], wb_t[:, kc, :], xnT[:, kc, nb * bw:(nb + 1) * bw],
                             start=(kc == 0), stop=(kc == 15), r=[bwb, bxn], w=[bpt])
                    if kind == "in":
                        eng = "act" if nb % 2 == 0 else "dve"
                        k.CP(eng, so[:, nb * bw:(nb + 1) * bw], pt[:, 0:bw], r=[bpt], w=[bso])
                    else:
                        k.ACT(so[:, nb * bw:(nb + 1) * bw], pt[:, 0:bw], AF.Sigmoid, bias=bgc[:, cc:cc + 1],
                              r=[bpt, bbgc], w=[bso])
                if kind == "in":
                    k.DMA("act", k.pT[c0:c0 + 128, sbi * TB:(sbi + 1) * TB], so[:], r=[bso],
                          w=[k.R("pT", cc, sbi)])
                else:
                    k.DMA("act", k.gT[c0:c0 + 128, sbi * TB:(sbi + 1) * TB], so[:], r=[bso],
                          w=[k.R("gT", cc, sbi)])
                it += 1
        k.S.barrier()
        k.sb_ptr = mark
        wtf, bwtf = k.sb("wtf", [128, 4, NTM], F32)
        wtb, bwtb = k.sb("wtb", [128, 16, NTM], BF16)
        for q4 in range(4):
            k.DMA("sp", wtf[:], w_in[q4 * 512:(q4 + 1) * 512, O_TM:NIN].rearrange("(kc p) c -> p kc c", p=128),
                  r=[], w=[bwtf])
            k.CP("pool", wtb[:, q4 * 4:(q4 + 1) * 4, :], wtf[:], r=[bwtf], w=[bwtb])
        so2 = [k.sb("so2%d" % i, [128, NTM], F32) for i in range(2)]
        for tt in range(TB // 128):
            so, bso = so2[tt % 2]
            p0, bp0 = k.ps[(tt % 2) * 2]
            p1, bp1 = k.ps[(tt % 2) * 2 + 1]
            for kc in range(16):
                k.MM(p0[:, 0:512], xnT[:, kc, tt * 128:(tt + 1) * 128], wtb[:, kc, 0:512],
                     start=(kc == 0), stop=(kc == 15), r=[bwtb, bxn], w=[bp0])
            for kc in range(16):
                k.MM(p1[:, 0:NTM - 512], xnT[:, kc, tt * 128:(tt + 1) * 128], wtb[:, kc, 512:NTM],
                     start=(kc == 0), stop=(kc == 15), r=[bwtb, bxn], w=[bp1])
            k.CP("act", so[:, 0:512], p0[:, 0:512], r=[bp0], w=[bso])
            k.CP("dve", so[:, 512:NTM], p1[:, 0:NTM - 512], r=[bp1], w=[bso])
            row0 = sbi * TB + tt * 128
            k.DMA("act", k.tm[row0:row0 + 128, :], so[:], r=[bso], w=[k.R("tm", row0 // 128)])


def st_rot(k):
    T = k.T
    k.stage_reset()
    TWO_PI = 2.0 * math.pi
    posi, bpi = k.sb("posi", [128, T], I32)
    ang, ba = k.sb("ang", [128, T], F32)
    a2, ba2 = k.sb("a2", [128, T], F32)
    kq, bk = k.sb("kq", [128, T], F32)
    ki, bki = k.sb("ki", [128, T], I32)
    m, bm = k.sb("m", [128, T], F32)
    k.DMA("sp", posi[:], k.pos_in[0:1, :].partition_broadcast(128), w=[bpi])
    k.CP("dve", ang[:], posi[:], r=[bpi], w=[ba])
    k.TS("dve", ang[:], ang[:], k.cst[:, C_INVF:C_INVF + 1], None, ALU.mult, r=[ba, k.bcst], w=[ba])
    for which, dst in ((0, k.rotS), (1, k.rotC)):
        shift = 0.0 if which == 0 else math.pi / 2
        k.TS("dve", a2[:], ang[:], shift, None, ALU.add, r=[ba], w=[ba2])
        k.TS("dve", kq[:], a2[:], 1.0 / TWO_PI, None, ALU.mult, r=[ba2], w=[bk])
        k.CP("dve", ki[:], kq[:], r=[bk], w=[bki])
        k.CP("dve", kq[:], ki[:], r=[bki], w=[bk])
        k.STT("dve", a2[:], kq[:], -TWO_PI, a2[:], ALU.mult, ALU.add, r=[bk, ba2], w=[ba2])
        k.TS("dve", m[:], a2[:], math.pi, None, ALU.is_gt, r=[ba2], w=[bm])
        k.STT("dve", a2[:], m[:], -TWO_PI, a2[:], ALU.mult, ALU.add, r=[bm, ba2], w=[ba2])
        k.TS("dve", m[:], a2[:], -math.pi, None, ALU.is_lt, r=[ba2], w=[bm])
        k.STT("dve", a2[:], m[:], TWO_PI, a2[:], ALU.mult, ALU.add, r=[bm, ba2], w=[ba2])
        k.TS("dve", a2[:], a2[:], math.pi, -math.pi, ALU.min, ALU.max, r=[ba2], w=[ba2])
        k.ACT(kq[:], a2[:], AF.Sin, r=[ba2], w=[bk])
        if which == 0:
            k.TS("dve", kq[:], kq[:], k.cst[:, C_SGN:C_SGN + 1], None, ALU.mult, r=[bk, k.bcst], w=[bk])
        k.DMA("sp", dst[:, :], kq[:], r=[bk], w=[k.R("rot", which)])


def st_mixA(k, l):
    T = k.T
    k.stage_reset()
    cw, bcw = k.sb("cwa", [128, 3, 4], F32)
    for tap in range(3):
        k.DMA("sp", cw[:, tap, :], k.w["conv_a"][l][tap].rearrange("(c p) -> p c", p=128), w=[bcw], slow=True)
    b_, bb = k.sb("b_", [128, T], F32)
    c_, bc = k.sb("c_", [128, T], F32)
    v_, bv = k.sb("v_", [128, T], F32)
    acc, bacc = k.sb("acc", [128, T], F32)
    yb, byb = k.sb("yb", [128, T], BF16)
    nsb = max(1, T // 2048)
    for c4 in range(4):
        rd = [k.R("pT", (O_BA // 128) + c4, s) for s in range(nsb)]
        k.DMA("sp", b_[:], k.pT[O_BA + c4 * 128:O_BA + (c4 + 1) * 128, :], r=rd, w=[bb])
        rd = [k.R("pT", (O_CA // 128) + c4, s) for s in range(nsb)]
        k.DMA("sp", c_[:], k.pT[O_CA + c4 * 128:O_CA + (c4 + 1) * 128, :], r=rd, w=[bc])
        rd = [k.R("pT", (O_VA // 128) + c4, s) for s in range(nsb)]
        k.DMA("sp", v_[:], k.pT[O_VA + c4 * 128:O_VA + (c4 + 1) * 128, :], r=rd, w=[bv])
        k.TT("dve", c_[:], c_[:], v_[:], ALU.mult, r=[bc, bv], w=[bc])
        k.ACT(acc[:], c_[:], AF.Copy, scale=cw[:, 1, c4:c4 + 1], r=[bc, bcw], w=[bacc])
        k.STT("dve", acc[:, 1:T], c_[:, 0:T - 1], cw[:, 0, c4:c4 + 1], acc[:, 1:T], ALU.mult, ALU.add,
              r=[bc, bcw, bacc], w=[bacc])
        k.STT("dve", acc[:, 0:T - 1], c_[:, 1:T], cw[:, 2, c4:c4 + 1], acc[:, 0:T - 1], ALU.mult, ALU.add,
              r=[bc, bcw, bacc], w=[bacc])
        k.TT("dve", yb[:], acc[:], b_[:], ALU.mult, r=[bacc, bb], w=[byb])
        k.DMA("act", k.oT[c4 * 128:(c4 + 1) * 128, :], yb[:], r=[byb], w=[k.R("oT", c4)])


def st_attn(k, l):
    T, NT = k.T, k.NT
    lam_init = 0.8 - 0.6 * math.exp(-0.3 * l)
    k.stage_reset()
    nsb = max(1, T // 2048)
    CT, bCT = k.sb("CT", [128, T], F32)
    SN, bSN = k.sb("SN", [128, T], F32)
    k.DMA("sp", CT[:], k.rotC[:, :], r=[k.R("rot", 1)], w=[bCT])
    k.DMA("sp", SN[:], k.rotS[:, :], r=[k.R("rot", 0)], w=[bSN])
    raw, braw = k.sb("raw", [128, T], F32)
    qT, bqT = k.sb("qTr", [128, T], BF16)
    kT, bkT = k.sb("kTr", [128, T], BF16)
    vext, bvx = k.sb("vext", [128, NT, 130], BF16)
    oBT, boBT = k.sb("oBT", [128, T], BF16)
    gq, bgq = k.sb("gq", [128, 1], F32)
    gk, bgk = k.sb("gk", [128, 1], F32)
    for half in range(2):
        k.DMA("sp", gq[half * 64:(half + 1) * 64, :], k.w["q_norm"][l].rearrange("(p o) -> p o", o=1), w=[bgq], slow=True)
        k.DMA("sp", gk[half * 64:(half + 1) * 64, :], k.w["k_norm"][l].rearrange("(p o) -> p o", o=1), w=[bgk], slow=True)
    sgc, bsgc = k.sb("sgc", [128, 1], F32)
    k.DMA("sp", sgc[:], k.w["subln"][l].rearrange("(p o) -> p o", o=1), w=[bsgc], slow=True)
    k.TS("dve", sgc[:], sgc[:], 1.0 - lam_init, None, ALU.mult, r=[bsgc], w=[bsgc])
    onesb, bonesb = k.sb("onesb", [128, 128], BF16)
    k.MSET("dve", onesb[:], 1.0, w=[bonesb])
    rden, brden = k.sb("rden", [1, 2, 512], F32)
    lv, blv = k.sb("lv", [1, 4, 64], F32)
    for i, n in enumerate(("lambda_q1", "lambda_k1", "lambda_q2", "lambda_k2")):
        k.DMA("sp", lv[0:1, i, :], k.w[n][l:l + 1, :], w=[blv])
    lp, blp = k.sb("lp", [1, 2, 64], F32)
    k.TT("dve", lp[0:1, 0, :], lv[0:1, 0, :], lv[0:1, 1, :], ALU.mult, r=[blv], w=[blp])
    k.TT("dve", lp[0:1, 1, :], lv[0:1, 2, :], lv[0:1, 3, :], ALU.mult, r=[blv], w=[blp])
    ls, bls = k.sb("ls", [1, 2], F32)
    k.S.op("dve", lambda e: e.reduce_sum(out=ls[0:1, 0:2], in_=lp[0:1, :, :], axis=AX.X), [blp], [bls])
    k.ACT(ls[0:1, 0:2], ls[0:1, 0:2], AF.Exp, r=[bls], w=[bls])
    nl, bnl = k.sb("nl", [1, 1], F32)
    k.TT("dve", nl[0:1, 0:1], ls[0:1, 1:2], ls[0:1, 0:1], ALU.subtract, r=[bls], w=[bnl])
    k.TS("dve", nl[0:1, 0:1], nl[0:1, 0:1], -lam_init, None, ALU.add, r=[bnl], w=[bnl])
    nlb, bnlb = k.sb("nlb", [128, 1], F32)
    p7, bp7 = k.ps[7]
    k.MM(p7[:, 0:1], k.cst[0:1, C_ONES:C_ONES + 128], nl[0:1, 0:1], r=[k.bcst, bnl], w=[bp7])
    k.CP("dve", nlb[:], p7[:, 0:1], r=[bp7], w=[bnlb])
    import os
    STOP = int(os.environ.get("ATTN_STOP", "99"))
    if STOP == 1:
        return
    tmp = [k.sb("atmp%d" % i, [128, 512], F32) for i in range(4)]
    PT = [k.sb("PT%d" % i, [128, 512], BF16) for i in range(4)]
    o1, bo1 = k.sb("o1", [128, 128], F32)
    o2, bo2 = k.sb("o2", [128, 128], F32)
    ob, bob = k.sb("ob", [128, 128], BF16)
    rd, brd = k.sb("rd", [128, 2], F32)
    ssq, bssq = k.sb("assq", [128, 1], F32)
    rsd, brsd = k.sb("arsd", [128, 1], F32)
    junk, bj = k.sb("ajunk", [128, 128], F32)
    k.MSET("pool", vext[:, :, 128:130], 1.0, w=[bvx])
    BW = min(512, T)
    for h in range(6):
        for which, dstT, bdst, gcol, bgc_, off in ((0, qT, bqT, gq, bgq, O_QB), (1, kT, bkT, gk, bgk, O_KB)):
            rdl = [k.R("pT", off // 128 + h, s) for s in range(nsb)]
            k.DMA("sp", raw[:], k.pT[off + h * 128:off + (h + 1) * 128, :], r=rdl, w=[braw])
            for blk in range(T // BW):
                sl = slice(blk * BW, (blk + 1) * BW)
                (t0, bt0), (t1, bt1), (t2, bt2), (t3, bt3) = tmp
                pa, bpa = k.ps[4 + blk % 2]
                pb, bpb = k.ps[6 + blk % 2]
                k.ACT(t0[:, 0:BW], raw[:, sl], AF.Square, r=[braw], w=[bt0])
                k.MM(pa[:, 0:BW], k.cst[:, C_BD64:C_BD64 + 128], t0[:, 0:BW], r=[k.bcst, bt0], w=[bpa])
                k.ACT(t1[:, 0:BW], pa[:, 0:BW], AF.Sqrt, bias=k.epsc[:, 0:1], scale=1.0 / 64, r=[bpa, k.bepsc], w=[bt1])
                k.RCP(t1[:, 0:BW], t1[:, 0:BW], r=[bt1], w=[bt1])
                k.STT("dve", t2[:, 0:BW], raw[:, sl], gcol[:, 0:1], t1[:, 0:BW], ALU.mult, ALU.mult,
                      r=[braw, bgc_, bt1], w=[bt2])
                k.MM(pb[:, 0:BW], k.cst[:, C_RMAT:C_RMAT + 128], t2[:, 0:BW], r=[k.bcst, bt2], w=[bpb])
                k.TT("dve", t3[:, 0:BW], t2[:, 0:BW], CT[:, sl], ALU.mult, r=[bt2, bCT], w=[bt3])
                k.TT("dve", t0[:, 0:BW], pb[:, 0:BW], SN[:, sl], ALU.mult, r=[bpb, bSN], w=[bt0])
                k.TT("dve", dstT[:, sl], t3[:, 0:BW], t0[:, 0:BW], ALU.add, r=[bt3, bt0], w=[bdst])
        if STOP == 2:
            return
        rdl = [k.R("pT", O_VB // 128 + h, s) for s in range(nsb)]
        k.DMA("sp", raw[:], k.pT[O_VB + h * 128:O_VB + (h + 1) * 128, :], r=rdl, w=[braw])
        for tt in range(NT):
            pa, bpa = k.ps[4 + tt % 4]
            k.TR(pa[:, 0:128], raw[:, tt * 128:(tt + 1) * 128], k.cst[:, C_IDENT:C_IDENT + 128], r=[braw, k.bcst], w=[bpa])
            k.CP("act" if tt % 2 else "dve", vext[:, tt, 0:128], pa[:, 0:128], r=[bpa], w=[bvx])
        if STOP == 3:
            return
        for qb in range(T // BW):
            steps = [(s_, kc) for s_ in range(2) for kc in range(NT)]
            qsl = slice(qb * BW, (qb + 1) * BW)
            if qb % 2 == 0:
                pe_warm(k, 16, bank=7)

            def issue_S(i):
                s_, kc = steps[i]
                pst, bpst = k.ps[4 + i % 4]
                k.MM(pst[:, 0:BW], kT[s_ * 64:(s_ + 1) * 64, kc * 128:(kc + 1) * 128],
                     qT[s_ * 64:(s_ + 1) * 64, qsl], r=[bkT, bqT], w=[bpst])
            LA = 3
            for i0_ in range(min(LA, len(steps))):
                issue_S(i0_)
            for i, (s, kc) in enumerate(steps):
                if i + LA < len(steps):
                    issue_S(i + LA)
                pst, bpst = k.ps[4 + i % 4]
                pt_, bpt_ = PT[i % 4]
                k.ACT(pt_[:, 0:BW], pst[:, 0:BW], AF.Exp, scale=0.125, r=[bpst], w=[bpt_])
                po, bpo = k.ps[s]
                pd, bpd = k.ps[2 + s]
                k.MM(po[:, 0:BW], vext[:, kc, 0:128], pt_[:, 0:BW], start=(kc == 0), stop=(kc == NT - 1),
                     r=[bpt_, bvx], w=[bpo])
                k.MM(pd[:, 0:BW], onesb[:, 0:128], pt_[:, 0:BW], start=(kc == 0), stop=(kc == NT - 1),
                     r=[bpt_, bonesb], w=[bpd])
            (t0, bt0), (t1, bt1), (t2, bt2), (t3, bt3) = tmp
            k.RCP(t0[:, 0:BW], k.ps[2][0][:, 0:BW], r=[k.ps[2][1]], w=[bt0])
            k.RCP(t1[:, 0:BW], k.ps[3][0][:, 0:BW], r=[k.ps[3][1]], w=[bt1])
            k.TT("dve", t0[:, 0:BW], k.ps[0][0][:, 0:BW], t0[:, 0:BW], ALU.mult, r=[k.ps[0][1], bt0], w=[bt0])
            k.STT("dve", t1[:, 0:BW], k.ps[1][0][:, 0:BW], nlb[:, 0:1], t1[:, 0:BW], ALU.mult, ALU.mult,
                  r=[k.ps[1][1], bnlb, bt1], w=[bt1])
            k.TT("dve", t2[:, 0:BW], t0[:, 0:BW], t1[:, 0:BW], ALU.add, r=[bt0, bt1], w=[bt2])
            k.ACT(t3[:, 0:BW], t2[:, 0:BW], AF.Square, r=[bt2], w=[bt3])
            pss_, bpss = k.ps[6]
            k.MM(pss_[:, 0:BW], k.cst[:, C_ONES:C_ONES + 128], t3[:, 0:BW], r=[k.bcst, bt3], w=[bpss])
            k.ACT(t0[:, 0:BW], pss_[:, 0:BW], AF.Sqrt, bias=k.epsc[:, 0:1], scale=1.0 / 128, r=[bpss, k.bepsc], w=[bt0])
            k.RCP(t0[:, 0:BW], t0[:, 0:BW], r=[bt0], w=[bt0])
            k.STT("dve", oBT[:, qsl], t2[:, 0:BW], sgc[:, 0:1], t0[:, 0:BW], ALU.mult, ALU.mult, r=[bt2, bsgc, bt0], w=[boBT])
        k.DMA("act", k.oT[512 + h * 128:512 + (h + 1) * 128, :], oBT[:], r=[boBT], w=[k.R("oT", 4 + h)])
        k.S.barrier()
    if "dq" in k.dbg:
        dq = k.dram("dq", [128, T], BF16)
        dk = k.dram("dk", [128, T], BF16)
        dv = k.dram("dv", [128, NT * 130], BF16)
        k.DMA("sp", dq[:, :], qT[:], r=[bqT], w=[k.R("dq")])
        k.DMA("sp", dk[:, :], kT[:], r=[bkT], w=[k.R("dk")])
        k.DMA("sp", dv[:, :], vext[:].rearrange("p a b -> p (a b)"), r=[bvx], w=[k.R("dv")])


def st_merge(k, l, xsrc, xtag):
    T = k.T
    k.stage_reset()
    BW = min(512, T)
    nqt = BW // 128
    wabc = [(k.w["w_out_a"][l], 0, 4), (k.w["w_out_b"][l], 4, 6), (k.w["w_out_c"][l], 10, 6)]
    w_o = k.w["w_o"][l]
    WA, bWA = k.sb("mWA", [128, 16, D], BF16)
    WO, bWO = k.sb("mWO", [128, 16, D], BF16)
    wf = [k.sb("mwf%d" % i, [128, 16, 128], F32) for i in range(2)]
    oTb, boTb = k.sb("oTb", [128, 16, BW], BF16)
    mT, bmT = k.sb("mT", [128, 16, BW], BF16)
    gt = [k.sb("mgt%d" % i, [128, 3, BW], BF16) for i in range(2)]
    t1, bt1 = k.sb("mt1", [128, BW], F32)
    t2, bt2 = k.sb("mt2", [128, BW], F32)
    xt = [k.sb("mxt%d" % i, [128, 512], F32) for i in range(2)]
    ce = ("dve", "act", "pool")
    for oc in range(16):
        wf_t, bwf = wf[oc % 2]
        for (wap, c0, nk) in wabc:
            k.DMA("sp", wf_t[:, c0:c0 + nk, :], wap[:, oc * 128:(oc + 1) * 128].rearrange("(kc p) c -> p kc c", p=128),
                  w=[bwf])
        k.CP(ce[oc % 3], WA[:, :, oc * 128:(oc + 1) * 128], wf_t[:], r=[bwf], w=[bWA])
    for oc in range(16):
        wf_t, bwf = wf[oc % 2]
        k.DMA("sp", wf_t[:], w_o[:, oc * 128:(oc + 1) * 128].rearrange("(kc p) c -> p kc c", p=128), w=[bwf])
        k.CP(ce[(oc + 1) % 3], WO[:, :, oc * 128:(oc + 1) * 128], wf_t[:], r=[bwf], w=[bWO])
    it = 0
    nsb = max(1, T // 2048)
    for tb in range(T // BW):
        tsl = slice(tb * BW, (tb + 1) * BW)
        for c in range(16):
            k.DMA("sp", oTb[:, c, :], k.oT[c * 128:(c + 1) * 128, tsl], r=[k.R("oT", c)], w=[boTb])
        for oc in range(16):
            g_t, bg = gt[it % 2]
            it += 1
            for br in range(3):
                row = br * 2048 + oc * 128
                k.DMA("act", g_t[:, br, :], k.gT[row:row + 128, tsl],
                      r=[k.R("gT", row // 128, s_) for s_ in range(nsb)], w=[bg])
            pss = []
            for bi, (wap, c0, nk) in enumerate(wabc):
                pt, bpt = k.ps[bi + 3 * (oc % 2)]
                for kc in range(c0, c0 + nk):
                    k.MM(pt[:, 0:BW], WA[:, kc, oc * 128:(oc + 1) * 128], oTb[:, kc, :], start=(kc == c0),
                         stop=(kc == c0 + nk - 1), r=[bWA, boTb], w=[bpt])
                pss.append((pt, bpt))
            k.TT("dve", t1[:], pss[0][0][:, 0:BW], g_t[:, 0, :], ALU.mult, r=[pss[0][1], bg], w=[bt1])
            k.TT("dve", t2[:], pss[1][0][:, 0:BW], g_t[:, 1, :], ALU.mult, r=[pss[1][1], bg], w=[bt2])
            k.TT("dve", t1[:], t1[:], t2[:], ALU.add, r=[bt1, bt2], w=[bt1])
            k.TT("dve", t2[:], pss[2][0][:, 0:BW], g_t[:, 2, :], ALU.mult, r=[pss[2][1], bg], w=[bt2])
            k.TT("dve", mT[:, oc, :], t1[:], t2[:], ALU.add, r=[bt1, bt2], w=[bmT])
        iq = 0
        for cb in range(4):
            for qt in range(nqt):
                x_t, bx = xt[iq % 2]
                pt, bpt = k.ps[6 + iq % 2]
                iq += 1
                row0 = tb * BW + qt * 128
                k.DMA("act", x_t[:], xsrc[row0:row0 + 128, cb * 512:(cb + 1) * 512], r=[k.R(xtag, row0 // 128)], w=[bx])
                for kc in range(16):
                    k.MM(pt[:, 0:512], mT[:, kc, qt * 128:(qt + 1) * 128], WO[:, kc, cb * 512:(cb + 1) * 512],
                         start=(kc == 0), stop=(kc == 15), r=[bmT, bWO], w=[bpt])
                k.TT("dve", x_t[:], pt[:, 0:512], x_t[:], ALU.add, r=[bpt, bx], w=[bx])
                k.DMA("act", k.hbuf[row0:row0 + 128, cb * 512:(cb + 1) * 512], x_t[:], r=[bx], w=[k.R("h", row0 // 128, cb)])


def st_moe(k, l, xdst, xdtag):
    T, NT, CAP = k.T, k.NT, k.CAP
    SR = min(128, CAP)
    nst = (CAP + 127) // 128
    NSLOT = NE * CAP
    BIG = float(NSLOT + 1000)
    ident = k.cst[:, C_IDENT:C_IDENT + 128]

    def bndreg(eh):
        if getattr(k, "_bnd", None) is None:
            k._bnd = eh.alloc_register("bnd")
            eh.reg_mov(k._bnd, NSLOT - 1)
        return k._bnd
    k.stage_reset()
    mT, bmT = k.sb("mskT", [128, NT, 16], F32)
    wT, bwT = k.sb("wT", [128, NT, 16], F32)
    idxf, bxf = k.sb("idxf", [128, NT, 16], F32)
    idxi, bxi = k.sb("idxi", [128, NT, 16], I32)
    basee, bbe = k.sb("basee", [128, 16], F32)
    sm, bsm = k.sb("bis", [16, 8], F32)
    persist2 = k.sb_ptr
    lgT, blg = k.sb("lgT", [16, T], F32)
    persist = k.sb_ptr
    gbc, bg = k.sb("gbc2", [128, D], F32)
    k.DMA("sp", gbc[:], k.w["norm_ffn"][l].partition_broadcast(128), w=[bg])
    wr, bwr = k.sb("wr", [128, 16, NE], F32)
    k.DMA("sp", wr[:], k.w["w_router"][l].rearrange("(kc p) e -> p kc e", p=128), w=[bwr])
    xt = [k.sb("hx%d" % i, [128, D], F32) for i in range(2)]
    hnf = [k.sb("hnf%d" % i, [128, D], F32) for i in range(2)]
    hnb = [k.sb("hnb%d" % i, [128, D], BF16) for i in range(2)]
    hT = [k.sb("hT%d" % i, [128, 16, 128], F32) for i in range(2)]
    junk, bj = k.sb("mjunk", [128, D], BF16)
    ssq = [k.sb("mssq%d" % i, [128, 1], F32) for i in range(2)]
    rs = [k.sb("mrs%d" % i, [128, 1], F32) for i in range(2)]
    for tt in range(NT):
        x_t, bx = xt[tt % 2]
        f_t, bf = hnf[tt % 2]
        b_t, bb = hnb[tt % 2]
        h_t, bh = hT[tt % 2]
        sq, bsq = ssq[tt % 2]
        r_, br = rs[tt % 2]
        row0 = tt * 128
        k.DMA("sp", x_t[:], k.hbuf[row0:row0 + 128, :], r=[k.R("h", tt, cb) for cb in range(4)], w=[bx])
        k.ACT(junk[:], x_t[:], AF.Square, accum=sq[:], r=[bx], w=[bj, bsq])
        rstd_from_ssq(k, r_[:], sq[:], D, br, bsq)
        k.STT("dve", f_t[:], x_t[:], r_[:, 0:1], gbc[:], ALU.mult, ALU.mult, r=[bx, br, bg], w=[bf])
        k.CP("act", b_t[:], f_t[:], r=[bf], w=[bb])
        k.DMA("act", k.hn[row0:row0 + 128, :], b_t[:], r=[bb], w=[k.R("hn", tt)])
        for g4 in range(4):
            pt, bpt = k.ps[g4]
            for c in range(4):
                kc = g4 * 4 + c
                k.TR(pt[:, c * 128:(c + 1) * 128], f_t[:, kc * 128:(kc + 1) * 128], ident, r=[bf, k.bcst], w=[bpt])
            k.CP("act" if g4 % 2 else "dve", h_t[:, g4 * 4:(g4 + 1) * 4, :],
                 pt[:, 0:512].rearrange("p (c t) -> p c t", c=4), r=[bpt], w=[bh])
        pl, bpl = k.ps[4 + tt % 2]
        for kc in range(16):
            k.MM(pl[0:16, 0:128], wr[:, kc, :], h_t[:, kc, :], start=(kc == 0), stop=(kc == 15), r=[bwr, bh], w=[bpl])
        k.CP("act", lgT[:, row0:row0 + 128], pl[0:16, 0:128], r=[bpl], w=[blg])
    k.S.barrier()
    k.sb_ptr = persist
    aff, baf = k.sb("aff", [16, T], F32)
    tmpA, btA = k.sb("tmpA", [16, T], F32)
    k.ACT(aff[:], lgT[:], AF.Exp, r=[blg], w=[baf])
    BW = min(512, T)
    for blk in range(T // BW):
        sl = slice(blk * BW, (blk + 1) * BW)
        pt, bpt = k.ps[blk % 2]
        k.MM(pt[0:16, 0:BW], k.cst[0:16, C_ONES:C_ONES + 16], aff[:, sl], r=[k.bcst, baf], w=[bpt])
        k.RCP(tmpA[:, sl], pt[0:16, 0:BW], r=[bpt], w=[btA])
    k.TT("dve", aff[:], aff[:], tmpA[:], ALU.mult, r=[baf, btA], w=[baf])
    lo, hi, mid, cnt, ge, d1 = [sm[:, i:i + 1] for i in range(6)]
    k.MSET("dve", sm[:], 0.0, w=[bsm])
    k.MSET("dve", hi, 1.0, w=[bsm])
    for itn in range(30):
        k.TT("dve", mid, lo, hi, ALU.add, r=[bsm], w=[bsm])
        k.TS("dve", mid, mid, 0.5, None, ALU.mult, r=[bsm], w=[bsm])
        k.TS("dve", tmpA[:], aff[:], mid, 0.0, ALU.is_ge, ALU.add, accum=cnt, r=[baf, bsm], w=[btA, bsm])
        k.TS("dve", ge, cnt, float(CAP), None, ALU.is_ge, r=[bsm], w=[bsm])
        k.TT("dve", d1, mid, lo, ALU.subtract, r=[bsm], w=[bsm])
        k.STT("dve", lo, d1, ge, lo, ALU.mult, ALU.add, r=[bsm], w=[bsm])
        k.TT("dve", d1, hi, mid, ALU.subtract, r=[bsm], w=[bsm])
        k.STT("dve", hi, d1, ge, mid, ALU.mult, ALU.add, r=[bsm], w=[bsm])
    msk, bmk = k.sb("msk", [16, T], F32)
    k.TS("dve", msk[:], aff[:], lo, None, ALU.is_ge, r=[baf, bsm], w=[bmk])
    k.TT("dve", aff[:], aff[:], msk[:], ALU.mult, r=[baf, bmk], w=[baf])
    k.TS("dve", basee[:], k.cst[:, C_IOTA:C_IOTA + 16], float(CAP), None, ALU.mult, r=[k.bcst], w=[bbe])
    for tt in range(NT):
        pt, bpt = k.ps[tt % 2]
        k.TR(pt[:, 0:16], msk[:, tt * 128:(tt + 1) * 128], k.cst[0:16, C_IDENT:C_IDENT + 16], r=[bmk, k.bcst], w=[bpt])
        k.TR(pt[:, 16:32], aff[:, tt * 128:(tt + 1) * 128], k.cst[0:16, C_IDENT:C_IDENT + 16], r=[baf, k.bcst], w=[bpt])
        k.CP("dve", mT[:, tt, :], pt[:, 0:16], r=[bpt], w=[bmT])
        k.CP("act", wT[:, tt, :], pt[:, 16:32], r=[bpt], w=[bwT])
    for tt in range(NT):
        pt, bpt = k.ps[2 + tt % 2]
        for t2 in range(tt):
            k.MM(pt[:, 0:16], k.cst[:, C_ONES:C_ONES + 128], mT[:, t2, :], start=(t2 == 0), stop=False,
                 r=[k.bcst, bmT], w=[bpt])
        k.MM(pt[:, 0:16], k.cst[:, C_SLT:C_SLT + 128], mT[:, tt, :], start=(tt == 0), stop=True, r=[k.bcst, bmT], w=[bpt])
        k.TT("dve", idxf[:, tt, :], pt[:, 0:16], basee[:], ALU.add, r=[bpt, bbe], w=[bxf])
    k.TS("dve", idxf[:], idxf[:], -BIG, None, ALU.add, r=[bxf], w=[bxf])
    k.TT("dve", idxf[:], idxf[:], mT[:], ALU.mult, r=[bxf, bmT], w=[bxf])
    k.TS("dve", idxf[:], idxf[:], BIG, None, ALU.add, r=[bxf], w=[bxf])
    k.CP("dve", idxi[:], idxf[:], r=[bxf], w=[bxi])
    if "dbg_idx" in k.dbg:
        di = k.dram("dbg_idx", [128, NT * 16], I32)
        k.DMA("sp", di[:, :], idxi[:].rearrange("p a b -> p (a b)"), r=[bxi], w=[k.R("dbgidx")])
        dw = k.dram("dbg_w", [128, NT * 16], F32)
        k.DMA("sp", dw[:, :], wT[:].rearrange("p a b -> p (a b)"), r=[bwT], w=[k.R("dbgw")])
    k.S.barrier()
    k.sb_ptr = persist2
    hb = [k.sb("dhb%d" % i, [128, D], BF16) for i in range(2)]
    bxs = k.R("xs")
    for tt in range(NT):
        h_t, bh = hb[tt % 2]
        k.DMA("sp", h_t[:], k.hn[tt * 128:(tt + 1) * 128, :], r=[k.R("hn", tt)], w=[bh])
        for e in range(NE):
            off = idxi[:, tt, e:e + 1]

            def f(eh, off=off, h_t=h_t):
                return eh.indirect_dma_start(out=k.xs[:, :], out_offset=bass.IndirectOffsetOnAxis(ap=off, axis=0),
                                             in_=h_t[:, :], in_offset=None, bounds_check=bndreg(eh), oob_is_err=False)
            k.S.dma("pool", f, [bh, bxi], [k.R("xsw", tt, e)])
    k.S.barrier()
    k.sb_ptr = persist2
    xr = [k.sb("xr%d" % i, [SR, D], BF16) for i in range(2)]
    xsT, bxT = k.sb("xsT", [128, 16, CAP], BF16)
    hidT, bhid = k.sb("hidT", [128, 8, CAP], BF16)
    wf = [k.sb("ewf%d" % i, [128, 16, 512], F32) for i in range(2)]
    wbf = [k.sb("ewb%d" % i, [128, 16, 512], BF16) for i in range(2)]
    wdf = [k.sb("ewdf%d" % i, [128, 8, 512], F32) for i in range(2)]
    wdb = [k.sb("ewdb%d" % i, [128, 8, 512], BF16) for i in range(2)]
    sg, bsg = k.sb("esg", [128, CAP], F32)
    yt = [k.sb("eyt%d" % i, [SR, 512], F32) for i in range(2)]
    iw = 0
    idw = 0
    iy = 0
    for e in range(NE):
        for stl in range(nst):
            x_r, bxr = xr[stl % 2]
            r0 = e * CAP + stl * SR
            k.DMA("sp", x_r[:], k.xs[r0:r0 + SR, :], r=[], w=[bxr])
            for g4 in range(4):
                pt, bpt = k.ps[g4]
                ptb = pt[:].bitcast(BF16)
                for c in range(4):
                    kc = g4 * 4 + c
                    k.TR(ptb[:, c * SR:(c + 1) * SR], x_r[:, kc * 128:(kc + 1) * 128], k.identb[0:SR, 0:SR],
                         r=[bxr, k.bidentb], w=[bpt])
                k.CP("act" if g4 % 2 else "dve", xsT[:, g4 * 4:(g4 + 1) * 4, stl * SR:(stl + 1) * SR],
                     ptb[:, 0:4 * SR].rearrange("p (c t) -> p c t", c=4), r=[bpt], w=[bxT])
        for fcg in range(2):
            grp = []
            for which, wn in ((0, "w_e_gate"), (1, "w_e_up")):
                wf_t, bwf = wf[which]
                wb_t, bwb = wbf[which]
                k.DMA("sp", wf_t[:], k.w[wn][l, e][:, fcg * 512:(fcg + 1) * 512].rearrange("(kc p) c -> p kc c", p=128), w=[bwf])
                for piece in range(4):
                    k.CP(("dve", "pool", "act", "dve")[(iw + piece) % 4], wb_t[:, :, piece * 128:(piece + 1) * 128],
                         wf_t[:, :, piece * 128:(piece + 1) * 128], r=[bwf], w=[bwb])
                iw += 1
                grp.append((wb_t, bwb))
            for f4 in range(4):
                fc = fcg * 4 + f4
                pg, bpg = k.ps[4 + (fc % 2) * 2]
                pu, bpu = k.ps[5 + (fc % 2) * 2]
                for (wb_t, bwb), pp, bpp in ((grp[0], pg, bpg), (grp[1], pu, bpu)):
                    for kc in range(16):
                        k.MM(pp[:, 0:CAP], wb_t[:, kc, f4 * 128:(f4 + 1) * 128], xsT[:, kc, :], start=(kc == 0), stop=(kc == 15),
                             r=[bwb, bxT], w=[bpp])
                k.ACT(sg[:], pg[:, 0:CAP], AF.Silu, r=[bpg], w=[bsg])
                k.TT("dve", hidT[:, fc, :], pu[:, 0:CAP], sg[:], ALU.mult, r=[bpu, bsg], w=[bhid])
        for cb in range(4):
            wd_f, bwdf = wdf[idw % 2]
            wd_b, bwdb = wdb[idw % 2]
            idw += 1
            k.DMA("sp", wd_f[:], k.w["w_e_down"][l, e][:, cb * 512:(cb + 1) * 512].rearrange("(fc p) c -> p fc c", p=128), w=[bwdf])
            k.CP("pool", wd_b[:, 0:4, :], wd_f[:, 0:4, :], r=[bwdf], w=[bwdb])
            k.CP("dve", wd_b[:, 4:8, :], wd_f[:, 4:8, :], r=[bwdf], w=[bwdb])
            for stl in range(nst):
                py, bpy = k.ps[iy % 4]
                y_t, by = yt[iy % 2]
                iy += 1
                for fc in range(8):
                    k.MM(py[0:SR, 0:512], hidT[:, fc, stl * SR:(stl + 1) * SR], wd_b[:, fc, :], start=(fc == 0), stop=(fc == 7),
                         r=[bhid, bwdb], w=[bpy])
                k.CP("act" if iy % 2 else "dve", y_t[:], py[0:SR, 0:512], r=[bpy], w=[by])
                r0 = e * CAP + stl * SR
                k.DMA("act", k.ys[r0:r0 + SR, cb * 512:(cb + 1) * 512], y_t[:], r=[by], w=[k.R("ysw", e, stl, cb)])
    k.S.barrier()
    k.sb_ptr = persist2
    acc = [k.sb("cacc%d" % i, [128, D], F32) for i in range(2)]
    gb = [k.sb("cgb%d" % i, [128, D], F32) for i in range(3)]
    for g_t, bgb in gb:
        k.MSET("pool", g_t[:], 0.0, w=[bgb])
    ig = 0
    for tt in range(NT):
        a_t, ba = acc[tt % 2]
        k.DMA("sp", a_t[:], k.hbuf[tt * 128:(tt + 1) * 128, :], r=[], w=[ba])
        for e in range(NE):
            g_t, bgb = gb[ig % 3]
            ig += 1
            off = idxi[:, tt, e:e + 1]

            def f(eh, off=off, g_t=g_t):
                return eh.indirect_dma_start(out=g_t[:, :], out_offset=None, in_=k.ys[:, :],
                                             in_offset=bass.IndirectOffsetOnAxis(ap=off, axis=0),
                                             bounds_check=bndreg(eh), oob_is_err=False)
            k.S.dma("pool", f, [bxi], [bgb])
            k.STT("dve", a_t[:], g_t[:], wT[:, tt, e:e + 1], a_t[:], ALU.mult, ALU.add, r=[bgb, bwT, ba], w=[ba])
        k.DMA("act", xdst[tt * 128:(tt + 1) * 128, :], a_t[:], r=[ba], w=[k.R(xdtag, tt)])


def st_delta(k, l):
    T, NT = k.T, k.NT
    k.stage_reset()
    nsb = max(1, T // 2048)
    ident = k.cst[:, C_IDENT:C_IDENT + 128]
    ones = k.cst[:, C_ONES:C_ONES + 128]
    bc = k.bcst
    rr = [0]

    def PR():
        c = rr[0]
        rr[0] += 1
        b, q = c % 8, (c // 8) % 4
        return k.ps[b][0][:, q * 128:(q + 1) * 128], k.ps[b][1]

    def PR2():
        c = rr[0]
        rr[0] += 1
        b, q = c % 8, 2 * ((c // 8) % 2)
        return k.ps[b][0][:, q * 128:(q + 2) * 128], k.ps[b][1], k.ps[b][1]

    regs = [(k.ps[i % 8][0][:, (i // 8) * 128:(i // 8 + 1) * 128], k.ps[i % 8][1]) for i in range(32)]
    prm, bprm = k.sb("dprm", [128, 24], F32)
    for i, n in enumerate(("dt_bias_f", "dt_bias_b", "a_log_f", "a_log_b")):
        k.DMA("sp", prm[:, i * 6:(i + 1) * 6], k.w[n][l].partition_broadcast(128), w=[bprm])
    negA, bnA = k.sb("negA", [128, 12], F32)
    k.ACT(negA[:], prm[:, 12:24], AF.Exp, r=[bprm], w=[bnA])
    k.TS("dve", negA[:], negA[:], -1.0, None, ALU.mult, r=[bnA], w=[bnA])
    smA, bsmA = k.sb("smA", [128, NT, 24], F32)
    for tt in range(NT):
        k.DMA("sp", smA[:, tt, :], k.tm[tt * 128:(tt + 1) * 128, 768:792], r=[k.R("tm", tt)], w=[bsmA])
    beta, bbeta = k.sb("dbeta", [128, NT, 12], F32)
    gg, bgg = k.sb("dg", [128, NT, 12], F32)
    gc, bgc = k.sb("dgc", [128, NT, 12], F32)
    eg, beg = k.sb("deg", [128, NT, 12], F32)
    kd, bkd = k.sb("dkd", [128, NT, 12], F32)
    gend, bgend = k.sb("dgend", [128, NT, 24], F32)
    k.ACT(beta[:], smA[:, :, 0:12], AF.Sigmoid, r=[bsmA], w=[bbeta])
    for tt in range(NT):
        k.TT("dve", gg[:, tt, :], smA[:, tt, 12:24], prm[:, 0:12], ALU.add, r=[bsmA, bprm], w=[bgg])
    k.ACT(gg[:], gg[:], AF.Exp, r=[bgg], w=[bgg])
    k.ACT(gg[:], gg[:], AF.Ln, bias=1.0, r=[bgg], w=[bgg])
    for tt in range(NT):
        k.TT("dve", gg[:, tt, :], gg[:, tt, :], negA[:], ALU.mult, r=[bgg, bnA], w=[bgg])
    for tt in range(NT):
        p, bp = PR()
        k.MM(p[:, 0:6], k.cst[:, C_CUMF:C_CUMF + 128], gg[:, tt, 0:6], r=[bc, bgg], w=[bp])
        k.MM(p[:, 6:12], k.cst[:, C_CUMB:C_CUMB + 128], gg[:, tt, 6:12], r=[bc, bgg], w=[bp])
        k.CP("dve", gc[:, tt, :], p[:, 0:12], r=[bp], w=[bgc])
        p2, bp2 = PR()
        k.MM(p2[:, 0:6], k.cst[:, C_LASTF:C_LASTF + 128], gc[:, tt, 0:6], r=[bc, bgc], w=[bp2])
        k.MM(p2[:, 6:12], k.cst[:, C_LASTB:C_LASTB + 128], gc[:, tt, 6:12], r=[bc, bgc], w=[bp2])
        k.TT("dve", kd[:, tt, :], p2[:, 0:12], gc[:, tt, :], ALU.subtract, r=[bp2, bgc], w=[bkd])
        p3, bp3 = PR()
        for ci, (cf, cb_) in enumerate(((C_SELFA, C_SELBA), (C_SELFB, C_SELBB))):
            k.MM(p3[:, ci * 12:ci * 12 + 6], k.cst[:, cf:cf + 128], gc[:, tt, 0:6], r=[bc, bgc], w=[bp3])
            k.MM(p3[:, ci * 12 + 6:ci * 12 + 12], k.cst[:, cb_:cb_ + 128], gc[:, tt, 6:12], r=[bc, bgc], w=[bp3])
        k.CP("dve", gend[:, tt, :], p3[:, 0:24], r=[bp3], w=[bgend])
    k.ACT(eg[:], gc[:], AF.Exp, r=[bgc], w=[beg])
    k.ACT(kd[:], kd[:], AF.Exp, r=[bkd], w=[bkd])
    k.ACT(gend[:], gend[:], AF.Exp, r=[bgend], w=[bgend])
    ogb, bogb = k.sb("ogb", [128, 128], F32)
    k.DMA("sp", ogb[:], k.w["o_norm"][l].partition_broadcast(128), w=[bogb])
    cwc, bcwc = k.sb("cwc", [128, 3, 18], F32)
    for tap in range(3):
        k.DMA("sp", cwc[:, tap, :], k.w["conv_c"][l][tap].rearrange("(c p) -> p c", p=128), w=[bcwc], slow=True)
    qT, bqT = k.sb("dqT", [128, T], F32)
    kT, bkT = k.sb("dkT", [128, T], F32)
    Kt, bKt = k.sb("dKt", [128, NT, 128], F32)
    Vt, bVt = k.sb("dVt", [128, NT, 128], F32)
    of_, bof = k.sb("dof", [128, NT, 128], F32)
    ob_, bob = k.sb("dob", [128, NT, 128], F32)
    oCT, boCT = k.sb("oCT", [128, T], BF16)
    raw = of_[:].rearrange("p a b -> p (a b)")
    acc = ob_[:].rearrange("p a b -> p (a b)")
    BW = min(512, T)
    tmpb = [k.sb("dtb%d" % i, [128, BW], F32) for i in range(2)]

    BUFS = [[None, None], [None, None]]
    for d_ in range(2):
        for par_ in range(2):
            B = {}
            for nm in ("DG", "DEC", "LM", "LT", "ATT", "ATTT", "PA", "PAT", "PB", "PBT", "WT", "KD"):
                B[nm] = k.sb("%s%d%d" % (nm, d_, par_), [128, 128], F32)
            for nm in ("RHS", "XX"):
                B[nm] = k.sb("%s%d%d" % (nm, d_, par_), [128, 256], F32)
            BUFS[d_][par_] = B
    VN = [k.sb("VN%d" % d_, [128, 128], F32) for d_ in range(2)]
    O1 = [k.sb("O1%d" % d_, [128, 128], F32) for d_ in range(2)]
    SS = [[k.sb("S%d_%d" % (d, i), [128, 128], F32) for i in range(2)] for d in range(2)]
    gout, bgout = k.sb("gout", [128, 128], F32)
    osum, bosum = k.sb("osum", [128, 128], F32)
    onb, bonb = k.sb("onb", [128, 128], BF16)
    fssq, bfssq = k.sb("fssq", [128, 1], F32)
    frs, bfrs = k.sb("frs", [128, 1], F32)
    fj, bfj = k.sb("fj", [128, 128], F32)
    masks = ((C_NEGF, C_STRF), (C_NEGB, C_STRB))

    for h in range(6):
        for which, off, dst, bdst in ((0, O_QC, qT, bqT), (1, O_KC, kT, bkT), (2, O_VC, None, None)):
            ch = which * 6 + h
            rdl = [k.R("pT", off // 128 + h, s) for s in range(nsb)]
            k.DMA("sp", raw, k.pT[off + h * 128:off + (h + 1) * 128, :], r=rdl, w=[bof])
            k.ACT(acc, raw, AF.Copy, scale=cwc[:, 1, ch:ch + 1], r=[bof, bcwc], w=[bob])
            k.STT("dve", acc[:, 1:T], raw[:, 0:T - 1], cwc[:, 0, ch:ch + 1], acc[:, 1:T], ALU.mult, ALU.add,
                  r=[bof, bcwc, bob], w=[bob])
            k.STT("dve", acc[:, 0:T - 1], raw[:, 1:T], cwc[:, 2, ch:ch + 1], acc[:, 0:T - 1], ALU.mult, ALU.add,
                  r=[bof, bcwc, bob], w=[bob])
            k.ACT(acc, acc, AF.Silu, r=[bob], w=[bob])
            if which < 2:
                for blk in range(T // BW):
                    sl = slice(blk * BW, (blk + 1) * BW)
                    (t0, bt0), (t1, bt1) = tmpb
                    k.ACT(t0[:], acc[:, sl], AF.Square, r=[bob], w=[bt0])
                    pb4 = k.ps[blk % 2]
                    k.MM(pb4[0][:, 0:BW], ones, t0[:], r=[bc, bt0], w=[pb4[1]])
                    k.ACT(t1[:], pb4[0][:, 0:BW], AF.Sqrt, bias=k.epsc[:, 0:1], r=[pb4[1], k.bepsc], w=[bt1])
                    k.RCP(t1[:], t1[:], r=[bt1], w=[bt1])
                    if which == 0:
                        k.STT("dve", dst[:, sl], acc[:, sl], 128.0 ** -0.5, t1[:], ALU.mult, ALU.mult, r=[bob, bt1], w=[bdst])
                    else:
                        k.TT("dve", dst[:, sl], acc[:, sl], t1[:], ALU.mult, r=[bob, bt1], w=[bdst])
            else:
                for tt in range(NT):
                    p, bp = regs[8 + tt % 8]
                    k.TR(p, acc[:, tt * 128:(tt + 1) * 128], ident, r=[bob, bc], w=[bp])
                    k.CP("act" if tt % 2 else "dve", Vt[:, tt, :], p, r=[bp], w=[bVt])
        for tt in range(NT):
            p, bp = regs[8 + tt % 8]
            k.TR(p, kT[:, tt * 128:(tt + 1) * 128], ident, r=[bkT, bc], w=[bp])
            k.CP("act" if tt % 2 else "dve", Kt[:, tt, :], p, r=[bp], w=[bKt])
        k.S.barrier()
        rr[0] = 0
        for d in range(2):
            k.MSET("dve", SS[d][0][0][:], 0.0, w=[SS[d][0][1]])
        scur = [0, 0]

        def intra(d, it):
            par = it % 2
            tt = it if d == 0 else NT - 1 - it
            col = d * 6 + h
            tsl = slice(tt * 128, (tt + 1) * 128)
            cneg, cstr = masks[d]
            bsc = beta[:, tt, col:col + 1]
            gsc = gc[:, tt, col:col + 1]
            esc = eg[:, tt, col:col + 1]
            B = BUFS[d][par]
            (dg, bdg), (dec, bdec), (lm, blm), (lt, blt) = B["DG"], B["DEC"], B["LM"], B["LT"]
            (att, batt), (attT, battT), (rhs, brhs), (xx, bxx) = B["ATT"], B["ATTT"], B["RHS"], B["XX"]
            (wt, bwt), (kdt, bkdt) = B["WT"], B["KD"]
            pkk, bpkk = PR()
            k.MM(pkk, kT[:, tsl], kT[:, tsl], r=[bkT], w=[bpkk])
            k.ACT(dg[:], ident, AF.Copy, scale=gsc, r=[bc, bgc], w=[bdg])
            yield
            pg, bpg = PR()
            k.MM(pg, ones, dg[:], r=[bc, bdg], w=[bpg])
            k.STT("dve", dec[:], pg, -1.0, k.cst[:, cneg:cneg + 128], ALU.mult, ALU.add, r=[bpg, bc], w=[bdec])
            yield
            k.ACT(dec[:], dec[:], AF.Exp, bias=gsc, r=[bdec, bgc], w=[bdec])
            k.ACT(rhs[:, 0:128], Vt[:, tt, :], AF.Copy, scale=bsc, r=[bVt, bbeta], w=[brhs])
            k.TS("dve", rhs[:, 128:256], Kt[:, tt, :], bsc, esc, ALU.mult, ALU.mult, r=[bKt, bbeta, beg], w=[brhs])
            yield
            k.STT("dve", lm[:], pkk, bsc, dec[:], ALU.mult, ALU.mult, r=[bpkk, bbeta, bdec], w=[blm])
            k.TT("dve", lm[:], lm[:], k.cst[:, cstr:cstr + 128], ALU.mult, r=[blm, bc], w=[blm])
            yield
            p1, bp1 = PR()
            k.TR(p1, lm[:], ident, r=[blm, bc], w=[bp1])
            k.CP("act", lt[:], p1, r=[bp1], w=[blt])
            yield
            pqk, bpqk = PR()
            k.MM(pqk, qT[:, tsl], kT[:, tsl], r=[bqT, bkT], w=[bpqk])
            k.TT("dve", att[:], pqk, dec[:], ALU.mult, r=[bpqk, bdec], w=[batt])
            k.ACT(kdt[:], Kt[:, tt, :], AF.Copy, scale=kd[:, tt, col:col + 1], r=[bKt, bkd], w=[bkdt])
            yield
            px, bpxa, bpxb = PR2()
            k.MM(px, lt[:], rhs[:], r=[blt, brhs], w=[bpxa, bpxb])
            k.TT("dve", xx[:], rhs[:], px, ALU.subtract, r=[brhs, bpxa, bpxb], w=[bxx])
            yield
            p2, bp2 = PR()
            k.TR(p2, att[:], ident, r=[batt, bc], w=[bp2])
            k.CP("act", attT[:], p2, r=[bp2], w=[battT])
            yield
            P, bP = lm, blm
            PT_, bPT = lt, blt
            nxt = [(B["PA"], B["PAT"]), (B["PB"], B["PBT"])]
            for lvl in range(5):
                (np_, bnp), (npt, bnpt) = nxt[lvl % 2]
                pt2, bpt2 = PR()
                k.MM(pt2, P[:], PT_[:], r=[bP, bPT], w=[bpt2])
                k.CP("act", npt[:], pt2, r=[bpt2], w=[bnpt])
                if lvl < 4:
                    pp2, bpp2 = PR()
                    k.MM(pp2, PT_[:], P[:], r=[bP, bPT], w=[bpp2])
                    k.CP("pool" if False else "dve", np_[:], pp2, r=[bpp2], w=[bnp])
                yield
                px, bpxa, bpxb = PR2()
                k.MM(px, npt[:], xx[:], r=[bnpt, bxx], w=[bpxa, bpxb])
                k.TT("dve", xx[:], xx[:], px, ALU.add, r=[bxx, bpxa, bpxb], w=[bxx])
                P, bP, PT_, bPT = np_, bnp, npt, bnpt
                yield
            p3, bp3 = PR()
            k.TR(p3, xx[:, 128:256], ident, r=[bxx, bc], w=[bp3])
            k.CP("act", wt[:], p3, r=[bp3], w=[bwt])
            yield

        def rec(d, it):
            par = it % 2
            tt = it if d == 0 else NT - 1 - it
            col = d * 6 + h
            tsl = slice(tt * 128, (tt + 1) * 128)
            B = BUFS[d][par]
            (attT, battT), (xx, bxx), (wt, bwt), (kdt, bkdt) = B["ATTT"], B["XX"], B["WT"], B["KD"]
            (vn, bvn), (o1, bo1) = VN[d], O1[d]
            odst, bodst = (of_, bof) if d == 0 else (ob_, bob)
            for step in range(2):
                ci = step if d == 0 else 1 - step
                rows = slice(ci * 64, (ci + 1) * 64)
                S_, bS = SS[d][scur[d]]
                Sn, bSn = SS[d][1 - scur[d]]
                scur[d] = 1 - scur[d]
                pv, bpv = PR()
                k.MM(pv, wt[:], S_[:], r=[bwt, bS], w=[bpv])
                po1, bpo1 = PR()
                k.MM(po1, qT[:, tsl], S_[:], r=[bqT, bS], w=[bpo1])
                k.TT("dve", vn[rows, :], xx[rows, 0:128], pv[rows, :], ALU.subtract, r=[bxx, bpv], w=[bvn])
                k.ACT(o1[rows, :], po1[rows, :], AF.Copy, scale=eg[rows, tt, col:col + 1], r=[bpo1, beg], w=[bo1])
                yield
                pS, bpS = PR()
                k.MM(pS, kdt[rows, :], vn[rows, :], r=[bkdt, bvn], w=[bpS])
                gcol = ci * 12 + col
                k.STT("dve", Sn[:], S_[:], gend[:, tt, gcol:gcol + 1], pS, ALU.mult, ALU.add, r=[bS, bgend, bpS], w=[bSn])
                po2, bpo2 = PR()
                k.MM(po2, attT[rows, :], vn[rows, :], r=[battT, bvn], w=[bpo2])
                k.TT("pool" if False else "dve", odst[rows, tt, :], o1[rows, :], po2[rows, :], ALU.add, r=[bo1, bpo2], w=[bodst])
                yield

        def run_rr(gens):
            gens = list(gens)
            while gens:
                for g in list(gens):
                    try:
                        next(g)
                    except StopIteration:
                        gens.remove(g)

        pe_warm(k, 24, bank=7)
        run_rr([intra(0, 0), intra(1, 0)])
        for it in range(NT):
            if it % 8 == 7:
                pe_warm(k, 16, bank=7)
            gl = [rec(0, it), rec(1, it)]
            if it + 1 < NT:
                gl += [intra(0, it + 1), intra(1, it + 1)]
            run_rr(gl)
        for tt in range(NT):
            k.DMA("sp", gout[:], k.tm[tt * 128:(tt + 1) * 128, h * 128:(h + 1) * 128], r=[k.R("tm", tt)], w=[bgout])
            k.ACT(gout[:], gout[:], AF.Silu, r=[bgout], w=[bgout])
            k.TT("dve", osum[:], of_[:, tt, :], ob_[:, tt, :], ALU.add, r=[bof, bob], w=[bosum])
            k.ACT(fj[:], osum[:], AF.Square, accum=fssq[:], r=[bosum], w=[bfj, bfssq])
            rstd_from_ssq(k, frs[:], fssq[:], 128, bfrs, bfssq)
            k.STT("dve", osum[:], osum[:], frs[:, 0:1], ogb[:], ALU.mult, ALU.mult, r=[bosum, bfrs, bogb], w=[bosum])
            k.TT("dve", onb[:], osum[:], gout[:], ALU.mult, r=[bosum, bgout], w=[bonb])
            pz, bpz = k.ps[tt % 2]
            pzb = pz[:].bitcast(BF16)
            k.TR(pzb[:, 0:128], onb[:], k.identb[:], r=[bonb, k.bidentb], w=[bpz])
            k.CP("act", oCT[:, tt * 128:(tt + 1) * 128], pzb[:, 0:128], r=[bpz], w=[boCT])
        k.DMA("act", k.oT[1280 + h * 128:1280 + (h + 1) * 128, :], oCT[:], r=[boCT], w=[k.R("oT", 10 + h)])
        k.S.barrier()


def build(T, L, dbg=(), stages=None):
    k = KB(T, L, dbg, stages)
    nc = k.nc
    k.pT = k.dram("pT", [6144, T], F32)
    k.tm = k.dram("tm", [T, NTM], F32)
    k.gT = k.dram("gT", [6144, T], BF16)
    k.oT = k.dram("oT", [D, T], BF16)
    k.hbuf = k.dram("hbuf", [T, D], F32)
    k.xbuf = k.dram("xbuf", [T, D], F32)
    k.rotC = k.dram("rotC", [128, T], F32)
    k.hn = k.dram("hn", [T, D], BF16)
    k.xs = k.dram("xs", [NE * (2 * T // NE), D], BF16)
    k.ys = k.dram("ys", [NE * (2 * T // NE), D], F32)
    k.rotS = k.dram("rotS", [128, T], F32)
    st_setup(k)
    epsc, bepsc = k.sb("epsc", [128, 1], F32)
    k.MSET("dve", epsc[:], EPS, w=[bepsc])
    k.epsc, k.bepsc = epsc, bepsc
    k.sb_base = k.sb_ptr
    for l in range(L):
        xsrc = k.x_in if l == 0 else k.xbuf
        xdst = k.y_out if l == L - 1 else k.xbuf
        if k.on("proj"):
            st_proj(k, l, xsrc, "xres")
        if k.on("mixA"):
            st_mixA(k, l)
        if k.on("rot") and l == 0:
            st_rot(k)
        if k.on("attn"):
            st_attn(k, l)
        if k.on("delta"):
            st_delta(k, l)
        if k.stages is not None and "zeroC" in k.stages:
            k.stage_reset()
            zt, bz = k.sb("zt", [128, T], BF16)
            k.MSET("dve", zt[:], 0.0, w=[bz])
            for c in range(10, 16):
                k.DMA("sp", k.oT[c * 128:(c + 1) * 128, :], zt[:], r=[bz], w=[k.R("oT", c)])
        if k.on("merge"):
            st_merge(k, l, xsrc, "xres")
        if k.stages is not None and "copyh" in k.stages:
            k.stage_reset()
            ct, bct = k.sb("ct", [128, D], F32)
            for tt in range(T // 128):
                k.DMA("sp", ct[:], k.x_in[tt * 128:(tt + 1) * 128, :], w=[bct])
                for cb in range(4):
                    k.DMA("sp", k.hbuf[tt * 128:(tt + 1) * 128, cb * 512:(cb + 1) * 512], ct[:, cb * 512:(cb + 1) * 512],
                          r=[bct], w=[k.R("h", tt, cb)])
        if k.on("moe"):
            st_moe(k, l, xdst, "xres")
    k.S.finish()
    return k


T_FULL = 4096
L_FULL = 2
N_CORES = 4
_CACHE = {}


def kernel(**inputs):
    x = np.ascontiguousarray(inputs["x"], dtype=np.float32)
    pos = np.ascontiguousarray(inputs["positions"]).astype(np.int32)
    B = x.shape[0]
    if "k" not in _CACHE:
        _CACHE["k"] = build(T_FULL, L_FULL)
    k = _CACHE["k"]
    cst = make_consts()
    in_maps = []
    for c in range(N_CORES):
        b = c % B
        m = {"x": x[b], "pos": pos[b:b + 1], "cst": cst}
        for n in k.w:
            m[n] = np.ascontiguousarray(inputs[n], dtype=np.float32)
        in_maps.append(m)
    res = run_bass_kernel_spmd(k.nc, in_maps, core_ids=list(range(N_CORES)))
    out = np.stack([res.results[b]["y"] for b in range(B)], axis=0)
    return out.astype(np.float32)
```

```python
import math
from contextlib import ExitStack
import numpy as np
import concourse.bass as bass
import concourse.mybir as mybir
from concourse.bass_utils import run_bass_kernel_spmd

F32 = mybir.dt.float32
BF16 = mybir.dt.bfloat16
I32 = mybir.dt.int32
AF = mybir.ActivationFunctionType
ALU = mybir.AluOpType
AX = mybir.AxisListType

D = 2048
NIN = 6936
NG = 6144
NE = 16
FF = 1024
EPS = 1e-6
NEG = -1.0e30
ENGS = ("pe", "act", "dve", "pool", "sp")
DQ = ("sp", "act", "pool")
NDSEM = 8


class Buf:
    __slots__ = ("name", "last_w", "readers")

    def __init__(self, name=""):
        self.name = name
        self.last_w = None
        self.readers = []


class Sched:
    def __init__(self, nc, stack):
        self.nc = nc
        self.q = {e: [] for e in ENGS}
        self.cnt = {e: 0 for e in ENGS}
        self.seen = {e: {} for e in ENGS}
        self.stack = stack
        self.epoch = {e: 0 for e in ENGS}
        self.esems = {(e, 0): stack.enter_context(nc.semaphore("s_" + e)) for e in ENGS}
        self.dsem = {e: [stack.enter_context(nc.semaphore("d_%s%d" % (e, i))) for i in range(NDSEM)]
                     for e in DQ}
        self.dcnt = {e: 0 for e in DQ}
        self.dlast = {e: [0] * NDSEM for e in DQ}
        self.nops = 0

    def _sem(self, key):
        return self.esems[(key[1], key[2])] if key[0] == "e" else self.dsem[key[1]][key[2]]

    def _ekey(self, e):
        return ("e", e, self.epoch[e])

    def _need(self, eng, tok, waits):
        if tok is None:
            return
        key, val = tok
        if key[0] == "e" and key[1] == "pe" and eng == "pe":
            return
        if self.seen[eng].get(key, 0) >= val:
            return
        if waits.get(key, 0) < val:
            waits[key] = val

    def _deps(self, eng, reads, writes):
        waits = {}
        for b in reads:
            self._need(eng, b.last_w, waits)
        for b in writes:
            self._need(eng, b.last_w, waits)
            for r in b.readers:
                self._need(eng, r, waits)
        return waits

    def _commit(self, tok, reads, writes):
        for b in reads:
            b.readers.append(tok)
            if len(b.readers) > 48:
                mx = {}
                for k, v in b.readers:
                    if mx.get(k, 0) < v:
                        mx[k] = v
                b.readers = list(mx.items())
        for b in writes:
            b.last_w = tok
            b.readers = []

    def op(self, eng, fn, reads=(), writes=()):
        waits = self._deps(eng, reads, writes)
        for key, val in waits.items():
            self.seen[eng][key] = val
        self.cnt[eng] += 1
        n = self.cnt[eng]
        sem = self.esems[(eng, self.epoch[eng])]
        wl = [(self._sem(k), v) for k, v in waits.items()]

        def emit(e):
            for s, v in wl:
                e.wait_ge(s, v)
            fn(e).then_inc(sem, 1)
        self.q[eng].append(emit)
        self.nops += 1
        tok = (self._ekey(eng), n)
        self._commit(tok, reads, writes)
        return tok

    def dma(self, eng, fn, reads=(), writes=()):
        waits = self._deps(eng, reads, writes)
        j = self.dcnt[eng]
        self.dcnt[eng] += 1
        i = j % NDSEM
        key = ("d", eng, i)
        prev = self.dlast[eng][i]
        if prev and self.seen[eng].get(key, 0) < prev:
            if waits.get(key, 0) < prev:
                waits[key] = prev
        for k, v in waits.items():
            self.seen[eng][k] = v
        val = prev + 16
        self.dlast[eng][i] = val
        sem = self.dsem[eng][i]
        wl = [(self._sem(k), v) for k, v in waits.items()]

        def emit(e):
            for s, v in wl:
                e.wait_ge(s, v)
            fn(e).then_inc(sem, 16)
        self.q[eng].append(emit)
        self.nops += 1
        tok = (key, val)
        self._commit(tok, reads, writes)
        return tok

    def barrier(self):
        for e in ENGS:
            waits = {}
            for e2 in ENGS:
                key = self._ekey(e2)
                if self.cnt[e2] > self.seen[e].get(key, 0):
                    waits[key] = self.cnt[e2]
            for q in DQ:
                for i in range(NDSEM):
                    key = ("d", q, i)
                    v = self.dlast[q][i]
                    if v > self.seen[e].get(key, 0):
                        waits[key] = v
            for k, v in waits.items():
                self.seen[e][k] = v
            wl = [(self._sem(k), v) for k, v in waits.items()]

            def emit(eh, wl=wl):
                for s, v in wl:
                    eh.wait_ge(s, v)
            self.q[e].append(emit)
        for e in ENGS:
            if self.cnt[e] > 16000:
                self.epoch[e] += 1
                self.cnt[e] = 0
                self.esems[(e, self.epoch[e])] = self.stack.enter_context(
                    self.nc.semaphore("s_%s_%d" % (e, self.epoch[e])))

    def finish(self):
        self.barrier()
        nc = self.nc
        with nc.Block() as block:
            @block.tensor
            def _(e):
                for f in self.q["pe"]:
                    f(e)

            @block.scalar
            def _(e):
                for f in self.q["act"]:
                    f(e)

            @block.vector
            def _(e):
                for f in self.q["dve"]:
                    f(e)

            @block.gpsimd
            def _(e):
                for f in self.q["pool"]:
                    f(e)

            @block.sync
            def _(e):
                for f in self.q["sp"]:
                    f(e)


C_IDENT, C_ONES, C_BD64, C_RMAT, C_CUMF, C_CUMB, C_NEGF, C_NEGB, C_STRF, C_STRB, \
    C_SELFA, C_SELFB, C_SELBA, C_SELBB, C_SLT = [i * 128 for i in range(15)]
C_LASTF = 15 * 128
C_LASTB = 16 * 128
C_INVF = 17 * 128
C_SGN = C_INVF + 1
C_IOTA = C_SGN + 1
NCST = C_IOTA + 128


def make_consts():
    c = np.zeros((128, NCST), np.float32)
    i = np.arange(128)[:, None]
    j = np.arange(128)[None, :]
    same = (i // 64) == (j // 64)
    c[:, C_IDENT:C_IDENT + 128] = (i == j)
    c[:, C_ONES:C_ONES + 128] = 1.0
    c[:, C_BD64:C_BD64 + 128] = same
    dd = np.arange(128) % 64
    r = np.zeros((128, 128), np.float32)
    for d in range(128):
        m = d % 64
        if m < 8:
            r[d + 8, d] = 1.0
        elif m < 16:
            r[d - 8, d] = 1.0
    c[:, C_RMAT:C_RMAT + 128] = r
    incl_f = same & (j <= i)
    incl_b = same & (j >= i)
    c[:, C_CUMF:C_CUMF + 128] = incl_f.T
    c[:, C_CUMB:C_CUMB + 128] = incl_b.T
    c[:, C_NEGF:C_NEGF + 128] = np.where(incl_f, 0.0, NEG)
    c[:, C_NEGB:C_NEGB + 128] = np.where(incl_b, 0.0, NEG)
    c[:, C_STRF:C_STRF + 128] = same & (j < i)
    c[:, C_STRB:C_STRB + 128] = same & (j > i)
    for off, row in ((C_SELFA, 63), (C_SELFB, 127), (C_SELBA, 0), (C_SELBB, 64)):
        c[row, off:off + 128] = 1.0
    c[:, C_LASTF:C_LASTF + 128] = (i == (j // 64) * 64 + 63)
    c[:, C_LASTB:C_LASTB + 128] = (i == (j // 64) * 64)
    c[:, C_SLT:C_SLT + 128] = (i < j)
    invf = np.where(dd < 16, 500000.0 ** (-((dd % 8).astype(np.float64)) / 8.0), 0.0)
    c[:, C_INVF] = invf
    c[:, C_SGN] = np.where(dd < 8, -1.0, np.where(dd < 16, 1.0, 0.0))
    c[:, C_IOTA:C_IOTA + 128] = np.arange(128)[None, :]
    return c


O_BA, O_CA, O_VA = 0, 512, 1024
O_QB, O_KB, O_VB = 1536, 2304, 3072
O_QC, O_KC, O_VC = 3840, 4608, 5376
O_TM = 6144
NTM = NIN - O_TM

W_NAMES = ["norm_mix", "w_in", "conv_a", "q_norm", "k_norm", "lambda_q1", "lambda_k1",
           "lambda_q2", "lambda_k2", "subln", "conv_c", "a_log_f", "a_log_b", "dt_bias_f",
           "dt_bias_b", "o_norm", "w_out_a", "w_out_b", "w_out_c", "w_gate", "b_gate", "w_o",
           "norm_ffn", "w_router", "w_e_gate", "w_e_up", "w_e_down"]
W_SHAPES = {
    "norm_mix": [D], "w_in": [D, NIN], "conv_a": [3, 512], "q_norm": [64], "k_norm": [64],
    "lambda_q1": [64], "lambda_k1": [64], "lambda_q2": [64], "lambda_k2": [64], "subln": [128],
    "conv_c": [3, 2304], "a_log_f": [6], "a_log_b": [6], "dt_bias_f": [6], "dt_bias_b": [6],
    "o_norm": [128], "w_out_a": [512, D], "w_out_b": [768, D], "w_out_c": [768, D],
    "w_gate": [D, NG], "b_gate": [NG], "w_o": [D, D], "norm_ffn": [D], "w_router": [D, NE],
    "w_e_gate": [NE, D, FF], "w_e_up": [NE, D, FF], "w_e_down": [NE, FF, D],
}


class LazyW(dict):
    def __init__(self, k):
        super().__init__()
        self.k = k

    def __missing__(self, n):
        v = self.k.nc.dram_tensor(n, [self.k.L] + W_SHAPES[n], F32, kind="ExternalInput").ap()
        self[n] = v
        return v


class KB:
    def __init__(self, T, L, dbg=(), stages=None):
        self.T, self.L = T, L
        self.NT = T // 128
        self.CAP = 2 * T // NE
        self.dbg = set(dbg)
        self.stages = stages
        self.nc = nc = bass.Bass("TRN2", target_bir_lowering=False)
        self.st = ExitStack()
        self.S = Sched(nc, self.st)
        self.sb_base = 16512
        self.sb_ptr = 16512
        self.nalloc = 0
        self.regs = {}
        self.x_in = nc.dram_tensor("x", [T, D], F32, kind="ExternalInput").ap()
        self.pos_in = nc.dram_tensor("pos", [1, T], I32, kind="ExternalInput").ap()
        self.cst_in = nc.dram_tensor("cst", [128, NCST], F32, kind="ExternalInput").ap()
        self.w = LazyW(self)
        self.y_out = nc.dram_tensor("y", [T, D], F32, kind="ExternalOutput").ap()
        self.ps = []
        for i in range(8):
            t = nc.alloc_psum_tensor("psb%d" % i, [128, 512], F32)
            self.ps.append((t, Buf("ps%d" % i)))

    def dram(self, name, shape, dtype):
        kind = "ExternalOutput" if name in self.dbg else "Internal"
        return self.nc.dram_tensor(name, shape, dtype, kind=kind).ap()

    def sb(self, name, shape, dtype, bufs=None):
        esz = 4 if dtype in (F32, I32) else 2
        n = 1
        for s in shape[1:]:
            n *= s
        nbytes = (n * esz + 31) // 32 * 32
        self.nalloc += 1
        t = self.nc.alloc_sbuf_tensor_at("%s_%d" % (name, self.nalloc), list(shape), dtype,
                                         offset=self.sb_ptr)
        self.sb_ptr += nbytes
        assert self.sb_ptr <= 229344, ("SBUF overflow", name, self.sb_ptr)
        return t, Buf(name)

    def stage_reset(self):
        self.S.barrier()
        self.sb_ptr = self.sb_base

    def R(self, *key):
        b = self.regs.get(key)
        if b is None:
            b = self.regs[key] = Buf(str(key))
        return b

    def MM(self, out, lhsT, rhs, start=True, stop=True, r=(), w=()):
        self.S.op("pe", lambda e: e.matmul(out, lhsT=lhsT, rhs=rhs, start=start, stop=stop), r, w)

    def TR(self, out, in_, ident, r=(), w=()):
        self.S.op("pe", lambda e: e.transpose(out, in_, ident), r, w)

    def ACT(self, out, in_, func, bias=0.0, scale=1.0, accum=None, r=(), w=()):
        if accum is None:
            self.S.op("act", lambda e: e.activation(out=out, in_=in_, func=func, bias=bias, scale=scale), r, w)
        else:
            self.S.op("act", lambda e: e.activation(out=out, in_=in_, func=func, bias=bias, scale=scale,
                                                    accum_out=accum), r, w)

    def _eng(self, name):
        return name

    def TT(self, eng, out, in0, in1, op, r=(), w=()):
        self.S.op(eng, lambda e: e.tensor_tensor(out=out, in0=in0, in1=in1, op=op), r, w)

    def TS(self, eng, out, in0, s1, s2, op0, op1=None, accum=None, r=(), w=()):
        def f(e):
            kw = {}
            if op1 is not None:
                kw["op1"] = op1
            if accum is not None:
                kw["accum_out"] = accum
            return e.tensor_scalar(out=out, in0=in0, scalar1=s1, scalar2=s2, op0=op0, **kw)
        self.S.op(eng, f, r, w)

    def STT(self, eng, out, in0, scalar, in1, op0, op1, r=(), w=()):
        self.S.op(eng, lambda e: e.scalar_tensor_tensor(out=out, in0=in0, scalar=scalar, in1=in1,
                                                        op0=op0, op1=op1), r, w)

    def CP(self, eng, out, in_, r=(), w=()):
        if eng == "act":
            self.S.op("act", lambda e: e.copy(out=out, in_=in_), r, w)
        else:
            self.S.op(eng, lambda e: e.tensor_copy(out=out, in_=in_), r, w)

    def RCP(self, out, in_, r=(), w=()):
        self.S.op("dve", lambda e: e.reciprocal(out=out, in_=in_), r, w)

    def MSET(self, eng, ap, val, r=(), w=()):
        self.S.op(eng, lambda e: e.memset(ap, val), r, w)

    def DMA(self, q, out, in_, r=(), w=(), slow=False):
        if slow:
            self.S.dma(q, lambda e: e.dma_start(out=out, in_=in_, allow_slow_non_contiguous=True), r, w)
        else:
            self.S.dma(q, lambda e: e.dma_start(out=out, in_=in_), r, w)

    def on(self, name):
        return self.stages is None or name in self.stages


def st_setup(k):
    cst, bc = k.sb("cst", [128, NCST], F32)
    k.cst, k.bcst = cst, bc
    k.DMA("sp", cst[:], k.cst_in[:, :], w=[bc])
    idb, bidb = k.sb("identb", [128, 128], BF16)
    k.CP("dve", idb[:], cst[:, C_IDENT:C_IDENT + 128], r=[bc], w=[bidb])
    k.identb, k.bidentb = idb, bidb
    jb, bjb = k.sb("junkb", [128, 512], BF16)
    k.MSET("dve", jb[:], 0.0, w=[bjb])
    k.junkb, k.bjunkb = jb, bjb
    k.sb_base = k.sb_ptr


def pe_warm(k, n=16, bank=7):
    pt, bpt = k.ps[bank]
    for i in range(n):
        k.MM(pt[:, 0:512], k.identb[:], k.junkb[:], start=True, stop=True, r=[k.bidentb, k.bjunkb], w=[bpt])


def cs(k, off, n=128, p0=0, p1=128):
    return k.cst[p0:p1, off:off + n]


def rstd_from_ssq(k, rstd, ssq, n, brstd, bssq):
    k.ACT(rstd, ssq, AF.Sqrt, bias=k.epsc[0:rstd.shape[0], 0:1], scale=1.0 / n, r=[bssq, k.bepsc], w=[brstd])
    k.RCP(rstd, rstd, r=[brstd], w=[brstd])


def st_norm_T(k, src, gain_ap, dstT, bdst, t0, nt, tag):
    gbc, bg = k.sb("gbc", [128, D], F32)
    k.DMA("sp", gbc[:], gain_ap.partition_broadcast(128), w=[bg])
    xt = [k.sb("xt%d" % i, [128, D], F32) for i in range(2)]
    xs = [k.sb("xs%d" % i, [128, D], BF16) for i in range(2)]
    junk, bj = k.sb("junk", [128, D], BF16)
    ssq = [k.sb("ssq%d" % i, [128, 1], F32) for i in range(2)]
    rs = [k.sb("rs%d" % i, [128, 1], F32) for i in range(2)]
    for tt in range(nt):
        x_t, bx = xt[tt % 2]
        xs_t, bxs = xs[tt % 2]
        sq, bsq = ssq[tt % 2]
        r_, br = rs[tt % 2]
        row0 = (t0 + tt) * 128
        k.DMA("sp", x_t[:], src[row0:row0 + 128, :], r=[k.R(tag, t0 + tt)], w=[bx])
        k.ACT(junk[:], x_t[:], AF.Square, accum=sq[:], r=[bx], w=[bj, bsq])
        rstd_from_ssq(k, r_[:], sq[:], D, br, bsq)
        k.STT("dve", xs_t[:], x_t[:], r_[:, 0:1], gbc[:], ALU.mult, ALU.mult, r=[bx, br, bg], w=[bxs])
        for g4 in range(4):
            pt, bpt = k.ps[(tt * 4 + g4) % 4]
            ptb = pt[:].bitcast(BF16)
            for c in range(4):
                kc = g4 * 4 + c
                k.TR(ptb[:, c * 128:(c + 1) * 128], xs_t[:, kc * 128:(kc + 1) * 128], k.identb[:],
                     r=[bxs, k.bidentb], w=[bpt])
            eng = "act" if g4 % 2 == 0 else "dve"
            k.CP(eng, dstT[:, g4 * 4:(g4 + 1) * 4, tt * 128:(tt + 1) * 128],
                 ptb[:, 0:512].rearrange("p (c t) -> p c t", c=4), r=[bpt], w=[bdst])


def st_proj(k, l, xsrc, xtag):
    T = k.T
    TB = min(T, 2048)
    w_in, w_gate, b_gate = k.w["w_in"][l], k.w["w_gate"][l], k.w["b_gate"][l]
    for sbi in range(T // TB):
        k.stage_reset()
        xnT, bxn = k.sb("xnT", [128, 16, TB], BF16)
        mark = k.sb_ptr
        st_norm_T(k, xsrc, k.w["norm_mix"][l], xnT, bxn, sbi * TB // 128, TB // 128, xtag)
        k.S.barrier()
        k.sb_ptr = mark
        nblk = TB // 512 if TB >= 512 else 1
        bw = TB // nblk
        wf = [k.sb("wf%d" % i, [128, 16, 128], F32) for i in range(2)]
        wb = [k.sb("wb%d" % i, [128, 16, 128], BF16) for i in range(2)]
        stg = [k.sb("stg%d" % i, [128, TB], F32) for i in range(2)]
        stgb = [k.sb("stgb%d" % i, [128, TB], BF16) for i in range(2)]
        bgc, bbgc = k.sb("bgc", [128, 48], F32)
        k.DMA("sp", bgc[:], b_gate.rearrange("(c p) -> p c", p=128), w=[bbgc], slow=True)
        it = 0
        for kind, ncks in (("in", 48), ("gate", 48)):
            W = w_in if kind == "in" else w_gate
            for cc in range(ncks):
                wf_t, bwf = wf[it % 2]
                wb_t, bwb = wb[it % 2]
                c0 = cc * 128
                k.DMA("sp", wf_t[:], W[:, c0:c0 + 128].rearrange("(kc p) c -> p kc c", p=128), w=[bwf])
                k.CP("pool", wb_t[:], wf_t[:], r=[bwf], w=[bwb])
                if kind == "in":
                    so, bso = stg[it % 2]
                else:
                    so, bso = stgb[it % 2]
                for nb in range(nblk):
                    pt, bpt = k.ps[4 + (it * nblk + nb) % 4]
                    for kc in range(16):
                        k.MM(pt[:, 0:bw], wb_t[:, kc, :], xnT[:, kc, nb * bw:(nb + 1) * bw],
                             start=(kc == 0), stop=(kc == 15), r=[bwb, bxn], w=[bpt])
                    if kind == "in":
                        eng = "act" if nb % 2 == 0 else "dve"
                        k.CP(eng, so[:, nb * bw:(nb + 1) * bw], pt[:, 0:bw], r=[bpt], w=[bso])
                    else:
                        k.ACT(so[:, nb * bw:(nb + 1) * bw], pt[:, 0:bw], AF.Sigmoid, bias=bgc[:, cc:cc + 1],
                              r=[bpt, bbgc], w=[bso])
                if kind == "in":
                    k.DMA("act", k.pT[c0:c0 + 128, sbi * TB:(sbi + 1) * TB], so[:], r=[bso],
                          w=[k.R("pT", cc, sbi)])
                else:
                    k.DMA("act", k.gT[c0:c0 + 128, sbi * TB:(sbi + 1) * TB], so[:], r=[bso],
                          w=[k.R("gT", cc, sbi)])
                it += 1
        k.S.barrier()
        k.sb_ptr = mark
        wtf, bwtf = k.sb("wtf", [128, 4, NTM], F32)
        wtb, bwtb = k.sb("wtb", [128, 16, NTM], BF16)
        for q4 in range(4):
            k.DMA("sp", wtf[:], w_in[q4 * 512:(q4 + 1) * 512, O_TM:NIN].rearrange("(kc p) c -> p kc c", p=128),
                  r=[], w=[bwtf])
            k.CP("pool", wtb[:, q4 * 4:(q4 + 1) * 4, :], wtf[:], r=[bwtf], w=[bwtb])
        so2 = [k.sb("so2%d" % i, [128, NTM], F32) for i in range(2)]
        for tt in range(TB // 128):
            so, bso = so2[tt % 2]
            p0, bp0 = k.ps[(tt % 2) * 2]
            p1, bp1 = k.ps[(tt % 2) * 2 + 1]
            for kc in range(16):
                k.MM(p0[:, 0:512], xnT[:, kc, tt * 128:(tt + 1) * 128], wtb[:, kc, 0:512],
                     start=(kc == 0), stop=(kc == 15), r=[bwtb, bxn], w=[bp0])
            for kc in range(16):
                k.MM(p1[:, 0:NTM - 512], xnT[:, kc, tt * 128:(tt + 1) * 128], wtb[:, kc, 512:NTM],
                     start=(kc == 0), stop=(kc == 15), r=[bwtb, bxn], w=[bp1])
            k.CP("act", so[:, 0:512], p0[:, 0:512], r=[bp0], w=[bso])
            k.CP("dve", so[:, 512:NTM], p1[:, 0:NTM - 512], r=[bp1], w=[bso])
            row0 = sbi * TB + tt * 128
            k.DMA("act", k.tm[row0:row0 + 128, :], so[:], r=[bso], w=[k.R("tm", row0 // 128)])


def st_rot(k):
    T = k.T
    k.stage_reset()
    TWO_PI = 2.0 * math.pi
    posi, bpi = k.sb("posi", [128, T], I32)
    ang, ba = k.sb("ang", [128, T], F32)
    a2, ba2 = k.sb("a2", [128, T], F32)
    kq, bk = k.sb("kq", [128, T], F32)
    ki, bki = k.sb("ki", [128, T], I32)
    m, bm = k.sb("m", [128, T], F32)
    k.DMA("sp", posi[:], k.pos_in[0:1, :].partition_broadcast(128), w=[bpi])
    k.CP("dve", ang[:], posi[:], r=[bpi], w=[ba])
    k.TS("dve", ang[:], ang[:], k.cst[:, C_INVF:C_INVF + 1], None, ALU.mult, r=[ba, k.bcst], w=[ba])
    for which, dst in ((0, k.rotS), (1, k.rotC)):
        shift = 0.0 if which == 0 else math.pi / 2
        k.TS("dve", a2[:], ang[:], shift, None, ALU.add, r=[ba], w=[ba2])
        k.TS("dve", kq[:], a2[:], 1.0 / TWO_PI, None, ALU.mult, r=[ba2], w=[bk])
        k.CP("dve", ki[:], kq[:], r=[bk], w=[bki])
        k.CP("dve", kq[:], ki[:], r=[bki], w=[bk])
        k.STT("dve", a2[:], kq[:], -TWO_PI, a2[:], ALU.mult, ALU.add, r=[bk, ba2], w=[ba2])
        k.TS("dve", m[:], a2[:], math.pi, None, ALU.is_gt, r=[ba2], w=[bm])
        k.STT("dve", a2[:], m[:], -TWO_PI, a2[:], ALU.mult, ALU.add, r=[bm, ba2], w=[ba2])
        k.TS("dve", m[:], a2[:], -math.pi, None, ALU.is_lt, r=[ba2], w=[bm])
        k.STT("dve", a2[:], m[:], TWO_PI, a2[:], ALU.mult, ALU.add, r=[bm, ba2], w=[ba2])
        k.TS("dve", a2[:], a2[:], math.pi, -math.pi, ALU.min, ALU.max, r=[ba2], w=[ba2])
        k.ACT(kq[:], a2[:], AF.Sin, r=[ba2], w=[bk])
        if which == 0:
            k.TS("dve", kq[:], kq[:], k.cst[:, C_SGN:C_SGN + 1], None, ALU.mult, r=[bk, k.bcst], w=[bk])
        k.DMA("sp", dst[:, :], kq[:], r=[bk], w=[k.R("rot", which)])


def st_mixA(k, l):
    T = k.T
    k.stage_reset()
    cw, bcw = k.sb("cwa", [128, 3, 4], F32)
    for tap in range(3):
        k.DMA("sp", cw[:, tap, :], k.w["conv_a"][l][tap].rearrange("(c p) -> p c", p=128), w=[bcw], slow=True)
    b_, bb = k.sb("b_", [128, T], F32)
    c_, bc = k.sb("c_", [128, T], F32)
    v_, bv = k.sb("v_", [128, T], F32)
    acc, bacc = k.sb("acc", [128, T], F32)
    yb, byb = k.sb("yb", [128, T], BF16)
    nsb = max(1, T // 2048)
    for c4 in range(4):
        rd = [k.R("pT", (O_BA // 128) + c4, s) for s in range(nsb)]
        k.DMA("sp", b_[:], k.pT[O_BA + c4 * 128:O_BA + (c4 + 1) * 128, :], r=rd, w=[bb])
        rd = [k.R("pT", (O_CA // 128) + c4, s) for s in range(nsb)]
        k.DMA("sp", c_[:], k.pT[O_CA + c4 * 128:O_CA + (c4 + 1) * 128, :], r=rd, w=[bc])
        rd = [k.R("pT", (O_VA // 128) + c4, s) for s in range(nsb)]
        k.DMA("sp", v_[:], k.pT[O_VA + c4 * 128:O_VA + (c4 + 1) * 128, :], r=rd, w=[bv])
        k.TT("dve", c_[:], c_[:], v_[:], ALU.mult, r=[bc, bv], w=[bc])
        k.ACT(acc[:], c_[:], AF.Copy, scale=cw[:, 1, c4:c4 + 1], r=[bc, bcw], w=[bacc])
        k.STT("dve", acc[:, 1:T], c_[:, 0:T - 1], cw[:, 0, c4:c4 + 1], acc[:, 1:T], ALU.mult, ALU.add,
              r=[bc, bcw, bacc], w=[bacc])
        k.STT("dve", acc[:, 0:T - 1], c_[:, 1:T], cw[:, 2, c4:c4 + 1], acc[:, 0:T - 1], ALU.mult, ALU.add,
              r=[bc, bcw, bacc], w=[bacc])
        k.TT("dve", yb[:], acc[:], b_[:], ALU.mult, r=[bacc, bb], w=[byb])
        k.DMA("act", k.oT[c4 * 128:(c4 + 1) * 128, :], yb[:], r=[byb], w=[k.R("oT", c4)])


def st_attn(k, l):
    T, NT = k.T, k.NT
    lam_init = 0.8 - 0.6 * math.exp(-0.3 * l)
    k.stage_reset()
    nsb = max(1, T // 2048)
    CT, bCT = k.sb("CT", [128, T], F32)
    SN, bSN = k.sb("SN", [128, T], F32)
    k.DMA("sp", CT[:], k.rotC[:, :], r=[k.R("rot", 1)], w=[bCT])
    k.DMA("sp", SN[:], k.rotS[:, :], r=[k.R("rot", 0)], w=[bSN])
    raw, braw = k.sb("raw", [128, T], F32)
    qT, bqT = k.sb("qTr", [128, T], BF16)
    kT, bkT = k.sb("kTr", [128, T], BF16)
    vext, bvx = k.sb("vext", [128, NT, 130], BF16)
    oBT, boBT = k.sb("oBT", [128, T], BF16)
    gq, bgq = k.sb("gq", [128, 1], F32)
    gk, bgk = k.sb("gk", [128, 1], F32)
    for half in range(2):
        k.DMA("sp", gq[half * 64:(half + 1) * 64, :], k.w["q_norm"][l].rearrange("(p o) -> p o", o=1), w=[bgq], slow=True)
        k.DMA("sp", gk[half * 64:(half + 1) * 64, :], k.w["k_norm"][l].rearrange("(p o) -> p o", o=1), w=[bgk], slow=True)
    sgc, bsgc = k.sb("sgc", [128, 1], F32)
    k.DMA("sp", sgc[:], k.w["subln"][l].rearrange("(p o) -> p o", o=1), w=[bsgc], slow=True)
    k.TS("dve", sgc[:], sgc[:], 1.0 - lam_init, None, ALU.mult, r=[bsgc], w=[bsgc])
    onesb, bonesb = k.sb("onesb", [128, 128], BF16)
    k.MSET("dve", onesb[:], 1.0, w=[bonesb])
    dacc = [k.sb("dacc%d" % i, [128, 512], F32) for i in range(2)]
    lv, blv = k.sb("lv", [1, 4, 64], F32)
    for i, n in enumerate(("lambda_q1", "lambda_k1", "lambda_q2", "lambda_k2")):
        k.DMA("sp", lv[0:1, i, :], k.w[n][l:l + 1, :], w=[blv])
    lp, blp = k.sb("lp", [1, 2, 64], F32)
    k.TT("dve", lp[0:1, 0, :], lv[0:1, 0, :], lv[0:1, 1, :], ALU.mult, r=[blv], w=[blp])
    k.TT("dve", lp[0:1, 1, :], lv[0:1, 2, :], lv[0:1, 3, :], ALU.mult, r=[blv], w=[blp])
    ls, bls = k.sb("ls", [1, 2], F32)
    k.S.op("dve", lambda e: e.reduce_sum(out=ls[0:1, 0:2], in_=lp[0:1, :, :], axis=AX.X), [blp], [bls])
    k.ACT(ls[0:1, 0:2], ls[0:1, 0:2], AF.Exp, r=[bls], w=[bls])
    nl, bnl = k.sb("nl", [1, 1], F32)
    k.TT("dve", nl[0:1, 0:1], ls[0:1, 1:2], ls[0:1, 0:1], ALU.subtract, r=[bls], w=[bnl])
    k.TS("dve", nl[0:1, 0:1], nl[0:1, 0:1], -lam_init, None, ALU.add, r=[bnl], w=[bnl])
    nlb, bnlb = k.sb("nlb", [128, 1], F32)
    p7, bp7 = k.ps[7]
    k.MM(p7[:, 0:1], k.cst[0:1, C_ONES:C_ONES + 128], nl[0:1, 0:1], r=[k.bcst, bnl], w=[bp7])
    k.CP("dve", nlb[:], p7[:, 0:1], r=[bp7], w=[bnlb])
    import os
    STOP = int(os.environ.get("ATTN_STOP", "99"))
    if STOP == 1:
        return
    tmp = [k.sb("atmp%d" % i, [128, 512], F32) for i in range(4)]
    tmpB = [k.sb("atmpB%d" % i, [128, 512], F32) for i in range(4)]
    PT = [k.sb("PT%d" % i, [128, 512], BF16) for i in range(4)]
    o1, bo1 = k.sb("o1", [128, 128], F32)
    o2, bo2 = k.sb("o2", [128, 128], F32)
    ob, bob = k.sb("ob", [128, 128], BF16)
    rd, brd = k.sb("rd", [128, 2], F32)
    ssq, bssq = k.sb("assq", [128, 1], F32)
    rsd, brsd = k.sb("arsd", [128, 1], F32)
    junk, bj = k.sb("ajunk", [128, 128], F32)
    k.MSET("pool", vext[:, :, 128:130], 1.0, w=[bvx])
    BW = min(512, T)
    for h in range(6):
        for which, dstT, bdst, gcol, bgc_, off in ((0, qT, bqT, gq, bgq, O_QB), (1, kT, bkT, gk, bgk, O_KB)):
            rdl = [k.R("pT", off // 128 + h, s) for s in range(nsb)]
            k.DMA("sp", raw[:], k.pT[off + h * 128:off + (h + 1) * 128, :], r=rdl, w=[braw])
            for blk in range(T // BW):
                sl = slice(blk * BW, (blk + 1) * BW)
                (t0, bt0), (t1, bt1), (t2, bt2), (t3, bt3) = (tmp if blk % 2 == 0 else tmpB)
                pa, bpa = k.ps[4 + blk % 2]
                pb, bpb = k.ps[6 + blk % 2]
                k.ACT(t0[:, 0:BW], raw[:, sl], AF.Square, r=[braw], w=[bt0])
                k.MM(pa[:, 0:BW], k.cst[:, C_BD64:C_BD64 + 128], t0[:, 0:BW], r=[k.bcst, bt0], w=[bpa])
                k.ACT(t1[:, 0:BW], pa[:, 0:BW], AF.Sqrt, bias=k.epsc[:, 0:1], scale=1.0 / 64, r=[bpa, k.bepsc], w=[bt1])
                k.RCP(t1[:, 0:BW], t1[:, 0:BW], r=[bt1], w=[bt1])
                k.STT("dve", t2[:, 0:BW], raw[:, sl], gcol[:, 0:1], t1[:, 0:BW], ALU.mult, ALU.mult,
                      r=[braw, bgc_, bt1], w=[bt2])
                k.MM(pb[:, 0:BW], k.cst[:, C_RMAT:C_RMAT + 128], t2[:, 0:BW], r=[k.bcst, bt2], w=[bpb])
                k.TT("dve", t3[:, 0:BW], t2[:, 0:BW], CT[:, sl], ALU.mult, r=[bt2, bCT], w=[bt3])
                k.TT("dve", t0[:, 0:BW], pb[:, 0:BW], SN[:, sl], ALU.mult, r=[bpb, bSN], w=[bt0])
                k.TT("dve", dstT[:, sl], t3[:, 0:BW], t0[:, 0:BW], ALU.add, r=[bt3, bt0], w=[bdst])
        if STOP == 2:
            return
        rdl = [k.R("pT", O_VB // 128 + h, s) for s in range(nsb)]
        k.DMA("sp", raw[:], k.pT[O_VB + h * 128:O_VB + (h + 1) * 128, :], r=rdl, w=[braw])
        for tt in range(NT):
            pa, bpa = k.ps[4 + tt % 4]
            k.TR(pa[:, 0:128], raw[:, tt * 128:(tt + 1) * 128], k.cst[:, C_IDENT:C_IDENT + 128], r=[braw, k.bcst], w=[bpa])
            k.CP("act" if tt % 2 else "dve", vext[:, tt, 0:128], pa[:, 0:128], r=[bpa], w=[bvx])
        if STOP == 3:
            return
        for qb in range(T // BW):
            steps = [(s_, kc) for s_ in range(2) for kc in range(NT)]
            qsl = slice(qb * BW, (qb + 1) * BW)

            def issue_S(i):
                s_, kc = steps[i]
                pst, bpst = k.ps[4 + i % 4]
                k.MM(pst[:, 0:BW], kT[s_ * 64:(s_ + 1) * 64, kc * 128:(kc + 1) * 128],
                     qT[s_ * 64:(s_ + 1) * 64, qsl], r=[bkT, bqT], w=[bpst])
            LA = 3
            for i0_ in range(min(LA, len(steps))):
                issue_S(i0_)
            for i, (s, kc) in enumerate(steps):
                if i + LA < len(steps):
                    issue_S(i + LA)
                pst, bpst = k.ps[4 + i % 4]
                pt_, bpt_ = PT[i % 4]
                k.ACT(pt_[:, 0:BW], pst[:, 0:BW], AF.Exp, scale=0.125, r=[bpst], w=[bpt_])
                po, bpo = k.ps[s]
                pd, bpd = k.ps[2 + s]
                k.MM(po[:, 0:BW], vext[:, kc, 0:128], pt_[:, 0:BW], start=(kc == 0), stop=(kc == NT - 1),
                     r=[bpt_, bvx], w=[bpo])
                da, bda = dacc[s]
                if kc == 0:
                    k.CP("dve", da[:, 0:BW], pt_[:, 0:BW], r=[bpt_], w=[bda])
                else:
                    k.TT("dve", da[:, 0:BW], da[:, 0:BW], pt_[:, 0:BW], ALU.add, r=[bda, bpt_], w=[bda])
                if kc == NT - 1:
                    k.MM(pd[:, 0:BW], k.cst[:, C_ONES:C_ONES + 128], da[:, 0:BW], r=[k.bcst, bda], w=[bpd])
            (t0, bt0), (t1, bt1), (t2, bt2), (t3, bt3) = tmp
            k.RCP(t0[:, 0:BW], k.ps[2][0][:, 0:BW], r=[k.ps[2][1]], w=[bt0])
            k.RCP(t1[:, 0:BW], k.ps[3][0][:, 0:BW], r=[k.ps[3][1]], w=[bt1])
            k.TT("dve", t0[:, 0:BW], k.ps[0][0][:, 0:BW], t0[:, 0:BW], ALU.mult, r=[k.ps[0][1], bt0], w=[bt0])
            k.STT("dve", t1[:, 0:BW], k.ps[1][0][:, 0:BW], nlb[:, 0:1], t1[:, 0:BW], ALU.mult, ALU.mult,
                  r=[k.ps[1][1], bnlb, bt1], w=[bt1])
            k.TT("dve", t2[:, 0:BW], t0[:, 0:BW], t1[:, 0:BW], ALU.add, r=[bt0, bt1], w=[bt2])
            k.ACT(t3[:, 0:BW], t2[:, 0:BW], AF.Square, r=[bt2], w=[bt3])
            pss_, bpss = k.ps[6]
            k.MM(pss_[:, 0:BW], k.cst[:, C_ONES:C_ONES + 128], t3[:, 0:BW], r=[k.bcst, bt3], w=[bpss])
            k.ACT(t0[:, 0:BW], pss_[:, 0:BW], AF.Sqrt, bias=k.epsc[:, 0:1], scale=1.0 / 128, r=[bpss, k.bepsc], w=[bt0])
            k.RCP(t0[:, 0:BW], t0[:, 0:BW], r=[bt0], w=[bt0])
            k.STT("dve", oBT[:, qsl], t2[:, 0:BW], sgc[:, 0:1], t0[:, 0:BW], ALU.mult, ALU.mult, r=[bt2, bsgc, bt0], w=[boBT])
        k.DMA("act", k.oT[512 + h * 128:512 + (h + 1) * 128, :], oBT[:], r=[boBT], w=[k.R("oT", 4 + h)])
        k.S.barrier()
    if "dq" in k.dbg:
        dq = k.dram("dq", [128, T], BF16)
        dk = k.dram("dk", [128, T], BF16)
        dv = k.dram("dv", [128, NT * 130], BF16)
        k.DMA("sp", dq[:, :], qT[:], r=[bqT], w=[k.R("dq")])
        k.DMA("sp", dk[:, :], kT[:], r=[bkT], w=[k.R("dk")])
        k.DMA("sp", dv[:, :], vext[:].rearrange("p a b -> p (a b)"), r=[bvx], w=[k.R("dv")])


def st_merge(k, l, xsrc, xtag):
    T = k.T
    k.stage_reset()
    BW = min(512, T)
    nqt = BW // 128
    wabc = [(k.w["w_out_a"][l], 0, 4), (k.w["w_out_b"][l], 4, 6), (k.w["w_out_c"][l], 10, 6)]
    w_o = k.w["w_o"][l]
    WA, bWA = k.sb("mWA", [128, 16, D], BF16)
    WO, bWO = k.sb("mWO", [128, 16, D], BF16)
    wf = [k.sb("mwf%d" % i, [128, 16, 128], F32) for i in range(2)]
    oTb, boTb = k.sb("oTb", [128, 16, BW], BF16)
    mT, bmT = k.sb("mT", [128, 16, BW], BF16)
    gt = [k.sb("mgt%d" % i, [128, 3, BW], BF16) for i in range(2)]
    t1, bt1 = k.sb("mt1", [128, BW], F32)
    t2, bt2 = k.sb("mt2", [128, BW], F32)
    xt = [k.sb("mxt%d" % i, [128, 512], F32) for i in range(2)]
    ce = ("dve", "act", "pool")
    for oc in range(16):
        wf_t, bwf = wf[oc % 2]
        for (wap, c0, nk) in wabc:
            k.DMA("sp", wf_t[:, c0:c0 + nk, :], wap[:, oc * 128:(oc + 1) * 128].rearrange("(kc p) c -> p kc c", p=128),
                  w=[bwf])
        k.CP(ce[oc % 3], WA[:, :, oc * 128:(oc + 1) * 128], wf_t[:], r=[bwf], w=[bWA])
    for oc in range(16):
        wf_t, bwf = wf[oc % 2]
        k.DMA("sp", wf_t[:], w_o[:, oc * 128:(oc + 1) * 128].rearrange("(kc p) c -> p kc c", p=128), w=[bwf])
        k.CP(ce[(oc + 1) % 3], WO[:, :, oc * 128:(oc + 1) * 128], wf_t[:], r=[bwf], w=[bWO])
    it = 0
    nsb = max(1, T // 2048)
    for tb in range(T // BW):
        tsl = slice(tb * BW, (tb + 1) * BW)
        for c in range(16):
            k.DMA("sp", oTb[:, c, :], k.oT[c * 128:(c + 1) * 128, tsl], r=[k.R("oT", c)], w=[boTb])
        for oc in range(16):
            g_t, bg = gt[it % 2]
            it += 1
            for br in range(3):
                row = br * 2048 + oc * 128
                k.DMA("act", g_t[:, br, :], k.gT[row:row + 128, tsl],
                      r=[k.R("gT", row // 128, s_) for s_ in range(nsb)], w=[bg])
            pss = []
            for bi, (wap, c0, nk) in enumerate(wabc):
                pt, bpt = k.ps[bi + 3 * (oc % 2)]
                for kc in range(c0, c0 + nk):
                    k.MM(pt[:, 0:BW], WA[:, kc, oc * 128:(oc + 1) * 128], oTb[:, kc, :], start=(kc == c0),
                         stop=(kc == c0 + nk - 1), r=[bWA, boTb], w=[bpt])
                pss.append((pt, bpt))
            k.TT("dve", t1[:], pss[0][0][:, 0:BW], g_t[:, 0, :], ALU.mult, r=[pss[0][1], bg], w=[bt1])
            k.TT("dve", t2[:], pss[1][0][:, 0:BW], g_t[:, 1, :], ALU.mult, r=[pss[1][1], bg], w=[bt2])
            k.TT("dve", t1[:], t1[:], t2[:], ALU.add, r=[bt1, bt2], w=[bt1])
            k.TT("dve", t2[:], pss[2][0][:, 0:BW], g_t[:, 2, :], ALU.mult, r=[pss[2][1], bg], w=[bt2])
            k.TT("dve", mT[:, oc, :], t1[:], t2[:], ALU.add, r=[bt1, bt2], w=[bmT])
        iq = 0
        for cb in range(4):
            for qt in range(nqt):
                x_t, bx = xt[iq % 2]
                pt, bpt = k.ps[6 + iq % 2]
                iq += 1
                row0 = tb * BW + qt * 128
                k.DMA("act", x_t[:], xsrc[row0:row0 + 128, cb * 512:(cb + 1) * 512], r=[k.R(xtag, row0 // 128)], w=[bx])
                for kc in range(16):
                    k.MM(pt[:, 0:512], mT[:, kc, qt * 128:(qt + 1) * 128], WO[:, kc, cb * 512:(cb + 1) * 512],
                         start=(kc == 0), stop=(kc == 15), r=[bmT, bWO], w=[bpt])
                k.TT("dve", x_t[:], pt[:, 0:512], x_t[:], ALU.add, r=[bpt, bx], w=[bx])
                k.DMA("act", k.hbuf[row0:row0 + 128, cb * 512:(cb + 1) * 512], x_t[:], r=[bx], w=[k.R("h", row0 // 128, cb)])


def st_moe(k, l, xdst, xdtag):
    T, NT, CAP = k.T, k.NT, k.CAP
    SR = min(128, CAP)
    nst = (CAP + 127) // 128
    NSLOT = NE * CAP
    BIG = float(NSLOT + 1000)
    ident = k.cst[:, C_IDENT:C_IDENT + 128]

    def bndreg(eh):
        if getattr(k, "_bnd", None) is None:
            k._bnd = eh.alloc_register("bnd")
            eh.reg_mov(k._bnd, NSLOT - 1)
        return k._bnd
    k.stage_reset()
    mT, bmT = k.sb("mskT", [128, NT, 16], F32)
    wT, bwT = k.sb("wT", [128, NT, 16], F32)
    idxf, bxf = k.sb("idxf", [128, NT, 16], F32)
    idxi, bxi = k.sb("idxi", [128, NT, 16], I32)
    basee, bbe = k.sb("basee", [128, 16], F32)
    sm, bsm = k.sb("bis", [16, 8], F32)
    persist2 = k.sb_ptr
    lgT, blg = k.sb("lgT", [16, T], F32)
    persist = k.sb_ptr
    gbc, bg = k.sb("gbc2", [128, D], F32)
    k.DMA("sp", gbc[:], k.w["norm_ffn"][l].partition_broadcast(128), w=[bg])
    wr, bwr = k.sb("wr", [128, 16, NE], F32)
    k.DMA("sp", wr[:], k.w["w_router"][l].rearrange("(kc p) e -> p kc e", p=128), w=[bwr])
    xt = [k.sb("hx%d" % i, [128, D], F32) for i in range(2)]
    hnf = [k.sb("hnf%d" % i, [128, D], F32) for i in range(2)]
    hnb = [k.sb("hnb%d" % i, [128, D], BF16) for i in range(2)]
    hT = [k.sb("hT%d" % i, [128, 16, 128], F32) for i in range(2)]
    junk, bj = k.sb("mjunk", [128, D], BF16)
    ssq = [k.sb("mssq%d" % i, [128, 1], F32) for i in range(2)]
    rs = [k.sb("mrs%d" % i, [128, 1], F32) for i in range(2)]
    for tt in range(NT):
        x_t, bx = xt[tt % 2]
        f_t, bf = hnf[tt % 2]
        b_t, bb = hnb[tt % 2]
        h_t, bh = hT[tt % 2]
        sq, bsq = ssq[tt % 2]
        r_, br = rs[tt % 2]
        row0 = tt * 128
        k.DMA("sp", x_t[:], k.hbuf[row0:row0 + 128, :], r=[k.R("h", tt, cb) for cb in range(4)], w=[bx])
        k.ACT(junk[:], x_t[:], AF.Square, accum=sq[:], r=[bx], w=[bj, bsq])
        rstd_from_ssq(k, r_[:], sq[:], D, br, bsq)
        k.STT("dve", f_t[:], x_t[:], r_[:, 0:1], gbc[:], ALU.mult, ALU.mult, r=[bx, br, bg], w=[bf])
        k.CP("act", b_t[:], f_t[:], r=[bf], w=[bb])
        k.DMA("act", k.hn[row0:row0 + 128, :], b_t[:], r=[bb], w=[k.R("hn", tt)])
        for g4 in range(4):
            pt, bpt = k.ps[g4]
            for c in range(4):
                kc = g4 * 4 + c
                k.TR(pt[:, c * 128:(c + 1) * 128], f_t[:, kc * 128:(kc + 1) * 128], ident, r=[bf, k.bcst], w=[bpt])
            k.CP("act" if g4 % 2 else "dve", h_t[:, g4 * 4:(g4 + 1) * 4, :],
                 pt[:, 0:512].rearrange("p (c t) -> p c t", c=4), r=[bpt], w=[bh])
        pl, bpl = k.ps[4 + tt % 2]
        for kc in range(16):
            k.MM(pl[0:16, 0:128], wr[:, kc, :], h_t[:, kc, :], start=(kc == 0), stop=(kc == 15), r=[bwr, bh], w=[bpl])
        k.CP("act", lgT[:, row0:row0 + 128], pl[0:16, 0:128], r=[bpl], w=[blg])
    k.S.barrier()
    k.sb_ptr = persist
    aff, baf = k.sb("aff", [16, T], F32)
    tmpA, btA = k.sb("tmpA", [16, T], F32)
    k.ACT(aff[:], lgT[:], AF.Exp, r=[blg], w=[baf])
    BW = min(512, T)
    for blk in range(T // BW):
        sl = slice(blk * BW, (blk + 1) * BW)
        pt, bpt = k.ps[blk % 2]
        k.MM(pt[0:16, 0:BW], k.cst[0:16, C_ONES:C_ONES + 16], aff[:, sl], r=[k.bcst, baf], w=[bpt])
        k.RCP(tmpA[:, sl], pt[0:16, 0:BW], r=[bpt], w=[btA])
    k.TT("dve", aff[:], aff[:], tmpA[:], ALU.mult, r=[baf, btA], w=[baf])
    lo, hi, mid, cnt, ge, d1 = [sm[:, i:i + 1] for i in range(6)]
    k.MSET("dve", sm[:], 0.0, w=[bsm])
    k.MSET("dve", hi, 1.0, w=[bsm])
    for itn in range(30):
        k.TT("dve", mid, lo, hi, ALU.add, r=[bsm], w=[bsm])
        k.TS("dve", mid, mid, 0.5, None, ALU.mult, r=[bsm], w=[bsm])
        k.TS("dve", tmpA[:], aff[:], mid, 0.0, ALU.is_ge, ALU.add, accum=cnt, r=[baf, bsm], w=[btA, bsm])
        k.TS("dve", ge, cnt, float(CAP), None, ALU.is_ge, r=[bsm], w=[bsm])
        k.TT("dve", d1, mid, lo, ALU.subtract, r=[bsm], w=[bsm])
        k.STT("dve", lo, d1, ge, lo, ALU.mult, ALU.add, r=[bsm], w=[bsm])
        k.TT("dve", d1, hi, mid, ALU.subtract, r=[bsm], w=[bsm])
        k.STT("dve", hi, d1, ge, mid, ALU.mult, ALU.add, r=[bsm], w=[bsm])
    msk, bmk = k.sb("msk", [16, T], F32)
    k.TS("dve", msk[:], aff[:], lo, None, ALU.is_ge, r=[baf, bsm], w=[bmk])
    k.TT("dve", aff[:], aff[:], msk[:], ALU.mult, r=[baf, bmk], w=[baf])
    k.TS("dve", basee[:], k.cst[:, C_IOTA:C_IOTA + 16], float(CAP), None, ALU.mult, r=[k.bcst], w=[bbe])
    for tt in range(NT):
        pt, bpt = k.ps[tt % 2]
        k.TR(pt[:, 0:16], msk[:, tt * 128:(tt + 1) * 128], k.cst[0:16, C_IDENT:C_IDENT + 16], r=[bmk, k.bcst], w=[bpt])
        k.TR(pt[:, 16:32], aff[:, tt * 128:(tt + 1) * 128], k.cst[0:16, C_IDENT:C_IDENT + 16], r=[baf, k.bcst], w=[bpt])
        k.CP("dve", mT[:, tt, :], pt[:, 0:16], r=[bpt], w=[bmT])
        k.CP("act", wT[:, tt, :], pt[:, 16:32], r=[bpt], w=[bwT])
    for tt in range(NT):
        pt, bpt = k.ps[2 + tt % 2]
        for t2 in range(tt):
            k.MM(pt[:, 0:16], k.cst[:, C_ONES:C_ONES + 128], mT[:, t2, :], start=(t2 == 0), stop=False,
                 r=[k.bcst, bmT], w=[bpt])
        k.MM(pt[:, 0:16], k.cst[:, C_SLT:C_SLT + 128], mT[:, tt, :], start=(tt == 0), stop=True, r=[k.bcst, bmT], w=[bpt])
        k.TT("dve", idxf[:, tt, :], pt[:, 0:16], basee[:], ALU.add, r=[bpt, bbe], w=[bxf])
    k.TS("dve", idxf[:], idxf[:], -BIG, None, ALU.add, r=[bxf], w=[bxf])
    k.TT("dve", idxf[:], idxf[:], mT[:], ALU.mult, r=[bxf, bmT], w=[bxf])
    k.TS("dve", idxf[:], idxf[:], BIG, None, ALU.add, r=[bxf], w=[bxf])
    k.CP("dve", idxi[:], idxf[:], r=[bxf], w=[bxi])
    if "dbg_idx" in k.dbg:
        di = k.dram("dbg_idx", [128, NT * 16], I32)
        k.DMA("sp", di[:, :], idxi[:].rearrange("p a b -> p (a b)"), r=[bxi], w=[k.R("dbgidx")])
        dw = k.dram("dbg_w", [128, NT * 16], F32)
        k.DMA("sp", dw[:, :], wT[:].rearrange("p a b -> p (a b)"), r=[bwT], w=[k.R("dbgw")])
    k.S.barrier()
    k.sb_ptr = persist2
    hb = [k.sb("dhb%d" % i, [128, D], BF16) for i in range(2)]
    bxs = k.R("xs")
    for tt in range(NT):
        h_t, bh = hb[tt % 2]
        k.DMA("sp", h_t[:], k.hn[tt * 128:(tt + 1) * 128, :], r=[k.R("hn", tt)], w=[bh])
        for e in range(NE):
            off = idxi[:, tt, e:e + 1]

            def f(eh, off=off, h_t=h_t):
                return eh.indirect_dma_start(out=k.xs[:, :], out_offset=bass.IndirectOffsetOnAxis(ap=off, axis=0),
                                             in_=h_t[:, :], in_offset=None, bounds_check=bndreg(eh), oob_is_err=False)
            k.S.dma("pool", f, [bh, bxi], [k.R("xsw", tt, e)])
    k.S.barrier()
    k.sb_ptr = persist2
    xr = [k.sb("xr%d" % i, [SR, D], BF16) for i in range(2)]
    xsT, bxT = k.sb("xsT", [128, 16, CAP], BF16)
    hidT, bhid = k.sb("hidT", [128, 8, CAP], BF16)
    wf = [k.sb("ewf%d" % i, [128, 16, 512], F32) for i in range(2)]
    wbf = [k.sb("ewb%d" % i, [128, 16, 512], BF16) for i in range(2)]
    wdf = [k.sb("ewdf%d" % i, [128, 8, 512], F32) for i in range(2)]
    wdb = [k.sb("ewdb%d" % i, [128, 8, 512], BF16) for i in range(2)]
    sg, bsg = k.sb("esg", [128, CAP], F32)
    yt = [k.sb("eyt%d" % i, [SR, 512], F32) for i in range(2)]
    iw = 0
    idw = 0
    iy = 0
    for e in range(NE):
        for stl in range(nst):
            x_r, bxr = xr[stl % 2]
            r0 = e * CAP + stl * SR
            k.DMA("sp", x_r[:], k.xs[r0:r0 + SR, :], r=[], w=[bxr])
            for g4 in range(4):
                pt, bpt = k.ps[g4]
                ptb = pt[:].bitcast(BF16)
                for c in range(4):
                    kc = g4 * 4 + c
                    k.TR(ptb[:, c * SR:(c + 1) * SR], x_r[:, kc * 128:(kc + 1) * 128], k.identb[0:SR, 0:SR],
                         r=[bxr, k.bidentb], w=[bpt])
                k.CP("act" if g4 % 2 else "dve", xsT[:, g4 * 4:(g4 + 1) * 4, stl * SR:(stl + 1) * SR],
                     ptb[:, 0:4 * SR].rearrange("p (c t) -> p c t", c=4), r=[bpt], w=[bxT])
        for fcg in range(2):
            grp = []
            for which, wn in ((0, "w_e_gate"), (1, "w_e_up")):
                wf_t, bwf = wf[which]
                wb_t, bwb = wbf[which]
                k.DMA("sp", wf_t[:], k.w[wn][l, e][:, fcg * 512:(fcg + 1) * 512].rearrange("(kc p) c -> p kc c", p=128), w=[bwf])
                for piece in range(4):
                    k.CP(("dve", "pool", "act", "dve")[(iw + piece) % 4], wb_t[:, :, piece * 128:(piece + 1) * 128],
                         wf_t[:, :, piece * 128:(piece + 1) * 128], r=[bwf], w=[bwb])
                iw += 1
                grp.append((wb_t, bwb))
            for f4 in range(4):
                fc = fcg * 4 + f4
                pg, bpg = k.ps[4 + (fc % 2) * 2]
                pu, bpu = k.ps[5 + (fc % 2) * 2]
                for (wb_t, bwb), pp, bpp in ((grp[0], pg, bpg), (grp[1], pu, bpu)):
                    for kc in range(16):
                        k.MM(pp[:, 0:CAP], wb_t[:, kc, f4 * 128:(f4 + 1) * 128], xsT[:, kc, :], start=(kc == 0), stop=(kc == 15),
                             r=[bwb, bxT], w=[bpp])
                k.ACT(sg[:], pg[:, 0:CAP], AF.Silu, r=[bpg], w=[bsg])
                k.TT("dve", hidT[:, fc, :], pu[:, 0:CAP], sg[:], ALU.mult, r=[bpu, bsg], w=[bhid])
        for cb in range(4):
            wd_f, bwdf = wdf[idw % 2]
            wd_b, bwdb = wdb[idw % 2]
            idw += 1
            k.DMA("sp", wd_f[:], k.w["w_e_down"][l, e][:, cb * 512:(cb + 1) * 512].rearrange("(fc p) c -> p fc c", p=128), w=[bwdf])
            k.CP("pool", wd_b[:, 0:4, :], wd_f[:, 0:4, :], r=[bwdf], w=[bwdb])
            k.CP("dve", wd_b[:, 4:8, :], wd_f[:, 4:8, :], r=[bwdf], w=[bwdb])
            for stl in range(nst):
                py, bpy = k.ps[iy % 4]
                y_t, by = yt[iy % 2]
                iy += 1
                for fc in range(8):
                    k.MM(py[0:SR, 0:512], hidT[:, fc, stl * SR:(stl + 1) * SR], wd_b[:, fc, :], start=(fc == 0), stop=(fc == 7),
                         r=[bhid, bwdb], w=[bpy])
                k.CP("act" if iy % 2 else "dve", y_t[:], py[0:SR, 0:512], r=[bpy], w=[by])
                r0 = e * CAP + stl * SR
                k.DMA("act", k.ys[r0:r0 + SR, cb * 512:(cb + 1) * 512], y_t[:], r=[by], w=[k.R("ysw", e, stl, cb)])
    k.S.barrier()
    k.sb_ptr = persist2
    acc = [k.sb("cacc%d" % i, [128, D], F32) for i in range(2)]
    gb = [k.sb("cgb%d" % i, [128, D], F32) for i in range(3)]
    for g_t, bgb in gb:
        k.MSET("pool", g_t[:], 0.0, w=[bgb])
    ig = 0
    for tt in range(NT):
        a_t, ba = acc[tt % 2]
        k.DMA("sp", a_t[:], k.hbuf[tt * 128:(tt + 1) * 128, :], r=[], w=[ba])
        for e in range(NE):
            g_t, bgb = gb[ig % 3]
            ig += 1
            off = idxi[:, tt, e:e + 1]

            def f(eh, off=off, g_t=g_t):
                return eh.indirect_dma_start(out=g_t[:, :], out_offset=None, in_=k.ys[:, :],
                                             in_offset=bass.IndirectOffsetOnAxis(ap=off, axis=0),
                                             bounds_check=bndreg(eh), oob_is_err=False)
            k.S.dma("pool", f, [bxi], [bgb])
            k.STT("dve", a_t[:], g_t[:], wT[:, tt, e:e + 1], a_t[:], ALU.mult, ALU.add, r=[bgb, bwT, ba], w=[ba])
        k.DMA("act", xdst[tt * 128:(tt + 1) * 128, :], a_t[:], r=[ba], w=[k.R(xdtag, tt)])


def st_delta(k, l):
    T, NT = k.T, k.NT
    k.stage_reset()
    nsb = max(1, T // 2048)
    ident = k.cst[:, C_IDENT:C_IDENT + 128]
    ones = k.cst[:, C_ONES:C_ONES + 128]
    bc = k.bcst
    rr = [0]

    def PR():
        c = rr[0]
        rr[0] += 1
        b, q = c % 8, (c // 8) % 4
        return k.ps[b][0][:, q * 128:(q + 1) * 128], k.ps[b][1]

    def PR2():
        c = rr[0]
        rr[0] += 1
        b, q = c % 8, 2 * ((c // 8) % 2)
        return k.ps[b][0][:, q * 128:(q + 2) * 128], k.ps[b][1], k.ps[b][1]

    regs = [(k.ps[i % 8][0][:, (i // 8) * 128:(i // 8 + 1) * 128], k.ps[i % 8][1]) for i in range(32)]
    prm, bprm = k.sb("dprm", [128, 24], F32)
    for i, n in enumerate(("dt_bias_f", "dt_bias_b", "a_log_f", "a_log_b")):
        k.DMA("sp", prm[:, i * 6:(i + 1) * 6], k.w[n][l].partition_broadcast(128), w=[bprm])
    negA, bnA = k.sb("negA", [128, 12], F32)
    k.ACT(negA[:], prm[:, 12:24], AF.Exp, r=[bprm], w=[bnA])
    k.TS("dve", negA[:], negA[:], -1.0, None, ALU.mult, r=[bnA], w=[bnA])
    smA, bsmA = k.sb("smA", [128, NT, 24], F32)
    for tt in range(NT):
        k.DMA("sp", smA[:, tt, :], k.tm[tt * 128:(tt + 1) * 128, 768:792], r=[k.R("tm", tt)], w=[bsmA])
    beta, bbeta = k.sb("dbeta", [128, NT, 12], F32)
    gg, bgg = k.sb("dg", [128, NT, 12], F32)
    gc, bgc = k.sb("dgc", [128, NT, 12], F32)
    eg, beg = k.sb("deg", [128, NT, 12], F32)
    kd, bkd = k.sb("dkd", [128, NT, 12], F32)
    gend, bgend = k.sb("dgend", [128, NT, 24], F32)
    k.ACT(beta[:], smA[:, :, 0:12], AF.Sigmoid, r=[bsmA], w=[bbeta])
    for tt in range(NT):
        k.TT("dve", gg[:, tt, :], smA[:, tt, 12:24], prm[:, 0:12], ALU.add, r=[bsmA, bprm], w=[bgg])
    k.ACT(gg[:], gg[:], AF.Exp, r=[bgg], w=[bgg])
    k.ACT(gg[:], gg[:], AF.Ln, bias=1.0, r=[bgg], w=[bgg])
    for tt in range(NT):
        k.TT("dve", gg[:, tt, :], gg[:, tt, :], negA[:], ALU.mult, r=[bgg, bnA], w=[bgg])
    for tt in range(NT):
        p, bp = PR()
        k.MM(p[:, 0:6], k.cst[:, C_CUMF:C_CUMF + 128], gg[:, tt, 0:6], r=[bc, bgg], w=[bp])
        k.MM(p[:, 6:12], k.cst[:, C_CUMB:C_CUMB + 128], gg[:, tt, 6:12], r=[bc, bgg], w=[bp])
        k.CP("dve", gc[:, tt, :], p[:, 0:12], r=[bp], w=[bgc])
        p2, bp2 = PR()
        k.MM(p2[:, 0:6], k.cst[:, C_LASTF:C_LASTF + 128], gc[:, tt, 0:6], r=[bc, bgc], w=[bp2])
        k.MM(p2[:, 6:12], k.cst[:, C_LASTB:C_LASTB + 128], gc[:, tt, 6:12], r=[bc, bgc], w=[bp2])
        k.TT("dve", kd[:, tt, :], p2[:, 0:12], gc[:, tt, :], ALU.subtract, r=[bp2, bgc], w=[bkd])
        p3, bp3 = PR()
        for ci, (cf, cb_) in enumerate(((C_SELFA, C_SELBA), (C_SELFB, C_SELBB))):
            k.MM(p3[:, ci * 12:ci * 12 + 6], k.cst[:, cf:cf + 128], gc[:, tt, 0:6], r=[bc, bgc], w=[bp3])
            k.MM(p3[:, ci * 12 + 6:ci * 12 + 12], k.cst[:, cb_:cb_ + 128], gc[:, tt, 6:12], r=[bc, bgc], w=[bp3])
        k.CP("dve", gend[:, tt, :], p3[:, 0:24], r=[bp3], w=[bgend])
    k.ACT(eg[:], gc[:], AF.Exp, r=[bgc], w=[beg])
    k.ACT(kd[:], kd[:], AF.Exp, r=[bkd], w=[bkd])
    k.ACT(gend[:], gend[:], AF.Exp, r=[bgend], w=[bgend])
    ogb, bogb = k.sb("ogb", [128, 128], F32)
    k.DMA("sp", ogb[:], k.w["o_norm"][l].partition_broadcast(128), w=[bogb])
    cwc, bcwc = k.sb("cwc", [128, 3, 18], F32)
    for tap in range(3):
        k.DMA("sp", cwc[:, tap, :], k.w["conv_c"][l][tap].rearrange("(c p) -> p c", p=128), w=[bcwc], slow=True)
    qT, bqT = k.sb("dqT", [128, T], F32)
    kT, bkT = k.sb("dkT", [128, T], F32)
    Kt, bKt = k.sb("dKt", [128, NT, 128], F32)
    Vt, bVt = k.sb("dVt", [128, NT, 128], F32)
    of_, bof = k.sb("dof", [128, NT, 128], F32)
    ob_, bob = k.sb("dob", [128, NT, 128], F32)
    oCT, boCT = k.sb("oCT", [128, T], BF16)
    raw = of_[:].rearrange("p a b -> p (a b)")
    acc = ob_[:].rearrange("p a b -> p (a b)")
    BW = min(512, T)
    tmpb = [k.sb("dtb%d" % i, [128, BW], F32) for i in range(4)]

    BUFS = [[None, None], [None, None]]
    for d_ in range(2):
        for par_ in range(2):
            B = {}
            for nm in ("DG", "DEC", "LM", "LT", "ATT", "ATTT", "PA", "PAT", "PB", "PBT", "WT", "KD"):
                B[nm] = k.sb("%s%d%d" % (nm, d_, par_), [128, 128], F32)
            for nm in ("RHS", "XX"):
                B[nm] = k.sb("%s%d%d" % (nm, d_, par_), [128, 256], F32)
            BUFS[d_][par_] = B
    VN = [k.sb("VN%d" % d_, [128, 128], F32) for d_ in range(2)]
    O1 = [k.sb("O1%d" % d_, [128, 128], F32) for d_ in range(2)]
    SS = [[k.sb("S%d_%d" % (d, i), [128, 128], F32) for i in range(2)] for d in range(2)]
    gout, bgout = k.sb("gout", [128, 128], F32)
    osum, bosum = k.sb("osum", [128, 128], F32)
    onb, bonb = k.sb("onb", [128, 128], BF16)
    fssq, bfssq = k.sb("fssq", [128, 1], F32)
    frs, bfrs = k.sb("frs", [128, 1], F32)
    fj, bfj = k.sb("fj", [128, 128], F32)
    masks = ((C_NEGF, C_STRF), (C_NEGB, C_STRB))

    for h in range(6):
        for which, off, dst, bdst in ((0, O_QC, qT, bqT), (1, O_KC, kT, bkT), (2, O_VC, None, None)):
            ch = which * 6 + h
            rdl = [k.R("pT", off // 128 + h, s) for s in range(nsb)]
            k.DMA("sp", raw, k.pT[off + h * 128:off + (h + 1) * 128, :], r=rdl, w=[bof])
            k.ACT(acc, raw, AF.Copy, scale=cwc[:, 1, ch:ch + 1], r=[bof, bcwc], w=[bob])
            k.STT("dve", acc[:, 1:T], raw[:, 0:T - 1], cwc[:, 0, ch:ch + 1], acc[:, 1:T], ALU.mult, ALU.add,
                  r=[bof, bcwc, bob], w=[bob])
            k.STT("dve", acc[:, 0:T - 1], raw[:, 1:T], cwc[:, 2, ch:ch + 1], acc[:, 0:T - 1], ALU.mult, ALU.add,
                  r=[bof, bcwc, bob], w=[bob])
            k.ACT(acc, acc, AF.Silu, r=[bob], w=[bob])
            if which < 2:
                for blk in range(T // BW):
                    sl = slice(blk * BW, (blk + 1) * BW)
                    (t0, bt0), (t1, bt1) = tmpb[2 * (blk % 2):2 * (blk % 2) + 2]
                    k.ACT(t0[:], acc[:, sl], AF.Square, r=[bob], w=[bt0])
                    pb4 = k.ps[blk % 2]
                    k.MM(pb4[0][:, 0:BW], ones, t0[:], r=[bc, bt0], w=[pb4[1]])
                    k.ACT(t1[:], pb4[0][:, 0:BW], AF.Sqrt, bias=k.epsc[:, 0:1], r=[pb4[1], k.bepsc], w=[bt1])
                    k.RCP(t1[:], t1[:], r=[bt1], w=[bt1])
                    if which == 0:
                        k.STT("dve", dst[:, sl], acc[:, sl], 128.0 ** -0.5, t1[:], ALU.mult, ALU.mult, r=[bob, bt1], w=[bdst])
                    else:
                        k.TT("dve", dst[:, sl], acc[:, sl], t1[:], ALU.mult, r=[bob, bt1], w=[bdst])
            else:
                for tt in range(NT):
                    p, bp = regs[8 + tt % 8]
                    k.TR(p, acc[:, tt * 128:(tt + 1) * 128], ident, r=[bob, bc], w=[bp])
                    k.CP("act" if tt % 2 else "dve", Vt[:, tt, :], p, r=[bp], w=[bVt])
        for tt in range(NT):
            p, bp = regs[8 + tt % 8]
            k.TR(p, kT[:, tt * 128:(tt + 1) * 128], ident, r=[bkT, bc], w=[bp])
            k.CP("act" if tt % 2 else "dve", Kt[:, tt, :], p, r=[bp], w=[bKt])
        k.S.barrier()
        rr[0] = 0
        for d in range(2):
            k.MSET("dve", SS[d][0][0][:], 0.0, w=[SS[d][0][1]])
        scur = [0, 0]

        def intra(d, it):
            par = it % 2
            tt = it if d == 0 else NT - 1 - it
            col = d * 6 + h
            tsl = slice(tt * 128, (tt + 1) * 128)
            cneg, cstr = masks[d]
            bsc = beta[:, tt, col:col + 1]
            gsc = gc[:, tt, col:col + 1]
            esc = eg[:, tt, col:col + 1]
            B = BUFS[d][par]
            (dg, bdg), (dec, bdec), (lm, blm), (lt, blt) = B["DG"], B["DEC"], B["LM"], B["LT"]
            (att, batt), (attT, battT), (rhs, brhs), (xx, bxx) = B["ATT"], B["ATTT"], B["RHS"], B["XX"]
            (wt, bwt), (kdt, bkdt) = B["WT"], B["KD"]
            pkk, bpkk = PR()
            k.MM(pkk, kT[:, tsl], kT[:, tsl], r=[bkT], w=[bpkk])
            k.ACT(dg[:], ident, AF.Copy, scale=gsc, r=[bc, bgc], w=[bdg])
            yield
            pg, bpg = PR()
            k.MM(pg, ones, dg[:], r=[bc, bdg], w=[bpg])
            k.STT("dve", dec[:], pg, -1.0, k.cst[:, cneg:cneg + 128], ALU.mult, ALU.add, r=[bpg, bc], w=[bdec])
            yield
            k.ACT(dec[:], dec[:], AF.Exp, bias=gsc, r=[bdec, bgc], w=[bdec])
            k.ACT(rhs[:, 0:128], Vt[:, tt, :], AF.Copy, scale=bsc, r=[bVt, bbeta], w=[brhs])
            k.TS("dve", rhs[:, 128:256], Kt[:, tt, :], bsc, esc, ALU.mult, ALU.mult, r=[bKt, bbeta, beg], w=[brhs])
            yield
            k.STT("dve", lm[:], pkk, bsc, dec[:], ALU.mult, ALU.mult, r=[bpkk, bbeta, bdec], w=[blm])
            k.TT("dve", lm[:], lm[:], k.cst[:, cstr:cstr + 128], ALU.mult, r=[blm, bc], w=[blm])
            yield
            p1, bp1 = PR()
            k.TR(p1, lm[:], ident, r=[blm, bc], w=[bp1])
            k.CP("act", lt[:], p1, r=[bp1], w=[blt])
            yield
            pqk, bpqk = PR()
            k.MM(pqk, qT[:, tsl], kT[:, tsl], r=[bqT, bkT], w=[bpqk])
            k.TT("dve", att[:], pqk, dec[:], ALU.mult, r=[bpqk, bdec], w=[batt])
            k.ACT(kdt[:], Kt[:, tt, :], AF.Copy, scale=kd[:, tt, col:col + 1], r=[bKt, bkd], w=[bkdt])
            yield
            px, bpxa, bpxb = PR2()
            k.MM(px, lt[:], rhs[:], r=[blt, brhs], w=[bpxa, bpxb])
            k.TT("dve", xx[:], rhs[:], px, ALU.subtract, r=[brhs, bpxa, bpxb], w=[bxx])
            yield
            p2, bp2 = PR()
            k.TR(p2, att[:], ident, r=[batt, bc], w=[bp2])
            k.CP("act", attT[:], p2, r=[bp2], w=[battT])
            yield
            P, bP = lm, blm
            PT_, bPT = lt, blt
            nxt = [(B["PA"], B["PAT"]), (B["PB"], B["PBT"])]
            for lvl in range(5):
                (np_, bnp), (npt, bnpt) = nxt[lvl % 2]
                pt2, bpt2 = PR()
                k.MM(pt2, P[:], PT_[:], r=[bP, bPT], w=[bpt2])
                k.CP("act", npt[:], pt2, r=[bpt2], w=[bnpt])
                if lvl < 4:
                    pp2, bpp2 = PR()
                    k.MM(pp2, PT_[:], P[:], r=[bP, bPT], w=[bpp2])
                    k.CP("pool" if False else "dve", np_[:], pp2, r=[bpp2], w=[bnp])
                yield
                px, bpxa, bpxb = PR2()
                k.MM(px, npt[:], xx[:], r=[bnpt, bxx], w=[bpxa, bpxb])
                k.TT("dve", xx[:], xx[:], px, ALU.add, r=[bxx, bpxa, bpxb], w=[bxx])
                P, bP, PT_, bPT = np_, bnp, npt, bnpt
                yield
            p3, bp3 = PR()
            k.TR(p3, xx[:, 128:256], ident, r=[bxx, bc], w=[bp3])
            k.CP("act", wt[:], p3, r=[bp3], w=[bwt])
            yield

        def rec(d, it):
            par = it % 2
            tt = it if d == 0 else NT - 1 - it
            col = d * 6 + h
            tsl = slice(tt * 128, (tt + 1) * 128)
            B = BUFS[d][par]
            (attT, battT), (xx, bxx), (wt, bwt), (kdt, bkdt) = B["ATTT"], B["XX"], B["WT"], B["KD"]
            (vn, bvn), (o1, bo1) = VN[d], O1[d]
            odst, bodst = (of_, bof) if d == 0 else (ob_, bob)
            for step in range(2):
                ci = step if d == 0 else 1 - step
                rows = slice(ci * 64, (ci + 1) * 64)
                S_, bS = SS[d][scur[d]]
                Sn, bSn = SS[d][1 - scur[d]]
                scur[d] = 1 - scur[d]
                pv, bpv = PR()
                k.MM(pv, wt[:], S_[:], r=[bwt, bS], w=[bpv])
                po1, bpo1 = PR()
                k.MM(po1, qT[:, tsl], S_[:], r=[bqT, bS], w=[bpo1])
                k.TT("dve", vn[rows, :], xx[rows, 0:128], pv[rows, :], ALU.subtract, r=[bxx, bpv], w=[bvn])
                k.ACT(o1[rows, :], po1[rows, :], AF.Copy, scale=eg[rows, tt, col:col + 1], r=[bpo1, beg], w=[bo1])
                yield
                pS, bpS = PR()
                k.MM(pS, kdt[rows, :], vn[rows, :], r=[bkdt, bvn], w=[bpS])
                gcol = ci * 12 + col
                k.STT("dve", Sn[:], S_[:], gend[:, tt, gcol:gcol + 1], pS, ALU.mult, ALU.add, r=[bS, bgend, bpS], w=[bSn])
                po2, bpo2 = PR()
                k.MM(po2, attT[rows, :], vn[rows, :], r=[battT, bvn], w=[bpo2])
                k.TT("pool" if False else "dve", odst[rows, tt, :], o1[rows, :], po2[rows, :], ALU.add, r=[bo1, bpo2], w=[bodst])
                yield

        def run_rr(gens):
            gens = list(gens)
            while gens:
                for g in list(gens):
                    try:
                        next(g)
                    except StopIteration:
                        gens.remove(g)

        run_rr([intra(0, 0), intra(1, 0)])
        for it in range(NT):
            gl = [rec(0, it), rec(1, it)]
            if it + 1 < NT:
                gl += [intra(0, it + 1), intra(1, it + 1)]
            run_rr(gl)
        for tt in range(NT):
            k.DMA("sp", gout[:], k.tm[tt * 128:(tt + 1) * 128, h * 128:(h + 1) * 128], r=[k.R("tm", tt)], w=[bgout])
            k.ACT(gout[:], gout[:], AF.Silu, r=[bgout], w=[bgout])
            k.TT("dve", osum[:], of_[:, tt, :], ob_[:, tt, :], ALU.add, r=[bof, bob], w=[bosum])
            k.ACT(fj[:], osum[:], AF.Square, accum=fssq[:], r=[bosum], w=[bfj, bfssq])
            rstd_from_ssq(k, frs[:], fssq[:], 128, bfrs, bfssq)
            k.STT("dve", osum[:], osum[:], frs[:, 0:1], ogb[:], ALU.mult, ALU.mult, r=[bosum, bfrs, bogb], w=[bosum])
            k.TT("dve", onb[:], osum[:], gout[:], ALU.mult, r=[bosum, bgout], w=[bonb])
            pz, bpz = k.ps[tt % 2]
            pzb = pz[:].bitcast(BF16)
            k.TR(pzb[:, 0:128], onb[:], k.identb[:], r=[bonb, k.bidentb], w=[bpz])
            k.CP("act", oCT[:, tt * 128:(tt + 1) * 128], pzb[:, 0:128], r=[bpz], w=[boCT])
        k.DMA("act", k.oT[1280 + h * 128:1280 + (h + 1) * 128, :], oCT[:], r=[boCT], w=[k.R("oT", 10 + h)])
        k.S.barrier()


def build(T, L, dbg=(), stages=None):
    k = KB(T, L, dbg, stages)
    nc = k.nc
    k.pT = k.dram("pT", [6144, T], F32)
    k.tm = k.dram("tm", [T, NTM], F32)
    k.gT = k.dram("gT", [6144, T], BF16)
    k.oT = k.dram("oT", [D, T], BF16)
    k.hbuf = k.dram("hbuf", [T, D], F32)
    k.xbuf = k.dram("xbuf", [T, D], F32)
    k.rotC = k.dram("rotC", [128, T], F32)
    k.hn = k.dram("hn", [T, D], BF16)
    k.xs = k.dram("xs", [NE * (2 * T // NE), D], BF16)
    k.ys = k.dram("ys", [NE * (2 * T // NE), D], F32)
    k.rotS = k.dram("rotS", [128, T], F32)
    st_setup(k)
    epsc, bepsc = k.sb("epsc", [128, 1], F32)
    k.MSET("dve", epsc[:], EPS, w=[bepsc])
    k.epsc, k.bepsc = epsc, bepsc
    k.sb_base = k.sb_ptr
    for l in range(L):
        xsrc = k.x_in if l == 0 else k.xbuf
        xdst = k.y_out if l == L - 1 else k.xbuf
        if k.on("proj"):
            st_proj(k, l, xsrc, "xres")
        if k.on("mixA"):
            st_mixA(k, l)
        if k.on("rot") and l == 0:
            st_rot(k)
        if k.on("attn"):
            st_attn(k, l)
        if k.on("delta"):
            st_delta(k, l)
        if k.stages is not None and "zeroC" in k.stages:
            k.stage_reset()
            zt, bz = k.sb("zt", [128, T], BF16)
            k.MSET("dve", zt[:], 0.0, w=[bz])
            for c in range(10, 16):
                k.DMA("sp", k.oT[c * 128:(c + 1) * 128, :], zt[:], r=[bz], w=[k.R("oT", c)])
        if k.on("merge"):
            st_merge(k, l, xsrc, "xres")
        if k.stages is not None and "copyh" in k.stages:
            k.stage_reset()
            ct, bct = k.sb("ct", [128, D], F32)
            for tt in range(T // 128):
                k.DMA("sp", ct[:], k.x_in[tt * 128:(tt + 1) * 128, :], w=[bct])
                for cb in range(4):
                    k.DMA("sp", k.hbuf[tt * 128:(tt + 1) * 128, cb * 512:(cb + 1) * 512], ct[:, cb * 512:(cb + 1) * 512],
                          r=[bct], w=[k.R("h", tt, cb)])
        if k.on("moe"):
            st_moe(k, l, xdst, "xres")
    k.S.finish()
    return k


T_FULL = 4096
L_FULL = 2
N_CORES = 4
_CACHE = {}


def kernel(**inputs):
    x = np.ascontiguousarray(inputs["x"], dtype=np.float32)
    pos = np.ascontiguousarray(inputs["positions"]).astype(np.int32)
    B = x.shape[0]
    if "k" not in _CACHE:
        _CACHE["k"] = build(T_FULL, L_FULL)
    k = _CACHE["k"]
    cst = make_consts()
    in_maps = []
    for c in range(N_CORES):
        b = c % B
        m = {"x": x[b], "pos": pos[b:b + 1], "cst": cst}
        for n in k.w:
            m[n] = np.ascontiguousarray(inputs[n], dtype=np.float32)
        in_maps.append(m)
    res = run_bass_kernel_spmd(k.nc, in_maps, core_ids=list(range(N_CORES)))
    out = np.stack([res.results[b]["y"] for b in range(B)], axis=0)
    return out.astype(np.float32)
```
